# Optimizing a Trainium2 kernel written in Bass

```python
import jax
import jax.numpy as jnp
from jax import lax
import numpy as np

D_MODEL = 1024
BATCH = 8
SEQ = 4096
DEPTH = 4

HEAD_DIM = 64
N_HEADS_A = 8
N_HEADS_B = 8
AB_WIDTH = (N_HEADS_A + N_HEADS_B) * HEAD_DIM
ROT_DIM = HEAD_DIM // 4
ROPE_THETA = 500000.0
MOBA_BLOCK = 256
MOBA_TOPK = 3
MOBA_QCHUNK = 16
DILATED_BRANCHES = ((128, 1), (512, 4), (2048, 16))
DILATED_QCHUNK = 32
HGRN_EXPAND = 128
N_HEADS_C = D_MODEL // HGRN_EXPAND
HGRN_DV = D_MODEL // N_HEADS_C
HGRN_CHUNK = 64
N_GROUPS = 4
EXPERTS_PER_GROUP = 8
N_EXPERTS = N_GROUPS * EXPERTS_PER_GROUP
TOPK_IN_GROUP = 2
D_EXPERT = D_MODEL // 2
MOE_BLOCK = 128
N_EVEN = (DEPTH + 1) // 2
N_ODD = DEPTH // 2
DEEPNORM_ALPHA = (2.0 * DEPTH) ** 0.25
DEEPNORM_BETA = (8.0 * DEPTH) ** -0.25
LN_EPS = 1e-5
RMS_EPS = 1e-6

kernel_name = 'hybrid_moba_dilated_hgrn2_hmoe'


def layer_norm(x, g, b):
    xf = x.astype(jnp.float32)
    mu = jnp.mean(xf, axis=-1, keepdims=True)
    var = jnp.mean(jnp.square(xf - mu), axis=-1, keepdims=True)
    return ((xf - mu) * lax.rsqrt(var + LN_EPS) * g + b).astype(x.dtype)


def rope_tables(seq):
    half = ROT_DIM // 2
    inv = ROPE_THETA ** (-jnp.arange(half, dtype=jnp.float32) / half)
    ang = jnp.arange(seq, dtype=jnp.float32)[:, None] * inv[None, :]
    return jnp.cos(ang), jnp.sin(ang)


def apply_partial_rope(x, cos, sin):
    half = ROT_DIM // 2
    c = cos.astype(x.dtype)
    s = sin.astype(x.dtype)
    x1 = x[..., :half]
    x2 = x[..., half:ROT_DIM]
    return jnp.concatenate([x1 * c - x2 * s, x2 * c + x1 * s, x[..., ROT_DIM:]], axis=-1)


def moba_attention(q, k, v):
    B, H, S, hd = q.shape
    nb = -(-S // MOBA_BLOCK)
    pad = nb * MOBA_BLOCK - S
    k_blk = jnp.pad(k, ((0, 0), (0, 0), (0, pad), (0, 0))).reshape(B, H, nb, MOBA_BLOCK, hd)
    v_blk = jnp.pad(v, ((0, 0), (0, 0), (0, pad), (0, 0))).reshape(B, H, nb, MOBA_BLOCK, hd)
    k_mean = jnp.mean(k_blk.astype(jnp.float32), axis=3)
    topk = min(MOBA_TOPK, nb - 1)
    scale = hd ** -0.5
    b_idx = jnp.arange(B)[:, None, None, None]
    h_idx = jnp.arange(H)[None, :, None, None]
    blk_ids = jnp.arange(nb)
    in_blk = jnp.arange(MOBA_BLOCK)

    def one_chunk(c):
        start = c * MOBA_QCHUNK
        qc = lax.dynamic_slice_in_dim(q, start, MOBA_QCHUNK, axis=2)
        t = start + jnp.arange(MOBA_QCHUNK)
        own = start // MOBA_BLOCK
        k_own = lax.dynamic_index_in_dim(k_blk, own, axis=2, keepdims=False)
        v_own = lax.dynamic_index_in_dim(v_blk, own, axis=2, keepdims=False)
        s_own = jnp.einsum('bhqd,bhkd->bhqk', qc, k_own).astype(jnp.float32) * scale
        s_own = jnp.where((own * MOBA_BLOCK + in_blk)[None, :] <= t[:, None], s_own, -jnp.inf)
        if topk == 0:
            p = jax.nn.softmax(s_own, axis=-1).astype(v.dtype)
            return jnp.einsum('bhqk,bhkd->bhqd', p, v_own)
        gate = jnp.einsum('bhqd,bhnd->bhqn', qc.astype(jnp.float32), k_mean)
        gate = jnp.where(blk_ids < own, gate, -jnp.inf)
        _, sel = lax.top_k(gate, topk)
        sel_ok = jnp.arange(topk) < own
        k_sel = k_blk[b_idx, h_idx, sel]
        v_sel = v_blk[b_idx, h_idx, sel]
        s_sel = jnp.einsum('bhqd,bhqnkd->bhqnk', qc, k_sel).astype(jnp.float32) * scale
        s_sel = jnp.where(sel_ok[:, None], s_sel, -jnp.inf)
        s = jnp.concatenate([s_sel.reshape(B, H, MOBA_QCHUNK, topk * MOBA_BLOCK), s_own], axis=-1)
        p = jax.nn.softmax(s, axis=-1).astype(v.dtype)
        p_sel = p[..., :topk * MOBA_BLOCK].reshape(B, H, MOBA_QCHUNK, topk, MOBA_BLOCK)
        p_own = p[..., topk * MOBA_BLOCK:]
        return (jnp.einsum('bhqnk,bhqnkd->bhqd', p_sel, v_sel)
                + jnp.einsum('bhqk,bhkd->bhqd', p_own, v_own))

    out = lax.map(one_chunk, jnp.arange(S // MOBA_QCHUNK))
    return jnp.moveaxis(out, 0, 2).reshape(B, H, S, hd)


def dilated_attention(q, k, v):
    B, H, S, hd = q.shape
    scale = hd ** -0.5

    def one_chunk(c):
        start = c * DILATED_QCHUNK
        qc = lax.dynamic_slice_in_dim(q, start, DILATED_QCHUNK, axis=2)
        t = start + jnp.arange(DILATED_QCHUNK)
        outs = []
        lses = []
        for window, dil in DILATED_BRANCHES:
            offs = dil * jnp.arange(window // dil + 1)
            idx = t[:, None] - offs[None, :]
            valid = idx >= 0
            idx = jnp.maximum(idx, 0)
            k_g = k[:, :, idx]
            v_g = v[:, :, idx]
            s = jnp.einsum('bhqd,bhqnd->bhqn', qc, k_g).astype(jnp.float32) * scale
            s = jnp.where(valid, s, -jnp.inf)
            m = jnp.max(s, axis=-1, keepdims=True)
            p = jnp.exp(s - m)
            den = jnp.sum(p, axis=-1, keepdims=True)
            outs.append(jnp.einsum('bhqn,bhqnd->bhqd', (p / den).astype(v.dtype), v_g))
            lses.append(m + jnp.log(den))
        w = jax.nn.softmax(jnp.concatenate(lses, axis=-1), axis=-1).astype(v.dtype)
        return jnp.einsum('bhqr,rbhqd->bhqd', w, jnp.stack(outs))

    out = lax.map(one_chunk, jnp.arange(S // DILATED_QCHUNK))
    return jnp.moveaxis(out, 0, 2).reshape(B, H, S, hd)


def attn_ab_mixer(x, w_in, w_out, cos, sin):
    B, S, _ = x.shape
    wa = N_HEADS_A * HEAD_DIM
    wb = N_HEADS_B * HEAD_DIM
    h = x @ w_in
    cuts = [wa, 2 * wa, 3 * wa, 3 * wa + wb, 3 * wa + 2 * wb]
    q_a, k_a, v_a, q_b, k_b, v_b = jnp.split(h, cuts, axis=-1)

    def heads(z, n):
        return z.reshape(B, S, n, HEAD_DIM).transpose(0, 2, 1, 3)

    o_a = moba_attention(apply_partial_rope(heads(q_a, N_HEADS_A), cos, sin),
                         apply_partial_rope(heads(k_a, N_HEADS_A), cos, sin),
                         heads(v_a, N_HEADS_A))
    o_b = dilated_attention(apply_partial_rope(heads(q_b, N_HEADS_B), cos, sin),
                            apply_partial_rope(heads(k_b, N_HEADS_B), cos, sin),
                            heads(v_b, N_HEADS_B))
    o = jnp.concatenate([o_a, o_b], axis=1).transpose(0, 2, 1, 3).reshape(B, S, AB_WIDTH)
    return o @ w_out


def hgrn2_mixer(x, w_in, w_out, norm_g, lb):
    B, S, D = x.shape
    H, dk, dv, C = N_HEADS_C, HGRN_EXPAND, HGRN_DV, HGRN_CHUNK
    h = x @ w_in
    q, fz, i, g = jnp.split(h, 4, axis=-1)

    def heads(z, d):
        return z.reshape(B, S, H, d).transpose(0, 2, 1, 3).astype(jnp.float32)

    q = heads(q, dk)
    fz = heads(fz, dk)
    v = heads(i, dv)
    lb_h = lb.astype(jnp.float32).reshape(H, dk)[None, :, None, :]
    log_f = jnp.logaddexp(jnp.log(lb_h), jnp.log1p(-lb_h) + jax.nn.log_sigmoid(fz))
    kk = (1.0 - lb_h) * jax.nn.sigmoid(-fz)
    nc = S // C

    def to_chunks(z):
        return z.reshape(B, H, nc, C, z.shape[-1]).transpose(2, 0, 1, 3, 4)

    causal = jnp.tril(jnp.ones((C, C), dtype=bool))

    def step(state, inp):
        qc, kc, vc, lfc = inp
        b = jnp.cumsum(lfc, axis=2)
        inter = jnp.einsum('bhcd,bhde->bhce', qc * jnp.exp(b), state)
        diff = b[:, :, :, None, :] - b[:, :, None, :, :]
        decay = jnp.exp(jnp.where(causal[:, :, None], diff, -jnp.inf))
        attn = jnp.einsum('bhid,bhijd,bhjd->bhij', qc, decay, kc)
        intra = jnp.einsum('bhij,bhje->bhie', attn, vc)
        b_last = b[:, :, -1:, :]
        state = (jnp.exp(b_last[:, :, 0, :])[..., None] * state
                 + jnp.einsum('bhjd,bhje->bhde', kc * jnp.exp(b_last - b), vc))
        return state, inter + intra

    s0 = jnp.zeros((B, H, dk, dv), jnp.float32)
    _, o = lax.scan(step, s0, (to_chunks(q), to_chunks(kk), to_chunks(v), to_chunks(log_f)))
    o = o.transpose(1, 2, 0, 3, 4).reshape(B, H, S, dv)
    o = o * lax.rsqrt(jnp.mean(jnp.square(o), axis=-1, keepdims=True) + RMS_EPS) * norm_g
    o = o.transpose(0, 2, 1, 3).reshape(B, S, D) * jax.nn.silu(g.astype(jnp.float32))
    return o.astype(x.dtype) @ w_out


def hier_moe(x, wg, bg, we, be, w1, w3, w2):
    B, S, D = x.shape
    xt = x.reshape(-1, D)
    T = xt.shape[0]
    lg = (xt @ wg).astype(jnp.float32) + bg
    grp = jnp.argmax(lg, axis=-1)
    pg = jnp.take_along_axis(jax.nn.softmax(lg, axis=-1), grp[:, None], axis=-1)
    le = ((xt @ we).astype(jnp.float32) + be).reshape(T, N_GROUPS, EXPERTS_PER_GROUP)
    le_g = jnp.take_along_axis(le, grp[:, None, None], axis=1)[:, 0]
    top_l, top_i = lax.top_k(le_g, TOPK_IN_GROUP)
    gate = pg * jax.nn.softmax(top_l, axis=-1)
    eid = grp[:, None] * EXPERTS_PER_GROUP + top_i
    A = T * TOPK_IN_GROUP
    eid_f = eid.reshape(-1)
    tok_f = jnp.repeat(jnp.arange(T), TOPK_IN_GROUP)
    wt_f = gate.reshape(-1)
    order = jnp.argsort(eid_f)
    eid_s = eid_f[order]
    tok_s = tok_f[order]
    wt_s = wt_f[order]
    counts = jax.ops.segment_sum(jnp.ones((A,), jnp.int32), eid_f, num_segments=N_EXPERTS)
    starts = jnp.cumsum(counts) - counts
    padded = (counts + MOE_BLOCK - 1) // MOE_BLOCK * MOE_BLOCK
    pends = jnp.cumsum(padded)
    pstarts = pends - padded
    dest = pstarts[eid_s] + jnp.arange(A) - starts[eid_s]
    n_blk = (A + N_EXPERTS * (MOE_BLOCK - 1) + MOE_BLOCK - 1) // MOE_BLOCK
    P = n_blk * MOE_BLOCK
    buf_tok = jnp.zeros((P,), jnp.int32).at[dest].set(tok_s.astype(jnp.int32))
    buf_wt = jnp.zeros((P,), jnp.float32).at[dest].set(wt_s)
    blk_exp = jnp.minimum(jnp.searchsorted(pends, jnp.arange(n_blk) * MOE_BLOCK, side='right'),
                          N_EXPERTS - 1)

    def expert_block(args):
        e, toks, wts = args
        xb = xt[toks]
        hb = jax.nn.silu(xb @ w1[e]) * (xb @ w3[e])
        return (hb @ w2[e]) * wts[:, None].astype(xb.dtype)

    yb = lax.map(expert_block, (blk_exp, buf_tok.reshape(n_blk, MOE_BLOCK),
                                buf_wt.reshape(n_blk, MOE_BLOCK)))
    y = jax.ops.segment_sum(yb.reshape(P, D), buf_tok, num_segments=T)
    return y.reshape(B, S, D)


def setup_inputs(seed: int = 0) -> dict:
    key = jax.random.key(seed)
    ks = jax.random.split(key, 17)
    f32 = jnp.float32
    D, F = D_MODEL, D_EXPERT
    nrm = lambda k, shape: jax.random.normal(k, shape, f32)
    return {
        'x': nrm(ks[0], (BATCH, SEQ, D)),
        'ab_w_in': nrm(ks[1], (N_EVEN, D, 3 * AB_WIDTH)) * D ** -0.5,
        'ab_w_out': nrm(ks[2], (N_EVEN, AB_WIDTH, D)) * AB_WIDTH ** -0.5 * DEEPNORM_BETA,
        'c_w_in': nrm(ks[3], (N_ODD, D, 4 * D)) * D ** -0.5,
        'c_w_out': nrm(ks[4], (N_ODD, D, D)) * D ** -0.5 * DEEPNORM_BETA,
        'c_norm_g': 1.0 + 0.02 * nrm(ks[5], (N_ODD, HGRN_DV)),
        'hgrn_lb_logits': 0.1 * nrm(ks[6], (N_ODD, D)),
        'ln_g': 1.0 + 0.02 * nrm(ks[7], (DEPTH, 2, D)),
        'ln_b': 0.02 * nrm(ks[8], (DEPTH, 2, D)),
        'router_g_w': nrm(ks[9], (DEPTH, D, N_GROUPS)) * D ** -0.5,
        'router_g_b': 0.01 * nrm(ks[10], (DEPTH, N_GROUPS)),
        'router_e_w': nrm(ks[11], (DEPTH, D, N_EXPERTS)) * D ** -0.5,
        'router_e_b': 0.01 * nrm(ks[12], (DEPTH, N_EXPERTS)),
        'exp_w1': nrm(ks[13], (DEPTH, N_EXPERTS, D, F)) * D ** -0.5,
        'exp_w3': nrm(ks[14], (DEPTH, N_EXPERTS, D, F)) * D ** -0.5,
        'exp_w2': nrm(ks[15], (DEPTH, N_EXPERTS, F, D)) * F ** -0.5 * DEEPNORM_BETA,
    }


def reference(x, ab_w_in, ab_w_out, c_w_in, c_w_out, c_norm_g, hgrn_lb_logits, ln_g, ln_b,
              router_g_w, router_g_b, router_e_w, router_e_b, exp_w1, exp_w3, exp_w2):
    S = x.shape[1]
    cos, sin = rope_tables(S)
    lb_all = jnp.cumsum(jax.nn.softmax(hgrn_lb_logits.astype(jnp.float32), axis=0), axis=0)
    lb_all = lb_all - lb_all[0:1]
    for l in range(DEPTH):
        j = l // 2
        if l % 2 == 0:
            mix = attn_ab_mixer(x, ab_w_in[j], ab_w_out[j], cos, sin)
        else:
            mix = hgrn2_mixer(x, c_w_in[j], c_w_out[j], c_norm_g[j], lb_all[j])
        x = layer_norm(DEEPNORM_ALPHA * x + mix, ln_g[l, 0], ln_b[l, 0])
        ffn = hier_moe(x, router_g_w[l], router_g_b[l], router_e_w[l], router_e_b[l],
                       exp_w1[l], exp_w3[l], exp_w2[l])
        x = layer_norm(DEEPNORM_ALPHA * x + ffn, ln_g[l, 1], ln_b[l, 1])
    return x
```

```python
import contextlib
import os
LVL = int(os.environ.get('DBG_LVL', '9'))
import numpy as np
import ml_dtypes
import concourse.bass as bass
import concourse.mybir as mybir
from concourse.bass_utils import run_bass_kernel_spmd

F32 = mybir.dt.float32
BF16 = mybir.dt.bfloat16
AF = mybir.ActivationFunctionType
ALU = mybir.AluOpType
AX = mybir.AxisListType

T = 4096
D = 1024
DEPTH = 4
ALPHA = float((2.0 * DEPTH) ** 0.25)
LN_EPS = 1e-5
RMS_EPS = 1e-6
BIG = 30000.0
EPOCH = 30000
NSLOT = 8
CAP = 512
NS = 32 * CAP
I32 = mybir.dt.int32


class Reg:
    __slots__ = ("w", "r", "x")

    def __init__(self, x=False):
        self.w = {}
        self.r = {}
        self.x = x


class Eng:
    def __init__(self, name, e, is_pe=False):
        self.name = name
        self.e = e
        self.count = 0
        self.known = {}
        self.is_pe = is_pe
        self.dcount = 0


class Sched:
    def __init__(self, nc):
        self.nc = nc
        self.E = {
            "pe": Eng("pe", nc.tensor, True),
            "act": Eng("act", nc.scalar),
            "dve": Eng("dve", nc.vector),
            "pool": Eng("pool", nc.gpsimd),
            "sp": Eng("sp", nc.sync),
        }
        self.semh = {}
        self.nsem = 0

    def _sem(self, key):
        h = self.semh.get(key)
        if h is None:
            h = self.nc.alloc_semaphore("s_%s_%s_%d" % key)
            self.semh[key] = h
            self.nsem += 1
        return h

    def _waits(self, E, reads, writes):
        deps = {}
        for r in reads:
            for k, v in r.w.items():
                if deps.get(k, 0) < v:
                    deps[k] = v
            if r.x:
                for k, v in r.r.items():
                    if deps.get(k, 0) < v:
                        deps[k] = v
        for w in writes:
            for k, v in w.w.items():
                if deps.get(k, 0) < v:
                    deps[k] = v
            for k, v in w.r.items():
                if deps.get(k, 0) < v:
                    deps[k] = v
        for k, v in deps.items():
            if E.is_pe and k[0] == "pe" and k[1] == "c":
                continue
            if E.known.get(k, 0) < v:
                E.e.wait_ge(self._sem(k), v)
                E.known[k] = v

    def op(self, en, fn, reads, writes):
        E = self.E[en]
        self._waits(E, reads, writes)
        ins = fn()
        key = (en, "c", E.count // EPOCH)
        val = E.count % EPOCH + 1
        ins.then_inc(self._sem(key), 1)
        E.count += 1
        for r in reads:
            if r.x:
                r.w = {key: val}
                r.r = {}
            else:
                r.r[key] = val
        for w in writes:
            w.w = {key: val}
            w.r = {}

    def pe(self, fn, reads, writes):
        self.op("pe", fn, reads, writes)

    def act(self, fn, reads, writes):
        self.op("act", fn, reads, writes)

    def dve(self, fn, reads, writes):
        self.op("dve", fn, reads, writes)

    def pool(self, fn, reads, writes):
        self.op("pool", fn, reads, writes)

    def dma(self, qn, out, in_, reads, writes):
        Q = self.E[qn]
        self._waits(Q, reads, writes)
        i = Q.dcount
        slot = i % NSLOT
        tgt = 16 * (i // NSLOT + 1)
        assert tgt < 60000
        key = (qn, "d", slot)
        if i >= NSLOT and Q.known.get(key, 0) < tgt - 16:
            Q.e.wait_ge(self._sem(key), tgt - 16)
            Q.known[key] = tgt - 16
        Q.e.dma_start(out=out, in_=in_).then_inc(self._sem(key), 16)
        Q.dcount += 1
        for r in reads:
            if r.r.get(key, 0) < tgt:
                r.r[key] = tgt
        for w in writes:
            w.w = {key: tgt}
            w.r = {}

    def idma(self, out, out_off, in_, in_off, bound, reads, writes):
        Q = self.E["pool"]
        self._waits(Q, reads, writes)
        i = Q.dcount
        slot = i % NSLOT
        tgt = 16 * (i // NSLOT + 1)
        assert tgt < 60000
        key = ("pool", "d", slot)
        if i >= NSLOT and Q.known.get(key, 0) < tgt - 16:
            Q.e.wait_ge(self._sem(key), tgt - 16)
            Q.known[key] = tgt - 16
        Q.e.indirect_dma_start(out=out, out_offset=out_off, in_=in_, in_offset=in_off).then_inc(self._sem(key), 16)
        Q.dcount += 1
        for r in reads:
            if r.r.get(key, 0) < tgt:
                r.r[key] = tgt
        for w in writes:
            w.w = {key: tgt}
            w.r = {}

    def barrier(self):
        latest = {}
        for E in self.E.values():
            if E.count > 0:
                latest[(E.name, "c", (E.count - 1) // EPOCH)] = (E.count - 1) % EPOCH + 1
            for slot in range(min(NSLOT, E.dcount)):
                n = (E.dcount - slot + NSLOT - 1) // NSLOT
                latest[(E.name, "d", slot)] = 16 * n
        for E in self.E.values():
            for k, v in latest.items():
                if E.is_pe and k[0] == "pe" and k[1] == "c":
                    continue
                if E.known.get(k, 0) < v:
                    E.e.wait_ge(self._sem(k), v)
                    E.known[k] = v


class Phase:
    cnt = 0

    def __init__(self, S):
        self.S = S
        self.nc = S.nc
        self.stack = contextlib.ExitStack()

    def sb(self, shape, dt):
        Phase.cnt += 1
        return self.stack.enter_context(self.nc.sbuf_tensor("sb%d" % Phase.cnt, list(shape), dt))

    def ps(self, shape, dt=F32):
        Phase.cnt += 1
        return self.stack.enter_context(self.nc.psum_tensor("ps%d" % Phase.cnt, list(shape), dt))

    def rot_sb(self, n, shape, dt):
        return Rot([(self.sb(shape, dt), Reg()) for _ in range(n)])

    def rot_ps(self, n, shape, dt=F32):
        return Rot([(self.ps(shape, dt), Reg(True)) for _ in range(n)])

    def close(self):
        self.S.barrier()
        self.stack.close()


class Rot:
    def __init__(self, items):
        self.items = items
        self.i = 0

    def next(self):
        it = self.items[self.i % len(self.items)]
        self.i += 1
        return it


def make_consts():
    bf = ml_dtypes.bfloat16
    c = {}
    c["ident_bf"] = np.eye(128, dtype=np.float32).astype(bf)
    c["ident_f"] = np.eye(128, dtype=np.float32)
    half = 8
    inv = (np.float32(500000.0) ** (-np.arange(half, dtype=np.float32) / np.float32(half))).astype(np.float32)
    ang = (np.arange(T, dtype=np.float32)[:, None] * inv[None, :]).astype(np.float32)
    cos = np.cos(ang.astype(np.float64)).astype(np.float32)
    sin = np.sin(ang.astype(np.float64)).astype(np.float32)
    c2 = np.concatenate([cos, cos], axis=1)
    s2 = np.concatenate([-sin, sin], axis=1)
    c2e = np.tile(c2, (1, 8))
    s2e = np.tile(s2, (1, 8))
    c["c2e"] = np.ascontiguousarray(c2e.reshape(32, 128, 128).transpose(1, 0, 2).reshape(128, 4096))
    c["s2e"] = np.ascontiguousarray(s2e.reshape(32, 128, 128).transpose(1, 0, 2).reshape(128, 4096))

    def mult(d):
        m = np.zeros_like(d, dtype=np.float32)
        m += ((d >= 0) & (d <= 128))
        m += ((d >= 0) & (d % 4 == 0) & (d <= 512))
        m += ((d >= 0) & (d % 16 == 0) & (d <= 2048))
        return m

    kl = np.arange(128)[:, None]
    ql = np.arange(512)[None, :]
    dm = np.zeros((128, 20, 512), np.float32)
    for di in range(20):
        delta = -384 + 128 * di
        dm[:, di, :] = mult(delta + ql - kl)
    c["dm"] = dm.reshape(128, 20 * 512).astype(bf)
    cm = np.zeros((128, 4, 512), np.float32)
    for ci in range(4):
        delta = -384 + 128 * ci
        cm[:, ci, :] = (delta + ql - kl >= 0)
    c["cm"] = cm.reshape(128, 4 * 512).astype(bf)
    koh = np.zeros((16, T), np.float32)
    for b in range(16):
        koh[b, b * 256:(b + 1) * 256] = 1.0
    c["koh"] = koh.astype(bf)
    tt = np.arange(32)[:, None]
    bb = np.arange(16)[None, :]
    own = tt // 2
    valid = (bb < own).astype(np.float32)
    negv = (valid - 1.0) * BIG
    ownm1 = (bb == own).astype(np.float32) - 1.0
    c["valid"] = np.ascontiguousarray(np.broadcast_to(valid.reshape(1, 512), (128, 512))).astype(np.float32)
    c["negv"] = np.ascontiguousarray(np.broadcast_to(negv.reshape(1, 512), (128, 512))).astype(np.float32)
    c["ownm1"] = np.ascontiguousarray(np.broadcast_to(ownm1.reshape(1, 512), (128, 512))).astype(np.float32)
    j = np.arange(64)[:, None]
    i = np.arange(64)[None, :]
    c["mg"] = ((j <= i).astype(np.float32) - (j <= 31).astype(np.float32)).astype(np.float32)
    ind = np.zeros((64, 3), np.float32)
    ind[:, 0] = (np.arange(64) <= 31)
    ind[:, 1] = 1.0
    ind[:, 2] = (np.arange(64) > 31)
    c["ind"] = ind
    c["cmask8"] = np.ascontiguousarray(np.tile((i >= j).astype(np.float32), (1, 8)))
    c["ones_f"] = np.ones((128, 64), np.float32)
    tp = np.arange(128)[:, None]
    tq = np.arange(128)[None, :]
    c["u_bf"] = (tp < tq).astype(np.float32).astype(bf)
    c["ones_bf"] = np.ones((128, 128), np.float32).astype(bf)
    ecb = np.tile((np.arange(32, dtype=np.float32) * CAP)[None, :], (1, 4))
    c["ecb"] = np.ascontiguousarray(np.broadcast_to(ecb, (128, 128))).astype(np.float32)
    c["pid"] = np.ascontiguousarray(np.broadcast_to((NS + np.arange(128, dtype=np.float32))[:, None], (128, 8)))
    return c


CONST_DT = {"ident_bf": BF16, "dm": BF16, "cm": BF16, "koh": BF16, "u_bf": BF16, "ones_bf": BF16}


class Prog:
    def __init__(self, nlayers=DEPTH, debug=None):
        self.nc = nc = bass.Bass("TRN2", target_bir_lowering=False)
        self.S = Sched(nc)
        self.nlayers = nlayers
        self.debug = debug
        self.consts_np = make_consts()
        di = lambda name, shape, dt=F32: nc.dram_tensor(name, list(shape), dt, kind="ExternalInput").ap()
        self.x_in = di("x", [T, D])
        self.ab_w_in = di("ab_w_in", [2, D, 3072])
        self.ab_w_out = di("ab_w_out", [2, D, D])
        self.c_w_in = di("c_w_in", [2, D, 4096])
        self.c_w_out = di("c_w_out", [2, D, D])
        self.cng = di("cng", [2, 64, D])
        self.lbl = di("lbl", [2, 64, D])
        self.lng = di("lng", [8, 128, D])
        self.lnb = di("lnb", [8, 128, D])
        self.rw = di("rw", [4, D, 36])
        self.rb = di("rb", [4, 128, 36])
        self.w1 = di("exp_w1", [4, 32, D, 512])
        self.w3 = di("exp_w3", [4, 32, D, 512])
        self.w2 = di("exp_w2", [4, 32, 512, D])
        self.cd = {}
        for k, v in self.consts_np.items():
            self.cd[k] = di("c_" + k, v.shape, CONST_DT.get(k, F32))
        self.out = nc.dram_tensor("out", [T, D], F32, kind="ExternalOutput").ap()
        dt_ = lambda name, shape, dt: nc.dram_tensor(name, list(shape), dt).ap()
        self.xres = dt_("xres", [T, D], F32)
        self.xres_r = [Reg() for _ in range(64)]
        self.qkt = dt_("qkt", [4, 512, T], BF16)
        self.qkt_r = Reg()
        self.vd = dt_("vd", [T, D], BF16)
        self.vd_r = Reg()
        self.ot = dt_("ot", [D, T], BF16)
        self.ot_r = Reg()
        self.xt = dt_("xt", [D, T], BF16)
        self.xt_r = Reg()
        self.ident_bf = nc.alloc_sbuf_tensor("ident_bf", [128, 128], BF16)
        self.ident_f = nc.alloc_sbuf_tensor("ident_f", [128, 128], F32)
        self.posg = nc.alloc_sbuf_tensor("posg", [128, 32, 2], I32)
        self.g12 = nc.alloc_sbuf_tensor("g12", [128, 32, 2], F32)
        self.pg_r = [Reg() for _ in range(8)]
        self.xs = dt_("xs", [NS + 128, D], BF16)
        self.xs_r = Reg()
        self.yb = dt_("yb", [NS + 128, D], F32)
        self.yb_r = Reg()
        self.cr = Reg()
        S = self.S
        S.dma("sp", self.ident_bf[:], self.cd["ident_bf"], [], [self.cr])
        r2 = Reg()
        S.dma("sp", self.ident_f[:], self.cd["ident_f"], [], [r2])
        self.cr2 = r2

    def xsrc(self, l):
        return self.x_in if l == 0 else self.xres

    def layer_norm_tile(self, P, np_, y, y_r, g_bc, b_bc, gb_r, out_t, out_r, scr):
        nc, S = self.nc, self.S
        st, st_r, mv, mv_r, rs, rs_r = scr
        S.dve(lambda: nc.vector.bn_stats(out=st[:np_, 0:6], in_=y[:np_, 0:512]), [y_r], [st_r])
        S.dve(lambda: nc.vector.bn_stats(out=st[:np_, 6:12], in_=y[:np_, 512:1024]), [y_r], [st_r])
        S.dve(lambda: nc.vector.bn_aggr(out=mv[:np_, :], in_=st[:np_, :]), [st_r], [mv_r])
        S.dve(lambda: nc.vector.tensor_scalar(out=rs[:np_, :], in0=mv[:np_, 1:2], scalar1=LN_EPS, scalar2=None,
                                              op0=ALU.add), [mv_r], [rs_r])
        S.act(lambda: nc.scalar.activation(out=rs[:np_, :], in_=rs[:np_, :], func=AF.Ln), [rs_r], [rs_r])
        S.act(lambda: nc.scalar.activation(out=rs[:np_, :], in_=rs[:np_, :], func=AF.Exp, scale=-0.5), [rs_r], [rs_r])
        S.dve(lambda: nc.vector.tensor_scalar(out=out_t[:np_, :], in0=y[:np_, :], scalar1=mv[:np_, 0:1],
                                              scalar2=rs[:np_, 0:1], op0=ALU.subtract, op1=ALU.mult),
              [y_r, mv_r, rs_r], [out_r])
        S.pool(lambda: nc.gpsimd.tensor_tensor(out=out_t[:np_, :], in0=out_t[:np_, :], in1=g_bc[:np_, :], op=ALU.mult),
               [out_r, gb_r], [out_r])
        S.pool(lambda: nc.gpsimd.tensor_tensor(out=out_t[:np_, :], in0=out_t[:np_, :], in1=b_bc[:np_, :], op=ALU.add),
               [out_r, gb_r], [out_r])

    def phase_attn_proj(self, l):
        nc, S = self.nc, self.S
        j = l // 2
        P = Phase(S)
        W = P.sb([128, 8, 3072], BF16)
        w_r = []
        for k in range(8):
            r = Reg()
            S.dma("pool", W[:, k, :], self.ab_w_in[j, k * 128:(k + 1) * 128, :], [], [r])
            w_r.append(r)
        c2e = P.sb([128, 32, 128], F32)
        s2e = P.sb([128, 32, 128], F32)
        tab_r = Reg()
        tab_r2 = Reg()
        S.dma("sp", c2e[:].rearrange("p a b -> p (a b)"), self.cd["c2e"], [], [tab_r])
        S.dma("sp", s2e[:].rearrange("p a b -> p (a b)"), self.cd["s2e"], [], [tab_r2])
        xs = P.rot_sb(2, [128, D], F32)
        xT = P.rot_sb(2, [128, 8, 128], BF16)
        ps_t = P.rot_ps(2, [128, 512], F32)
        ps_o = P.rot_ps(3, [128, 512], F32)
        ps_tr = P.rot_ps(2, [128, 1024], BF16)
        qs = P.rot_sb(3, [128, 512], BF16)
        t1 = P.rot_sb(2, [128, 8, 16], F32)
        t2 = P.rot_sb(2, [128, 8, 16], F32)
        stg = P.rot_sb(2, [128, 16, 512], BF16)
        vst = P.rot_sb(2, [128, 1024], BF16)
        src = self.xsrc(l)
        for g in range(8):
            stg_t, stg_r = stg.next()
            for tl in range(4):
                t = g * 4 + tl
                x_t, x_r = xs.next()
                S.dma("sp", x_t[:], src[t * 128:(t + 1) * 128, :], [self.xres_r[2 * t], self.xres_r[2 * t + 1]], [x_r])
                xT_t, xT_r = xT.next()
                for hb in range(2):
                    pt, pt_r = ps_t.next()
                    for kk in range(4):
                        k = hb * 4 + kk
                        S.pe(lambda: nc.tensor.transpose(pt[:, kk * 128:(kk + 1) * 128], x_t[:, k * 128:(k + 1) * 128],
                                                         self.ident_f[:]), [x_r, self.cr2], [pt_r])
                    S.act(lambda: nc.scalar.copy(out=xT_t[:, hb * 4:(hb + 1) * 4, :].rearrange("p a b -> p (a b)"),
                                                 in_=pt[:, :]), [pt_r], [xT_r])
                v_t, v_r = vst.next()
                for cg in range(6):
                    if LVL < 2:
                        break
                    po, po_r = ps_o.next()
                    for k in range(8):
                        S.pe(lambda: nc.tensor.matmul(po[:, :], lhsT=xT_t[:, k, :], rhs=W[:, k, cg * 512:(cg + 1) * 512],
                                                      start=(k == 0), stop=(k == 7)), [xT_r, w_r[k]], [po_r])
                    if cg in (2, 5):
                        off = 0 if cg == 2 else 512
                        S.act(lambda: nc.scalar.copy(out=v_t[:, off:off + 512], in_=po[:, :]), [po_r], [v_r])
                        continue
                    cgi = {0: 0, 1: 1, 3: 2, 4: 3}[cg]
                    if LVL < 3:
                        continue
                    q_t, q_r = qs.next()
                    S.act(lambda: nc.scalar.copy(out=q_t[:, :], in_=po[:, :]), [po_r], [q_r])
                    a1, a1_r = t1.next()
                    a2, a2_r = t2.next()
                    pov = po[:, :].rearrange("p (h d) -> p h d", h=8)
                    qv = q_t[:, :].rearrange("p (h d) -> p h d", h=8)
                    cv = c2e[:, t, :].rearrange("p (h d) -> p h d", h=8)
                    sv = s2e[:, t, :].rearrange("p (h d) -> p h d", h=8)
                    S.dve(lambda: nc.vector.tensor_tensor(out=a1[:, :, :], in0=pov[:, :, 0:16], in1=cv, op=ALU.mult),
                          [po_r, tab_r], [a1_r])
                    S.dve(lambda: nc.vector.tensor_tensor(out=a2[:, :, 0:8], in0=pov[:, :, 8:16], in1=sv[:, :, 0:8],
                                                          op=ALU.mult), [po_r, tab_r2], [a2_r])
                    S.dve(lambda: nc.vector.tensor_tensor(out=a2[:, :, 8:16], in0=pov[:, :, 0:8], in1=sv[:, :, 8:16],
                                                          op=ALU.mult), [po_r, tab_r2], [a2_r])
                    S.dve(lambda: nc.vector.tensor_tensor(out=qv[:, :, 0:16], in0=a1[:, :, :], in1=a2[:, :, :], op=ALU.add),
                          [a1_r, a2_r], [q_r])
                    if LVL < 4:
                        continue
                    ptr, ptr_r = ps_tr.next()
                    for pr in range(4):
                        S.pe(lambda: nc.tensor.transpose(ptr[:, pr * 128:(pr + 1) * 128], q_t[:, pr * 128:(pr + 1) * 128],
                                                         self.ident_bf[:]), [q_r, self.cr], [ptr_r])
                    S.dve(lambda: nc.vector.tensor_copy(
                        out=stg_t[:, cgi * 4:(cgi + 1) * 4, tl * 128:(tl + 1) * 128],
                        in_=ptr[:, 0:512].rearrange("p (a b) -> p a b", a=4)), [ptr_r], [stg_r])
                if LVL >= 2:
                    S.dma("act", self.vd[t * 128:(t + 1) * 128, :], v_t[:, :], [v_r], [self.vd_r])
            for cgi in range(4):
                if LVL < 5:
                    break
                S.dma("sp", self.qkt[cgi, :, g * 512:(g + 1) * 512].rearrange("(a p) t -> p a t", p=128),
                      stg_t[:, cgi * 4:(cgi + 1) * 4, :], [stg_r], [self.qkt_r])
        P.close()

    def phase_attn(self, l):
        nc, S = self.nc, self.S
        P = Phase(S)
        dm = P.sb([128, 20, 512], BF16)
        cm = P.sb([128, 4, 512], BF16)
        m_r = Reg()
        m_r2 = Reg()
        S.dma("sp", dm[:].rearrange("p a b -> p (a b)"), self.cd["dm"], [], [m_r])
        S.dma("sp", cm[:].rearrange("p a b -> p (a b)"), self.cd["cm"], [], [m_r2])
        valid = P.sb([128, 512], F32)
        negv = P.sb([128, 512], F32)
        ownm1 = P.sb([128, 512], F32)
        ones_f = P.sb([128, 64], F32)
        k_r = Reg()
        for tl_, nm in ((valid, "valid"), (negv, "negv"), (ownm1, "ownm1"), (ones_f, "ones_f")):
            r = Reg()
            S.dma("sp", tl_[:], self.cd[nm], [], [r])
            k_r = r
        cst_r = Reg()
        QT = [P.sb([128, T], BF16) for _ in range(2)]
        KT = [P.sb([128, T], BF16) for _ in range(2)]
        VA = [P.sb([128, 32, 65], BF16) for _ in range(2)]
        QT_r = [Reg(), Reg()]
        QTb_r = [Reg(), Reg()]
        KT_r = [Reg(), Reg()]
        KTc_r = [Reg(), Reg()]
        VA_r = [[Reg() for _ in range(4)] for _ in range(2)]
        VAo_r = [Reg(), Reg()]
        for b in range(2):
            S.pool(lambda: nc.gpsimd.memset(QT[b][0:64, :], 0.0), [], [QTb_r[b]])
            S.pool(lambda: nc.gpsimd.memset(KT[b][0:64, :], 0.0), [], [KTc_r[b]])
            S.dma("sp", KT[b][0:16, :], self.cd["koh"], [KTc_r[b]], [KTc_r[b]])
            S.pool(lambda: nc.gpsimd.memset(VA[b][:, :, 64:65], 1.0), [], [VAo_r[b]])
        S.barrier()
        ps_s = P.rot_ps(3, [128, 512], F32)
        ps_acc = P.rot_ps(2, [128, 512], F32)
        ps_g = P.ps([128, 512], F32)
        ps_g_r = Reg(True)
        ps_b = P.rot_ps(1, [128, 1024], BF16)
        pb = P.rot_sb(4, [128, 512], BF16)
        km = P.sb([128, 16], F32)
        kmh = P.sb([128, 16], BF16)
        kml = P.sb([128, 16], BF16)
        kmt = P.sb([128, 16], F32)
        km_r = Reg()
        gm = P.sb([128, 32, 16], F32)
        gm_r = Reg()
        m8 = P.sb([128, 32, 8], F32)
        m8_r = Reg()
        sel = P.sb([128, 32, 16], F32)
        sel_r = Reg()
        bia = P.sb([128, 32, 16], BF16)
        bia_r = Reg()
        den = P.rot_sb(2, [128, 512], F32)
        bcs = P.rot_sb(2, [64, 512], F32)
        ost = P.rot_sb(2, [64, 512], BF16)
        def prep_steps(h):
            b = h % 2
            moba = h < 8
            qc, kc = (0, 1) if moba else (2, 3)
            hh = h % 8
            steps = []

            def loads():
                S.dma("sp", QT[b][64:128, :], self.qkt[qc, hh * 64:(hh + 1) * 64, :], [self.qkt_r], [QT_r[b]])
                S.dma("sp", KT[b][64:128, :], self.qkt[kc, hh * 64:(hh + 1) * 64, :], [self.qkt_r], [KT_r[b]])
                for q4 in range(4):
                    S.dma("sp", VA[b][:, q4 * 8:(q4 + 1) * 8, 0:64],
                          self.vd[q4 * 1024:(q4 + 1) * 1024, h * 64:(h + 1) * 64].rearrange("(t p) c -> p t c", p=128),
                          [self.vd_r], [VA_r[b][q4]])
            steps.append(loads)
            if not moba:
                return steps

            def gate1():
                S.dve(lambda: nc.vector.tensor_reduce(out=km[64:128, :],
                                                      in_=KT[b][64:128, :].rearrange("p (a c) -> p a c", a=16),
                                                      axis=AX.X, op=ALU.add), [KT_r[b]], [km_r])
                S.dve(lambda: nc.vector.tensor_scalar(out=km[64:128, :], in0=km[64:128, :], scalar1=1.0 / 256.0, scalar2=None,
                                                      op0=ALU.mult), [km_r], [km_r])
                S.dve(lambda: nc.vector.tensor_copy(out=kmh[64:128, :], in_=km[64:128, :]), [km_r], [km_r])
                S.dve(lambda: nc.vector.tensor_tensor(out=kmt[64:128, :], in0=km[64:128, :], in1=kmh[64:128, :],
                                                      op=ALU.subtract), [km_r], [km_r])
                S.dve(lambda: nc.vector.tensor_copy(out=kml[64:128, :], in_=kmt[64:128, :]), [km_r], [km_r])
                for t in range(32):
                    S.pe(lambda: nc.tensor.matmul(ps_g[:, t * 16:(t + 1) * 16], lhsT=QT[b][64:128, t * 128:(t + 1) * 128],
                                                  rhs=kmh[64:128, :], start=True, stop=False), [QT_r[b], km_r], [ps_g_r])
                    S.pe(lambda: nc.tensor.matmul(ps_g[:, t * 16:(t + 1) * 16], lhsT=QT[b][64:128, t * 128:(t + 1) * 128],
                                                  rhs=kml[64:128, :], start=False, stop=True), [QT_r[b], km_r], [ps_g_r])
                gmf = gm[:].rearrange("p a b -> p (a b)")
                S.dve(lambda: nc.vector.tensor_tensor(out=gmf, in0=ps_g[:, :], in1=negv[:], op=ALU.add), [ps_g_r], [gm_r])
            steps.append(gate1)

            def gate2():
                for t in range(32):
                    S.dve(lambda: nc.vector.max(out=m8[:, t, :], in_=gm[:, t, :]), [gm_r], [m8_r])
            steps.append(gate2)

            def gate3():
                S.dve(lambda: nc.vector.tensor_tensor(out=sel[:, :, :], in0=gm[:, :, :],
                                                      in1=m8[:, :, 2:3].to_broadcast([128, 32, 16]), op=ALU.is_ge),
                      [gm_r, m8_r], [sel_r])
                self_f = sel[:].rearrange("p a b -> p (a b)")
                S.dve(lambda: nc.vector.tensor_tensor(out=self_f, in0=self_f, in1=valid[:], op=ALU.mult), [sel_r], [sel_r])
                S.dve(lambda: nc.vector.tensor_tensor(out=self_f, in0=self_f, in1=ownm1[:], op=ALU.add), [sel_r], [sel_r])
                S.dve(lambda: nc.vector.tensor_scalar(out=bia[:].rearrange("p a b -> p (a b)"), in0=self_f, scalar1=BIG,
                                                      scalar2=None, op0=ALU.mult), [sel_r], [bia_r])
            steps.append(gate3)

            def mk_tr(g4):
                def tr():
                    pbt, pbt_r = ps_b.next()
                    for tt in range(8):
                        t = g4 * 8 + tt
                        S.pe(lambda: nc.tensor.transpose(pbt[0:16, tt * 128:(tt + 1) * 128], bia[:, t, :], self.ident_bf[:]),
                             [bia_r], [pbt_r])
                    S.act(lambda: nc.scalar.copy(out=QT[b][0:16, g4 * 1024:(g4 + 1) * 1024], in_=pbt[0:16, :]),
                          [pbt_r], [QTb_r[b]])
                return tr
            for g4 in range(4):
                steps.append(mk_tr(g4))
            return steps

        def attend(h, steps):
            b = h % 2
            moba = h < 8
            lo = 0 if moba else 64
            blocks = []
            for Q in range(8):
                ms = list(range(0, 4 * Q + 4)) if moba else list(range(max(0, 4 * Q - 16), 4 * Q + 4))
                for mi, m in enumerate(ms):
                    blocks.append((Q, mi, m, len(ms)))
            n = len(blocks)
            every = max(1, n // (len(steps) + 1)) if steps else n + 1
            sq = {}
            LOOK = 2

            def issue_S(i):
                Q, mi, m, nm = blocks[i]
                sp_, sp_r = ps_s.next()
                S.pe(lambda: nc.tensor.matmul(sp_[:, :], lhsT=KT[b][lo:128, m * 128:(m + 1) * 128],
                                              rhs=QT[b][lo:128, Q * 512:(Q + 1) * 512], start=True, stop=True),
                     [KT_r[b], KTc_r[b], QT_r[b], QTb_r[b]], [sp_r])
                sq[i] = (sp_, sp_r)

            fin = []

            def fin_pe(Q, acc, acc_r, dn, dn_r):
                bp, bp_r = ps_s.next()
                S.pe(lambda: nc.tensor.matmul(bp[0:64, :], lhsT=ones_f[64:65, 0:64], rhs=dn[64:65, :], start=True, stop=True),
                     [dn_r], [bp_r])
                bc, bc_r = bcs.next()
                S.act(lambda: nc.scalar.copy(out=bc[:, :], in_=bp[0:64, :]), [bp_r], [bc_r])
                o_t, o_r = ost.next()
                S.dve(lambda: nc.vector.tensor_tensor(out=o_t[:, :], in0=acc[0:64, :], in1=bc[:, :], op=ALU.mult),
                      [acc_r, bc_r], [o_r])
                S.dma("act", self.ot[h * 64:(h + 1) * 64, Q * 512:(Q + 1) * 512], o_t[:, :], [o_r], [self.ot_r])

            for i in range(min(LOOK, n)):
                issue_S(i)
            acc = acc_r = None
            for i in range(n):
                Q, mi, m, nm = blocks[i]
                di_ = (512 * Q - 128 * m + 384) // 128
                sp_, sp_r = sq.pop(i)
                p_t, p_r = pb.next()
                S.act(lambda: nc.scalar.activation(out=p_t[:, :], in_=sp_[:, :], func=AF.Exp, scale=0.125), [sp_r], [p_r])
                if moba:
                    if di_ <= 3:
                        S.dve(lambda: nc.vector.tensor_tensor(out=p_t[:, :], in0=p_t[:, :], in1=cm[:, di_, :], op=ALU.mult),
                              [p_r], [p_r])
                else:
                    S.dve(lambda: nc.vector.tensor_tensor(out=p_t[:, :], in0=p_t[:, :], in1=dm[:, di_, :], op=ALU.mult),
                          [p_r], [p_r])
                if i + LOOK < n:
                    issue_S(i + LOOK)
                if mi == 0:
                    acc, acc_r = ps_acc.next()
                S.pe(lambda: nc.tensor.matmul(acc[0:65, :], lhsT=VA[b][:, m, :], rhs=p_t[:, :],
                                              start=(mi == 0), stop=(mi == nm - 1)), [p_r, VA_r[b][m // 8], VAo_r[b]], [acc_r])
                for f in fin:
                    f[0] -= 1
                while fin and fin[0][0] <= 0:
                    f = fin.pop(0)
                    fin_pe(*f[1:])
                if mi == nm - 1:
                    dn, dn_r = den.next()
                    S.act(lambda: nc.scalar.copy(out=dn[64:65, :], in_=acc[64:65, :]), [acc_r], [dn_r])
                    S.dve(lambda: nc.vector.reciprocal(out=dn[64:65, :], in_=dn[64:65, :]), [dn_r], [dn_r])
                    fin.append([2, Q, acc, acc_r, dn, dn_r])
                if steps and (i + 1) % every == 0:
                    steps.pop(0)()
            while fin:
                f = fin.pop(0)
                fin_pe(*f[1:])
            while steps:
                steps.pop(0)()

        for st_ in prep_steps(0):
            st_()
        for h in range(16):
            nxt = prep_steps(h + 1) if h + 1 < 16 else []
            attend(h, nxt)
        P.close()


    def phase_hgrn(self, l):
        nc, S = self.nc, self.S
        j = l // 2
        P = Phase(S)
        V = nc.vector
        G_ = nc.gpsimd
        W = P.sb([128, 8, 4096], BF16)
        w_r = []
        for k in range(8):
            r = Reg()
            S.dma("pool", W[:, k, :], self.c_w_in[j, k * 128:(k + 1) * 128, :], [], [r])
            w_r.append(r)
        mg = P.sb([64, 64], F32)
        ind = P.sb([64, 3], F32)
        cmask8 = P.sb([64, 512], F32)
        cng = P.sb([64, D], F32)
        lb = P.sb([64, D], F32)
        oml = P.sb([64, D], F32)
        l0 = P.sb([64, D], F32)
        cr = Reg()
        for tl_, ap_ in ((mg, self.cd["mg"]), (ind, self.cd["ind"]), (cmask8, self.cd["cmask8"]), (cng, self.cng[j]),
                         (lb, self.lbl[1]), (l0, self.lbl[0])):
            S.dma("sp", tl_[:], ap_, [], [Reg()])
        S.barrier()
        if j == 0:
            S.dve(lambda: V.memset(lb[:], 0.0), [], [cr])
            S.dve(lambda: V.memset(oml[:], 1.0), [], [cr])
        else:
            S.dve(lambda: V.tensor_tensor(out=lb[:], in0=lb[:], in1=l0[:], op=ALU.subtract), [], [cr])
            S.act(lambda: nc.scalar.activation(out=lb[:], in_=lb[:], func=AF.Sigmoid), [cr], [cr])
            S.dve(lambda: V.tensor_scalar(out=oml[:], in0=lb[:], scalar1=-1.0, scalar2=1.0, op0=ALU.mult, op1=ALU.add),
                  [cr], [cr])
        Sst = P.sb([128, 8, 128], F32)
        Sst_r = Reg()
        S.dve(lambda: V.memset(Sst[:].rearrange("p a b -> p (a b)"), 0.0), [], [Sst_r])
        S.barrier()
        banks = P.rot_ps(6, [128, 512], F32)
        pbf = P.rot_ps(2, [128, 1024], BF16)
        xs = P.rot_sb(2, [64, D], F32)
        xT = P.rot_sb(2, [128, 8, 64], BF16)
        fbuf = P.rot_sb(1, [64, D], F32)
        lfb = P.rot_sb(1, [64, D], F32)
        kkb = P.rot_sb(1, [64, D], F32)
        eGb = P.rot_sb(1, [64, D], F32)
        enGb = P.rot_sb(1, [64, D], F32)
        qtb = P.rot_sb(2, [64, D], BF16)
        ktb = P.rot_sb(2, [64, D], BF16)
        vb = P.rot_sb(2, [64, D], BF16)
        sgb = P.rot_sb(2, [64, D], F32)
        abcb = P.rot_sb(2, [128, 8, 3], F32)
        qTb = P.rot_sb(2, [128, 8, 64], BF16)
        kTb = P.rot_sb(2, [128, 8, 64], BF16)
        ATb = P.rot_sb(2, [64, 512], BF16)
        smb = P.rot_sb(2, [128, 8, 128], BF16)
        tmpb = P.rot_sb(1, [128, 8, 128], F32)
        sqb = P.rot_sb(1, [64, D], F32)
        msb = P.rot_sb(2, [64, 16], F32)
        onb = P.rot_sb(1, [64, D], F32)
        ogb = P.rot_sb(2, [64, D], BF16)
        stg = P.rot_sb(2, [128, 8, 512], BF16)
        src = self.xsrc(l)
        idf = self.ident_f
        idb = self.ident_bf
        for g in range(8):
            stg_t, stg_r = stg.next()
            for tl in range(8):
                t = g * 8 + tl
                x_t, x_r = xs.next()
                S.dma("sp", x_t[:], src[t * 64:(t + 1) * 64, :], [self.xres_r[t]], [x_r])
                pt, pt_r = banks.next()
                for k in range(8):
                    S.pe(lambda: nc.tensor.transpose(pt[:, k * 64:(k + 1) * 64], x_t[:, k * 128:(k + 1) * 128], idf[0:64, 0:64]),
                         [x_r], [pt_r])
                xT_t, xT_r = xT.next()
                S.act(lambda: nc.scalar.copy(out=xT_t[:].rearrange("p a b -> p (a b)"), in_=pt[:, :]), [pt_r], [xT_r])

                def proj(cg):
                    po, po_r = banks.next()
                    for k in range(8):
                        S.pe(lambda: nc.tensor.matmul(po[0:64, :], lhsT=xT_t[:, k, :], rhs=W[:, k, cg * 512:(cg + 1) * 512],
                                                      start=(k == 0), stop=(k == 7)), [xT_r, w_r[k]], [po_r])
                    return po, po_r

                f_t, f_r = fbuf.next()
                for hf in range(2):
                    po, po_r = proj(2 + hf)
                    S.act(lambda: nc.scalar.activation(out=f_t[:, hf * 512:(hf + 1) * 512], in_=po[0:64, :], func=AF.Sigmoid),
                          [po_r], [f_r])
                S.dve(lambda: V.tensor_tensor(out=f_t[:], in0=f_t[:], in1=oml[:], op=ALU.mult), [f_r, cr], [f_r])
                S.dve(lambda: V.tensor_tensor(out=f_t[:], in0=f_t[:], in1=lb[:], op=ALU.add), [f_r, cr], [f_r])
                lf_t, lf_r = lfb.next()
                S.act(lambda: nc.scalar.activation(out=lf_t[:], in_=f_t[:], func=AF.Ln), [f_r], [lf_r])
                kk_t, kk_r = kkb.next()
                S.pool(lambda: G_.tensor_scalar(out=kk_t[:], in0=f_t[:], scalar1=-1.0, scalar2=1.0, op0=ALU.mult, op1=ALU.add),
                       [f_r], [kk_r])
                eG_t, eG_r = eGb.next()
                enG_t, enG_r = enGb.next()
                for hf in range(2):
                    pg_, pg_r = banks.next()
                    S.pe(lambda: nc.tensor.matmul(pg_[0:64, :], lhsT=mg[:, :], rhs=lf_t[:, hf * 512:(hf + 1) * 512],
                                                  start=True, stop=True), [lf_r], [pg_r])
                    S.act(lambda: nc.scalar.activation(out=eG_t[:, hf * 512:(hf + 1) * 512], in_=pg_[0:64, :], func=AF.Exp),
                          [pg_r], [eG_r])
                    S.act(lambda: nc.scalar.activation(out=enG_t[:, hf * 512:(hf + 1) * 512], in_=pg_[0:64, :], func=AF.Exp,
                                                       scale=-1.0), [pg_r], [enG_r])
                pst, pst_r = banks.next()
                for h in range(8):
                    S.pe(lambda: nc.tensor.matmul(pst[:, h * 3:(h + 1) * 3], lhsT=lf_t[:, h * 128:(h + 1) * 128], rhs=ind[:, :],
                                                  start=True, stop=True), [lf_r], [pst_r])
                abc, abc_r = abcb.next()
                S.act(lambda: nc.scalar.activation(out=abc[:].rearrange("p a b -> p (a b)"), in_=pst[:, 0:24], func=AF.Exp),
                      [pst_r], [abc_r])
                qt_t, qt_r = qtb.next()
                for hf in range(2):
                    po, po_r = proj(hf)
                    S.dve(lambda: V.tensor_tensor(out=qt_t[:, hf * 512:(hf + 1) * 512], in0=po[0:64, :],
                                                  in1=eG_t[:, hf * 512:(hf + 1) * 512], op=ALU.mult), [po_r, eG_r], [qt_r])
                kt_t, kt_r = ktb.next()
                S.pool(lambda: G_.tensor_tensor(out=kt_t[:], in0=kk_t[:], in1=enG_t[:], op=ALU.mult), [kk_r, enG_r], [kt_r])
                v_t, v_r = vb.next()
                for hf in range(2):
                    po, po_r = proj(4 + hf)
                    S.act(lambda: nc.scalar.copy(out=v_t[:, hf * 512:(hf + 1) * 512], in_=po[0:64, :]), [po_r], [v_r])
                sg_t, sg_r = sgb.next()
                for hf in range(2):
                    po, po_r = proj(6 + hf)
                    S.act(lambda: nc.scalar.activation(out=sg_t[:, hf * 512:(hf + 1) * 512], in_=po[0:64, :], func=AF.Silu),
                          [po_r], [sg_r])
                qT_t, qT_r = qTb.next()
                kT_t, kT_r = kTb.next()
                for (src_t, src_r, dst_t, dst_r) in ((qt_t, qt_r, qT_t, qT_r), (kt_t, kt_r, kT_t, kT_r)):
                    pb_, pb_r = pbf.next()
                    for h in range(8):
                        S.pe(lambda: nc.tensor.transpose(pb_[:, h * 64:(h + 1) * 64], src_t[:, h * 128:(h + 1) * 128],
                                                         idb[0:64, 0:64]), [src_r], [pb_r])
                    S.dve(lambda: V.tensor_copy(out=dst_t[:].rearrange("p a b -> p (a b)"), in_=pb_[:, 0:512]), [pb_r], [dst_r])
                pat, pat_r = banks.next()
                for h in range(8):
                    S.pe(lambda: nc.tensor.matmul(pat[0:64, h * 64:(h + 1) * 64], lhsT=kT_t[:, h, :], rhs=qT_t[:, h, :],
                                                  start=True, stop=True), [kT_r, qT_r], [pat_r])
                AT_t, AT_r = ATb.next()
                S.dve(lambda: V.tensor_tensor(out=AT_t[:, :], in0=pat[0:64, :], in1=cmask8[:, :], op=ALU.mult), [pat_r], [AT_r])
                sm_t, sm_r = smb.next()
                S.pool(lambda: G_.tensor_tensor(out=sm_t[:, :, :], in0=Sst[:, :, :],
                                                in1=abc[:, :, 0:1].to_broadcast([128, 8, 128]), op=ALU.mult),
                       [Sst_r, abc_r], [sm_r])
                po0, po0_r = banks.next()
                po1, po1_r = banks.next()
                for h in range(8):
                    ob, ob_r = (po0, po0_r) if h < 4 else (po1, po1_r)
                    c0 = (h % 4) * 128
                    S.pe(lambda: nc.tensor.matmul(ob[0:64, c0:c0 + 128], lhsT=qT_t[:, h, :], rhs=sm_t[:, h, :],
                                                  start=True, stop=False), [qT_r, sm_r], [ob_r])
                    S.pe(lambda: nc.tensor.matmul(ob[0:64, c0:c0 + 128], lhsT=AT_t[:, h * 64:(h + 1) * 64],
                                                  rhs=v_t[:, h * 128:(h + 1) * 128], start=False, stop=True),
                         [AT_r, v_r], [ob_r])
                pk0, pk0_r = banks.next()
                pk1, pk1_r = banks.next()
                for h in range(8):
                    kb, kb_r = (pk0, pk0_r) if h < 4 else (pk1, pk1_r)
                    c0 = (h % 4) * 128
                    S.pe(lambda: nc.tensor.matmul(kb[:, c0:c0 + 128], lhsT=kt_t[:, h * 128:(h + 1) * 128],
                                                  rhs=v_t[:, h * 128:(h + 1) * 128], start=True, stop=True),
                         [kt_r, v_r], [kb_r])
                tmp_t, tmp_r = tmpb.next()
                for hb, (kb, kb_r) in enumerate(((pk0, pk0_r), (pk1, pk1_r))):
                    S.dve(lambda: V.tensor_tensor(out=tmp_t[:, hb * 4:(hb + 1) * 4, :],
                                                  in0=kb[:, :].rearrange("p (a b) -> p a b", a=4),
                                                  in1=abc[:, hb * 4:(hb + 1) * 4, 2:3].to_broadcast([128, 4, 128]), op=ALU.mult),
                          [kb_r, abc_r], [tmp_r])
                S.pool(lambda: G_.tensor_tensor(out=Sst[:, :, :], in0=Sst[:, :, :],
                                                in1=abc[:, :, 1:2].to_broadcast([128, 8, 128]), op=ALU.mult),
                       [Sst_r, abc_r], [Sst_r])
                S.pool(lambda: G_.tensor_tensor(out=Sst[:, :, :], in0=Sst[:, :, :], in1=tmp_t[:, :, :], op=ALU.add),
                       [Sst_r, tmp_r], [Sst_r])
                sq_t, sq_r = sqb.next()
                for hb, (ob, ob_r) in enumerate(((po0, po0_r), (po1, po1_r))):
                    S.act(lambda: nc.scalar.activation(out=sq_t[:, hb * 512:(hb + 1) * 512], in_=ob[0:64, :], func=AF.Square),
                          [ob_r], [sq_r])
                ms_t, ms_r = msb.next()
                S.dve(lambda: V.tensor_reduce(out=ms_t[:, 0:8], in_=sq_t[:].rearrange("p (a b) -> p a b", a=8), axis=AX.X,
                                              op=ALU.add), [sq_r], [ms_r])
                S.dve(lambda: V.tensor_scalar(out=ms_t[:, 0:8], in0=ms_t[:, 0:8], scalar1=1.0 / 128.0, scalar2=RMS_EPS,
                                              op0=ALU.mult, op1=ALU.add), [ms_r], [ms_r])
                S.act(lambda: nc.scalar.activation(out=ms_t[:, 0:8], in_=ms_t[:, 0:8], func=AF.Ln), [ms_r], [ms_r])
                S.act(lambda: nc.scalar.activation(out=ms_t[:, 8:16], in_=ms_t[:, 0:8], func=AF.Exp, scale=-0.5), [ms_r], [ms_r])
                on_t, on_r = onb.next()
                for hb, (ob, ob_r) in enumerate(((po0, po0_r), (po1, po1_r))):
                    S.dve(lambda: V.tensor_tensor(out=on_t[:, hb * 512:(hb + 1) * 512].rearrange("p (a b) -> p a b", a=4),
                                                  in0=ob[0:64, :].rearrange("p (a b) -> p a b", a=4),
                                                  in1=ms_t[:, 8 + hb * 4:8 + (hb + 1) * 4].unsqueeze(2).to_broadcast([64, 4, 128]),
                                                  op=ALU.mult), [ob_r, ms_r], [on_r])
                S.pool(lambda: G_.tensor_tensor(out=on_t[:], in0=on_t[:], in1=cng[:], op=ALU.mult), [on_r], [on_r])
                og_t, og_r = ogb.next()
                S.pool(lambda: G_.tensor_tensor(out=og_t[:], in0=on_t[:], in1=sg_t[:], op=ALU.mult), [on_r, sg_r], [og_r])
                pb_, pb_r = pbf.next()
                for h in range(8):
                    S.pe(lambda: nc.tensor.transpose(pb_[:, h * 64:(h + 1) * 64], og_t[:, h * 128:(h + 1) * 128], idb[0:64, 0:64]),
                         [og_r], [pb_r])
                S.act(lambda: nc.scalar.copy(out=stg_t[:, :, tl * 64:(tl + 1) * 64],
                                             in_=pb_[:, 0:512].rearrange("p (a b) -> p a b", a=8)), [pb_r], [stg_r])
            for k in range(8):
                S.dma("act", self.ot[k * 128:(k + 1) * 128, g * 512:(g + 1) * 512], stg_t[:, k, :], [stg_r], [self.ot_r])
        P.close()

    def phase_out_ln_router(self, l, w_out_ap):
        nc, S = self.nc, self.S
        V = nc.vector
        P = Phase(S)
        W = P.sb([128, 8, D], BF16)
        w_r = []
        for k in range(8):
            r = Reg()
            S.dma("pool", W[:, k, :], w_out_ap[k * 128:(k + 1) * 128, :], [], [r])
            w_r.append(r)
        g_bc = P.sb([128, D], F32)
        b_bc = P.sb([128, D], F32)
        rw = P.sb([128, 8, 36], F32)
        rb = P.sb([128, 36], F32)
        u_bf = P.sb([128, 128], BF16)
        ones_bf = P.sb([128, 128], BF16)
        ecb = P.sb([128, 128], F32)
        carry = P.sb([128, 32], F32)
        pid = P.sb([128, 8], F32)
        S.dma("sp", pid[:], self.cd["pid"], [], [Reg()])
        for tl_, ap_ in ((g_bc, self.lng[l * 2]), (b_bc, self.lnb[l * 2]), (rw, self.rw[l].rearrange("(k p) n -> p k n", p=128)),
                         (rb, self.rb[l]), (u_bf, self.cd["u_bf"]), (ones_bf, self.cd["ones_bf"]), (ecb, self.cd["ecb"])):
            S.dma("sp", tl_[:], ap_, [], [Reg()])
        car_r = Reg()
        S.dve(lambda: V.memset(carry[:], 0.0), [], [car_r])
        S.barrier()
        gb_r = Reg()
        oT = Rot([(P.sb([128, 8, 512], BF16), [Reg() for _ in range(8)]) for _ in range(2)])
        xs = P.rot_sb(2, [128, D], F32)
        ys = P.rot_sb(2, [128, D], F32)
        xa = P.rot_sb(2, [128, D], F32)
        xbf = P.rot_sb(8, [128, D], BF16)
        ps_m = P.rot_ps(2, [128, 1024], F32)
        ps_t = P.rot_ps(1, [128, 1024], F32)
        ps_r = P.rot_ps(2, [128, 512], F32)
        xTf = P.rot_sb(2, [128, 8, 128], F32)
        st = P.sb([128, 12], F32)
        mv = P.sb([128, 2], F32)
        rs = P.sb([128, 1], F32)
        scr = (st, Reg(), mv, Reg(), rs, Reg())
        lgs = P.rot_sb(2, [128, 4, 36], F32)
        sm = P.sb([128, 16, 4], F32)
        oh = P.sb([128, 4, 4], F32)
        eg = P.sb([128, 4, 4], F32)
        lem = P.sb([128, 4, 32], F32)
        o1 = P.sb([128, 4, 32], F32)
        o2 = P.sb([128, 4, 32], F32)
        mbf = P.sb([128, 4, 32], BF16)
        rf = P.sb([128, 4, 32], F32)
        tmp = P.sb([128, 4, 32], F32)
        rk = P.sb([128, 4, 2], F32)
        bs = P.sb([128, 4, 2], F32)
        ov = P.sb([128, 4, 2], F32)
        ps_ = P.sb([128, 4, 2], F32)
        pd = P.sb([128, 4, 2], F32)
        possi = P.rot_sb(2, [128, 4, 2], I32)
        rr = Reg()
        src = self.xsrc(l)
        f3 = lambda t: t[:].rearrange("p a b -> p (a b)")
        for g in range(8):
            oT_t, o_regs = oT.next()
            for k in range(8):
                S.dma("sp", oT_t[:, k, :], self.ot[k * 128:(k + 1) * 128, g * 512:(g + 1) * 512], [self.ot_r], [o_regs[k]])
            lg, lg_r = lgs.next()
            xb_list = []
            for tl in range(4):
                t = g * 4 + tl
                x_t, x_r = xs.next()
                S.dma("sp", x_t[:], src[t * 128:(t + 1) * 128, :], [self.xres_r[2 * t], self.xres_r[2 * t + 1]], [x_r])
                pm, pm_r = ps_m.next()
                for hf in range(2):
                    for k in range(8):
                        S.pe(lambda: nc.tensor.matmul(pm[:, hf * 512:(hf + 1) * 512], lhsT=oT_t[:, k, tl * 128:(tl + 1) * 128],
                                                      rhs=W[:, k, hf * 512:(hf + 1) * 512], start=(k == 0), stop=(k == 7)),
                             [o_regs[k], w_r[k]], [pm_r])
                y_t, y_r = ys.next()
                for hf in range(2):
                    S.dve(lambda: V.scalar_tensor_tensor(out=y_t[:, hf * 512:(hf + 1) * 512],
                                                         in0=x_t[:, hf * 512:(hf + 1) * 512], scalar=ALPHA,
                                                         in1=pm[:, hf * 512:(hf + 1) * 512], op0=ALU.mult, op1=ALU.add),
                          [x_r, pm_r], [y_r])
                xa_t, xa_r = xa.next()
                self.layer_norm_tile(P, 128, y_t, y_r, g_bc, b_bc, gb_r, xa_t, xa_r, scr)
                S.dma("act", self.xres[t * 128:(t + 1) * 128, :], xa_t[:, :], [xa_r],
                      [self.xres_r[2 * t], self.xres_r[2 * t + 1]])
                xb_t, xb_r = xbf.next()
                S.act(lambda: nc.scalar.copy(out=xb_t[:, :], in_=xa_t[:, :]), [xa_r], [xb_r])
                xb_list.append((xb_t, xb_r))
                pt, pt_r = ps_t.next()
                for k in range(8):
                    S.pe(lambda: nc.tensor.transpose(pt[:, k * 128:(k + 1) * 128], xa_t[:, k * 128:(k + 1) * 128],
                                                     self.ident_f[:]), [xa_r], [pt_r])
                xf, xf_r = xTf.next()
                S.act(lambda: nc.scalar.copy(out=xf[:, 0:4, :], in_=pt[:, 0:512].rearrange("p (a b) -> p a b", a=4)),
                      [pt_r], [xf_r])
                S.dve(lambda: V.tensor_copy(out=xf[:, 4:8, :], in_=pt[:, 512:1024].rearrange("p (a b) -> p a b", a=4)),
                      [pt_r], [xf_r])
                pr, pr_r = ps_r.next()
                for k in range(8):
                    S.pe(lambda: nc.tensor.matmul(pr[:, 0:36], lhsT=xf[:, k, :], rhs=rw[:, k, :], start=(k == 0), stop=(k == 7)),
                         [xf_r], [pr_r])
                S.dve(lambda: V.tensor_tensor(out=lg[:, tl, :], in0=pr[:, 0:36], in1=rb[:, :], op=ALU.add), [pr_r], [lg_r])
            R_ = [lg_r, rr]
            LG = lg[:, :, 0:4]
            LE = lg[:, :, 4:36]
            bc4 = lambda j: sm[:, j, :].unsqueeze(2).to_broadcast([128, 4, 4])
            bc32 = lambda j: sm[:, j, :].unsqueeze(2).to_broadcast([128, 4, 32])
            S.dve(lambda: V.tensor_reduce(out=sm[:, 0, :], in_=LG, axis=AX.X, op=ALU.max), R_, [rr])
            S.dve(lambda: V.tensor_tensor(out=oh[:, :, :], in0=LG, in1=bc4(0), op=ALU.is_ge), R_, [rr])
            S.dve(lambda: V.tensor_tensor(out=eg[:, :, :], in0=LG, in1=bc4(0), op=ALU.subtract), R_, [rr])
            S.act(lambda: nc.scalar.activation(out=f3(eg), in_=f3(eg), func=AF.Exp), [rr], [rr])
            S.dve(lambda: V.tensor_reduce(out=sm[:, 1, :], in_=eg[:, :, :], axis=AX.X, op=ALU.add), [rr], [rr])
            S.dve(lambda: V.reciprocal(out=sm[:, 2, :], in_=sm[:, 1, :]), [rr], [rr])
            S.dve(lambda: V.tensor_scalar(out=f3(oh), in0=f3(oh), scalar1=-1.0, scalar2=BIG, op0=ALU.add, op1=ALU.mult),
                  [rr], [rr])
            S.dve(lambda: V.tensor_tensor(out=lem[:].rearrange("p a (g e) -> p (a g) e", g=4),
                                          in0=LE.rearrange("p a (g e) -> p a g e", g=4),
                                          in1=oh[:, :, :].unsqueeze(3).to_broadcast([128, 4, 4, 8]), op=ALU.add)
                  if False else
                  V.tensor_tensor(out=lem[:, :, :].rearrange("p a (g e) -> p a g e", g=4),
                                  in0=LE.rearrange("p a (g e) -> p a g e", g=4),
                                  in1=oh[:, :, :].unsqueeze(3).to_broadcast([128, 4, 4, 8]), op=ALU.add), R_, [rr])
            S.dve(lambda: V.tensor_reduce(out=sm[:, 3, :], in_=lem[:, :, :], axis=AX.X, op=ALU.max), [rr], [rr])
            S.dve(lambda: V.tensor_tensor(out=o1[:, :, :], in0=lem[:, :, :], in1=bc32(3), op=ALU.is_ge), [rr], [rr])
            S.dve(lambda: V.scalar_tensor_tensor(out=f3(lem), in0=f3(o1), scalar=-BIG, in1=f3(lem), op0=ALU.mult, op1=ALU.add),
                  [rr], [rr])
            S.dve(lambda: V.tensor_reduce(out=sm[:, 4, :], in_=lem[:, :, :], axis=AX.X, op=ALU.max), [rr], [rr])
            S.dve(lambda: V.tensor_tensor(out=o2[:, :, :], in0=lem[:, :, :], in1=bc32(4), op=ALU.is_ge), [rr], [rr])
            S.dve(lambda: V.tensor_tensor(out=sm[:, 5, :], in0=sm[:, 4, :], in1=sm[:, 3, :], op=ALU.subtract), [rr], [rr])
            S.act(lambda: nc.scalar.activation(out=sm[:, 6, :], in_=sm[:, 5, :], func=AF.Exp), [rr], [rr])
            S.dve(lambda: V.tensor_scalar(out=sm[:, 7, :], in0=sm[:, 6, :], scalar1=1.0, scalar2=None, op0=ALU.add), [rr], [rr])
            S.dve(lambda: V.reciprocal(out=sm[:, 8, :], in_=sm[:, 7, :]), [rr], [rr])
            pgr = self.pg_r[g]
            S.dve(lambda: V.tensor_tensor(out=self.g12[:, g * 4:(g + 1) * 4, 0], in0=sm[:, 8, :], in1=sm[:, 2, :], op=ALU.mult),
                  [rr], [pgr])
            S.dve(lambda: V.tensor_tensor(out=self.g12[:, g * 4:(g + 1) * 4, 1], in0=self.g12[:, g * 4:(g + 1) * 4, 0],
                                          in1=sm[:, 6, :], op=ALU.mult), [rr, pgr], [pgr])
            S.dve(lambda: V.tensor_tensor(out=f3(mbf), in0=f3(o1), in1=f3(o2), op=ALU.add), [rr], [rr])
            pp, pp_r = ps_r.next()
            S.pe(lambda: nc.tensor.matmul(pp[:, 0:128], lhsT=u_bf[:, :], rhs=f3(mbf), start=True, stop=True), [rr], [pp_r])
            S.pe(lambda: nc.tensor.matmul(pp[:, 128:256], lhsT=ones_bf[:, :], rhs=f3(mbf), start=True, stop=True), [rr], [pp_r])
            for tl in range(4):
                S.dve(lambda: V.tensor_tensor(out=rf[:, tl, :], in0=pp[:, tl * 32:(tl + 1) * 32], in1=carry[:, :], op=ALU.add),
                      [pp_r, car_r], [rr])
                S.dve(lambda: V.tensor_tensor(out=carry[:, :], in0=carry[:, :], in1=pp[:, 128 + tl * 32:128 + (tl + 1) * 32],
                                              op=ALU.add), [pp_r, car_r], [car_r])
            for kk, ok in enumerate((o1, o2)):
                S.dve(lambda: V.tensor_tensor(out=f3(tmp), in0=f3(ok), in1=f3(rf), op=ALU.mult), [rr], [rr])
                S.dve(lambda: V.tensor_reduce(out=rk[:, :, kk], in_=tmp[:, :, :], axis=AX.X, op=ALU.add), [rr], [rr])
                S.dve(lambda: V.tensor_tensor(out=f3(tmp), in0=f3(ok), in1=ecb[:, :], op=ALU.mult), [rr], [rr])
                S.dve(lambda: V.tensor_reduce(out=bs[:, :, kk], in_=tmp[:, :, :], axis=AX.X, op=ALU.add), [rr], [rr])
            S.dve(lambda: V.tensor_scalar(out=f3(ov), in0=f3(rk), scalar1=float(CAP), scalar2=None, op0=ALU.is_ge), [rr], [rr])
            S.dve(lambda: V.tensor_tensor(out=f3(ps_), in0=f3(rk), in1=f3(bs), op=ALU.add), [rr], [rr])
            S.dve(lambda: V.tensor_tensor(out=f3(pd), in0=pid[:, :], in1=f3(ps_), op=ALU.subtract), [rr], [rr])
            S.dve(lambda: V.tensor_tensor(out=f3(pd), in0=f3(pd), in1=f3(ov), op=ALU.mult), [rr], [rr])
            S.dve(lambda: V.tensor_tensor(out=f3(ps_), in0=f3(ps_), in1=f3(pd), op=ALU.add), [rr], [rr])
            S.dve(lambda: V.tensor_copy(out=self.posg[:, g * 4:(g + 1) * 4, :].rearrange("p a b -> p (a b)"), in_=f3(ps_)),
                  [rr], [pgr])
            for tl in range(4):
                xb_t, xb_r = xb_list[tl]
                for kk in range(2):
                    S.idma(self.xs[:, :], bass.IndirectOffsetOnAxis(ap=self.posg[:, g * 4 + tl, kk:kk + 1], axis=0), xb_t[:, :],
                           None, None, [xb_r, pgr], [self.xs_r])
        P.close()

    def phase_moe(self, l, last):
        nc, S = self.nc, self.S
        V = nc.vector
        P = Phase(S)
        g_bc = P.sb([128, D], F32)
        b_bc = P.sb([128, D], F32)
        S.dma("sp", g_bc[:], self.lng[l * 2 + 1], [], [Reg()])
        S.dma("sp", b_bc[:], self.lnb[l * 2 + 1], [], [Reg()])
        S.barrier()
        gb_r = Reg()
        W1 = P.rot_sb(2, [128, 8, 512], BF16)
        W3 = P.rot_sb(2, [128, 8, 512], BF16)
        W2 = P.rot_sb(2, [128, 4, D], BF16)
        xsl = P.rot_sb(2, [128, 4, D], BF16)
        xgT = P.rot_sb(2, [128, 8, 512], BF16)
        ps_tr = P.rot_ps(2, [128, 1024], BF16)
        ps_h1 = P.rot_ps(2, [128, 512], F32)
        ps_h3 = P.rot_ps(1, [128, 512], F32)
        ps_y = P.rot_ps(3, [128, 512], F32)
        sl = P.rot_sb(2, [128, 512], F32)
        hT = P.rot_sb(2, [128, 4, 512], BF16)
        ysb = P.rot_sb(3, [128, D], F32)
        NB = CAP // 128
        for e in range(32):
            w1_t, w1_r = W1.next()
            w3_t, w3_r = W3.next()
            w2_t, w2_r = W2.next()
            S.dma("pool", w1_t[:], self.w1[l, e].rearrange("(k p) f -> p k f", p=128), [], [w1_r])
            S.dma("pool", w3_t[:], self.w3[l, e].rearrange("(k p) f -> p k f", p=128), [], [w3_r])
            S.dma("pool", w2_t[:], self.w2[l, e].rearrange("(k p) f -> p k f", p=128), [], [w2_r])
            xs_t, xs_r = xsl.next()
            S.dma("sp", xs_t[:, 0:NB, :], self.xs[e * CAP:(e + 1) * CAP, :].rearrange("(b p) d -> p b d", p=128),
                  [self.xs_r], [xs_r])
            xg, xg_r = xgT.next()
            for k2 in range(4):
                ptr, ptr_r = ps_tr.next()
                for kk in range(2):
                    k = k2 * 2 + kk
                    for bb in range(NB):
                        S.pe(lambda: nc.tensor.transpose(ptr[:, kk * 512 + bb * 128:kk * 512 + (bb + 1) * 128],
                                                         xs_t[:, bb, k * 128:(k + 1) * 128], self.ident_bf[:]), [xs_r], [ptr_r])
                if k2 % 2 == 0:
                    S.act(lambda: nc.scalar.copy(out=xg[:, k2 * 2:k2 * 2 + 2, :].rearrange("p a b -> p (a b)"), in_=ptr[:, :]),
                          [ptr_r], [xg_r])
                else:
                    S.dve(lambda: V.tensor_copy(out=xg[:, k2 * 2:k2 * 2 + 2, :].rearrange("p a b -> p (a b)"), in_=ptr[:, :]),
                          [ptr_r], [xg_r])
            h_t, h_r = hT.next()
            for fc in range(4):
                p1, p1_r = ps_h1.next()
                p3, p3_r = ps_h3.next()
                for k in range(8):
                    S.pe(lambda: nc.tensor.matmul(p1[:, :], lhsT=w1_t[:, k, fc * 128:(fc + 1) * 128], rhs=xg[:, k, :],
                                                  start=(k == 0), stop=(k == 7)), [w1_r, xg_r], [p1_r])
                for k in range(8):
                    S.pe(lambda: nc.tensor.matmul(p3[:, :], lhsT=w3_t[:, k, fc * 128:(fc + 1) * 128], rhs=xg[:, k, :],
                                                  start=(k == 0), stop=(k == 7)), [w3_r, xg_r], [p3_r])
                s_t, s_r = sl.next()
                S.act(lambda: nc.scalar.activation(out=s_t[:, :], in_=p1[:, :], func=AF.Silu), [p1_r], [s_r])
                S.dve(lambda: V.tensor_tensor(out=h_t[:, fc, :], in0=p3[:, :], in1=s_t[:, :], op=ALU.mult), [p3_r, s_r], [h_r])
            for st_ in range(NB):
                y_t, y_r = ysb.next()
                for dh in range(2):
                    py, py_r = ps_y.next()
                    for fc in range(4):
                        S.pe(lambda: nc.tensor.matmul(py[:, :], lhsT=h_t[:, fc, st_ * 128:(st_ + 1) * 128],
                                                      rhs=w2_t[:, fc, dh * 512:(dh + 1) * 512], start=(fc == 0), stop=(fc == 3)),
                             [h_r, w2_r], [py_r])
                    if dh == 0:
                        S.act(lambda: nc.scalar.copy(out=y_t[:, 0:512], in_=py[:, :]), [py_r], [y_r])
                    else:
                        S.dve(lambda: V.tensor_copy(out=y_t[:, 512:1024], in_=py[:, :]), [py_r], [y_r])
                S.dma("act", self.yb[e * CAP + st_ * 128:e * CAP + (st_ + 1) * 128, :], y_t[:, :], [y_r], [self.yb_r])
        S.barrier()
        xs2 = P.rot_sb(2, [128, D], F32)
        ya = P.rot_sb(2, [128, D], F32)
        ybb = P.rot_sb(2, [128, D], F32)
        ys = P.rot_sb(2, [128, D], F32)
        xo = P.rot_sb(2, [128, D], F32)
        st = P.sb([128, 12], F32)
        mv = P.sb([128, 2], F32)
        rs = P.sb([128, 1], F32)
        scr = (st, Reg(), mv, Reg(), rs, Reg())
        dst = self.out if last else self.xres
        for t in range(32):
            pgr = self.pg_r[t // 4]
            x_t, x_r = xs2.next()
            S.dma("sp", x_t[:], self.xres[t * 128:(t + 1) * 128, :], [self.xres_r[2 * t], self.xres_r[2 * t + 1]], [x_r])
            a_t, a_r = ya.next()
            b_t, b_r = ybb.next()
            S.idma(a_t[:, :], None, self.yb[:, :], bass.IndirectOffsetOnAxis(ap=self.posg[:, t, 0:1], axis=0), NS + 127,
                   [self.yb_r, pgr], [a_r])
            S.idma(b_t[:, :], None, self.yb[:, :], bass.IndirectOffsetOnAxis(ap=self.posg[:, t, 1:2], axis=0), NS + 127,
                   [self.yb_r, pgr], [b_r])
            y_t, y_r = ys.next()
            S.act(lambda: nc.scalar.activation(out=y_t[:, :], in_=x_t[:, :], func=AF.Copy, scale=ALPHA), [x_r], [y_r])
            S.dve(lambda: V.scalar_tensor_tensor(out=y_t[:, :], in0=a_t[:, :], scalar=self.g12[:, t, 0:1], in1=y_t[:, :],
                                                 op0=ALU.mult, op1=ALU.add), [a_r, pgr, y_r], [y_r])
            S.dve(lambda: V.scalar_tensor_tensor(out=y_t[:, :], in0=b_t[:, :], scalar=self.g12[:, t, 1:2], in1=y_t[:, :],
                                                 op0=ALU.mult, op1=ALU.add), [b_r, pgr, y_r], [y_r])
            xo_t, xo_r = xo.next()
            self.layer_norm_tile(P, 128, y_t, y_r, g_bc, b_bc, gb_r, xo_t, xo_r, scr)
            S.dma("act", dst[t * 128:(t + 1) * 128, :], xo_t[:, :], [xo_r], [self.xres_r[2 * t], self.xres_r[2 * t + 1]])
        P.close()

    def dump(self, src_ap, shape, dt):
        nc, S = self.nc, self.S
        dbg = nc.dram_tensor("dbg", list(shape), dt, kind="ExternalOutput").ap()
        S.barrier()
        n = shape[0] // 128
        for i in range(n):
            S.dma("sp", dbg[i * 128:(i + 1) * 128, :], src_ap[i * 128:(i + 1) * 128, :], [], [Reg()])
        S.barrier()

    def init_scratch(self):
        nc, S = self.nc, self.S
        P = Phase(S)
        zb = P.sb([128, 8, D], BF16)
        zf = P.sb([128, D], F32)
        r = Reg()
        S.dve(lambda: nc.vector.memset(zb[:].rearrange("p a b -> p (a b)"), 0.0), [], [r])
        S.dve(lambda: nc.vector.memset(zf[:], 0.0), [], [r])
        for i in range(NS // 1024):
            S.dma("sp", self.xs[i * 1024:(i + 1) * 1024, :].rearrange("(p a) d -> p a d", p=128), zb[:, :, :], [r], [Reg()])
        S.dma("sp", self.xs[NS:NS + 128, :], zb[:, 0, :], [r], [Reg()])
        S.dma("sp", self.yb[NS:NS + 128, :], zf[:, :], [r], [Reg()])
        P.close()

    def build(self):
        dbg = self.debug
        self.init_scratch()
        for l in range(self.nlayers):
            if l % 2 == 0:
                self.phase_attn_proj(l)
                if dbg == "qkt%d" % l:
                    self.dump(self.qkt.rearrange("a b c -> (a b) c"), [2048, T], BF16)
                    return self.nc
                self.phase_attn(l)
                if dbg == "ot%d" % l:
                    self.dump(self.ot, [D, T], BF16)
                    return self.nc
                self.phase_out_ln_router(l, self.ab_w_out[l // 2])
            else:
                self.phase_hgrn(l)
                if dbg == "ot%d" % l:
                    self.dump(self.ot, [D, T], BF16)
                    return self.nc
                self.phase_out_ln_router(l, self.c_w_out[l // 2])
            if dbg == "xa%d" % l:
                self.dump(self.xres, [T, D], F32)
                return self.nc
            self.phase_moe(l, last=(l == self.nlayers - 1))
        self.S.barrier()
        return self.nc


def host_inputs(inputs):
    f = lambda a: np.ascontiguousarray(np.asarray(a, dtype=np.float32))
    d = {}
    d["ab_w_in"] = f(inputs["ab_w_in"])
    d["ab_w_out"] = f(inputs["ab_w_out"])
    d["c_w_in"] = f(inputs["c_w_in"])
    d["c_w_out"] = f(inputs["c_w_out"])
    d["exp_w1"] = f(inputs["exp_w1"])
    d["exp_w3"] = f(inputs["exp_w3"])
    d["exp_w2"] = f(inputs["exp_w2"])
    cng = np.tile(f(inputs["c_norm_g"]), (1, 8))
    d["cng"] = np.ascontiguousarray(np.broadcast_to(cng[:, None, :], (2, 64, D)))
    d["lbl"] = np.ascontiguousarray(np.broadcast_to(f(inputs["hgrn_lb_logits"])[:, None, :], (2, 64, D)))
    d["lng"] = np.ascontiguousarray(np.broadcast_to(f(inputs["ln_g"]).reshape(8, 1, D), (8, 128, D)))
    d["lnb"] = np.ascontiguousarray(np.broadcast_to(f(inputs["ln_b"]).reshape(8, 1, D), (8, 128, D)))
    d["rw"] = np.ascontiguousarray(np.concatenate([f(inputs["router_g_w"]), f(inputs["router_e_w"])], axis=2))
    rb = np.concatenate([f(inputs["router_g_b"]), f(inputs["router_e_b"])], axis=1)
    d["rb"] = np.ascontiguousarray(np.broadcast_to(rb[:, None, :], (4, 128, 36)))
    return d


_CACHE = {}


def kernel(**inputs):
    x = np.ascontiguousarray(np.asarray(inputs["x"], dtype=np.float32))
    if "prog" not in _CACHE:
        p = Prog()
        p.build()
        _CACHE["prog"] = p
    p = _CACHE["prog"]
    shared = host_inputs(inputs)
    for k, v in p.consts_np.items():
        shared["c_" + k] = v
    in_maps = []
    for c in range(8):
        m = dict(shared)
        m["x"] = x[c]
        in_maps.append(m)
    res = run_bass_kernel_spmd(p.nc, in_maps, core_ids=list(range(8)))
    return np.stack([np.asarray(r["out"], dtype=np.float32) for r in res.results], axis=0)
```

```python
import contextlib
import os
LVL = int(os.environ.get('DBG_LVL', '9'))
import numpy as np
import ml_dtypes
import concourse.bass as bass
import concourse.mybir as mybir
from concourse.bass_utils import run_bass_kernel_spmd

F32 = mybir.dt.float32
BF16 = mybir.dt.bfloat16
AF = mybir.ActivationFunctionType
ALU = mybir.AluOpType
AX = mybir.AxisListType

T = 4096
D = 1024
DEPTH = 4
ALPHA = float((2.0 * DEPTH) ** 0.25)
LN_EPS = 1e-5
RMS_EPS = 1e-6
BIG = 30000.0
EPOCH = 30000
NSLOT = 8
CAP = 512
NS = 32 * CAP
I32 = mybir.dt.int32


class Reg:
    __slots__ = ("w", "r", "x")

    def __init__(self, x=False):
        self.w = {}
        self.r = {}
        self.x = x


class Eng:
    def __init__(self, name, e, is_pe=False):
        self.name = name
        self.e = e
        self.count = 0
        self.known = {}
        self.is_pe = is_pe
        self.dcount = 0


class Sched:
    def __init__(self, nc):
        self.nc = nc
        self.E = {
            "pe": Eng("pe", nc.tensor, True),
            "act": Eng("act", nc.scalar),
            "dve": Eng("dve", nc.vector),
            "pool": Eng("pool", nc.gpsimd),
            "sp": Eng("sp", nc.sync),
        }
        self.semh = {}
        self.nsem = 0

    def _sem(self, key):
        h = self.semh.get(key)
        if h is None:
            h = self.nc.alloc_semaphore("s_%s_%s_%d" % key)
            self.semh[key] = h
            self.nsem += 1
        return h

    def _waits(self, E, reads, writes):
        deps = {}
        for r in reads:
            for k, v in r.w.items():
                if deps.get(k, 0) < v:
                    deps[k] = v
            if r.x:
                for k, v in r.r.items():
                    if deps.get(k, 0) < v:
                        deps[k] = v
        for w in writes:
            for k, v in w.w.items():
                if deps.get(k, 0) < v:
                    deps[k] = v
            for k, v in w.r.items():
                if deps.get(k, 0) < v:
                    deps[k] = v
        for k, v in deps.items():
            if E.is_pe and k[0] == "pe" and k[1] == "c":
                continue
            if E.known.get(k, 0) < v:
                E.e.wait_ge(self._sem(k), v)
                E.known[k] = v

    def op(self, en, fn, reads, writes):
        E = self.E[en]
        self._waits(E, reads, writes)
        ins = fn()
        key = (en, "c", E.count // EPOCH)
        val = E.count % EPOCH + 1
        ins.then_inc(self._sem(key), 1)
        E.count += 1
        for r in reads:
            if r.x:
                r.w = {key: val}
                r.r = {}
            else:
                r.r[key] = val
        for w in writes:
            w.w = {key: val}
            w.r = {}

    def pe(self, fn, reads, writes):
        self.op("pe", fn, reads, writes)

    def act(self, fn, reads, writes):
        self.op("act", fn, reads, writes)

    def dve(self, fn, reads, writes):
        self.op("dve", fn, reads, writes)

    def pool(self, fn, reads, writes):
        self.op("pool", fn, reads, writes)

    def dma(self, qn, out, in_, reads, writes):
        Q = self.E[qn]
        self._waits(Q, reads, writes)
        i = Q.dcount
        slot = i % NSLOT
        tgt = 16 * (i // NSLOT + 1)
        assert tgt < 60000
        key = (qn, "d", slot)
        if i >= NSLOT and Q.known.get(key, 0) < tgt - 16:
            Q.e.wait_ge(self._sem(key), tgt - 16)
            Q.known[key] = tgt - 16
        Q.e.dma_start(out=out, in_=in_).then_inc(self._sem(key), 16)
        Q.dcount += 1
        for r in reads:
            if r.r.get(key, 0) < tgt:
                r.r[key] = tgt
        for w in writes:
            w.w = {key: tgt}
            w.r = {}

    def idma(self, out, out_off, in_, in_off, bound, reads, writes):
        Q = self.E["pool"]
        self._waits(Q, reads, writes)
        i = Q.dcount
        slot = i % NSLOT
        tgt = 16 * (i // NSLOT + 1)
        assert tgt < 60000
        key = ("pool", "d", slot)
        if i >= NSLOT and Q.known.get(key, 0) < tgt - 16:
            Q.e.wait_ge(self._sem(key), tgt - 16)
            Q.known[key] = tgt - 16
        Q.e.indirect_dma_start(out=out, out_offset=out_off, in_=in_, in_offset=in_off).then_inc(self._sem(key), 16)
        Q.dcount += 1
        for r in reads:
            if r.r.get(key, 0) < tgt:
                r.r[key] = tgt
        for w in writes:
            w.w = {key: tgt}
            w.r = {}

    def barrier(self):
        latest = {}
        for E in self.E.values():
            if E.count > 0:
                latest[(E.name, "c", (E.count - 1) // EPOCH)] = (E.count - 1) % EPOCH + 1
            for slot in range(min(NSLOT, E.dcount)):
                n = (E.dcount - slot + NSLOT - 1) // NSLOT
                latest[(E.name, "d", slot)] = 16 * n
        for E in self.E.values():
            for k, v in latest.items():
                if E.is_pe and k[0] == "pe" and k[1] == "c":
                    continue
                if E.known.get(k, 0) < v:
                    E.e.wait_ge(self._sem(k), v)
                    E.known[k] = v


class Phase:
    cnt = 0

    def __init__(self, S):
        self.S = S
        self.nc = S.nc
        self.stack = contextlib.ExitStack()

    def sb(self, shape, dt):
        Phase.cnt += 1
        return self.stack.enter_context(self.nc.sbuf_tensor("sb%d" % Phase.cnt, list(shape), dt))

    def ps(self, shape, dt=F32):
        Phase.cnt += 1
        return self.stack.enter_context(self.nc.psum_tensor("ps%d" % Phase.cnt, list(shape), dt))

    def rot_sb(self, n, shape, dt):
        return Rot([(self.sb(shape, dt), Reg()) for _ in range(n)])

    def rot_ps(self, n, shape, dt=F32):
        return Rot([(self.ps(shape, dt), Reg(True)) for _ in range(n)])

    def close(self):
        self.S.barrier()
        self.stack.close()


class Rot:
    def __init__(self, items):
        self.items = items
        self.i = 0

    def next(self):
        it = self.items[self.i % len(self.items)]
        self.i += 1
        return it


def make_consts():
    bf = ml_dtypes.bfloat16
    c = {}
    c["ident_bf"] = np.eye(128, dtype=np.float32).astype(bf)
    c["ident_f"] = np.eye(128, dtype=np.float32)
    half = 8
    inv = (np.float32(500000.0) ** (-np.arange(half, dtype=np.float32) / np.float32(half))).astype(np.float32)
    ang = (np.arange(T, dtype=np.float32)[:, None] * inv[None, :]).astype(np.float32)
    cos = np.cos(ang.astype(np.float64)).astype(np.float32)
    sin = np.sin(ang.astype(np.float64)).astype(np.float32)
    c2 = np.concatenate([cos, cos], axis=1)
    s2 = np.concatenate([-sin, sin], axis=1)
    c2e = np.tile(c2, (1, 8))
    s2e = np.tile(s2, (1, 8))
    c["c2e"] = np.ascontiguousarray(c2e.reshape(32, 128, 128).transpose(1, 0, 2).reshape(128, 4096))
    c["s2e"] = np.ascontiguousarray(s2e.reshape(32, 128, 128).transpose(1, 0, 2).reshape(128, 4096))

    def mult(d):
        m = np.zeros_like(d, dtype=np.float32)
        m += ((d >= 0) & (d <= 128))
        m += ((d >= 0) & (d % 4 == 0) & (d <= 512))
        m += ((d >= 0) & (d % 16 == 0) & (d <= 2048))
        return m

    kl = np.arange(128)[:, None]
    ql = np.arange(512)[None, :]
    dm = np.zeros((128, 20, 512), np.float32)
    for di in range(20):
        delta = -384 + 128 * di
        dm[:, di, :] = mult(delta + ql - kl)
    with np.errstate(divide="ignore"):
        ldm = np.where(dm > 0, 8.0 * np.log(np.maximum(dm, 1e-30).astype(np.float64)), -BIG)
    ldm_hi = ldm.astype(np.float32).astype(bf)
    ldm_lo = (ldm - ldm_hi.astype(np.float64)).astype(np.float32)
    ldm_lo = np.where(dm > 0, ldm_lo, 0.0).astype(bf)
    c["ldm_hi"] = ldm_hi.reshape(128, 20 * 512)
    c["dmm"] = dm[:, 0:8, :].reshape(128, 8 * 512).astype(bf)
    c["dm_lo_need"] = np.array([float(np.any(ldm_lo[:, di, :].astype(np.float32) != 0)) for di in range(20)], np.float32)
    cm = np.zeros((128, 4, 512), np.float32)
    for ci in range(4):
        delta = -384 + 128 * ci
        cm[:, ci, :] = (delta + ql - kl >= 0)
    c["lcm"] = ((cm - 1.0) * BIG).reshape(128, 4 * 512).astype(bf)
    koh = np.zeros((16, T), np.float32)
    for b in range(16):
        koh[b, b * 256:(b + 1) * 256] = 1.0
    c["koh"] = koh.astype(bf)
    tt = np.arange(32)[:, None]
    bb = np.arange(16)[None, :]
    own = tt // 2
    valid = (bb < own).astype(np.float32)
    negv = (valid - 1.0) * BIG
    ownm1 = (bb == own).astype(np.float32) - 1.0
    c["valid"] = np.ascontiguousarray(np.broadcast_to(valid.reshape(1, 512), (128, 512))).astype(np.float32)
    c["negv"] = np.ascontiguousarray(np.broadcast_to(negv.reshape(1, 512), (128, 512))).astype(np.float32)
    c["ownm1"] = np.ascontiguousarray(np.broadcast_to(ownm1.reshape(1, 512), (128, 512))).astype(np.float32)
    j = np.arange(64)[:, None]
    i = np.arange(64)[None, :]
    c["mg"] = ((j <= i).astype(np.float32) - (j <= 31).astype(np.float32)).astype(np.float32)
    ind = np.zeros((64, 3), np.float32)
    ind[:, 0] = (np.arange(64) <= 31)
    ind[:, 1] = 1.0
    ind[:, 2] = (np.arange(64) > 31)
    c["ind"] = ind
    c["cmask8"] = np.ascontiguousarray(np.tile((i >= j).astype(np.float32), (1, 8)))
    c["ones_f"] = np.ones((128, 64), np.float32)
    tp = np.arange(128)[:, None]
    tq = np.arange(128)[None, :]
    c["u_bf"] = (tp < tq).astype(np.float32).astype(bf)
    c["ones_bf"] = np.ones((128, 128), np.float32).astype(bf)
    ecb = np.tile((np.arange(32, dtype=np.float32) * CAP)[None, :], (1, 4))
    c["ecb"] = np.ascontiguousarray(np.broadcast_to(ecb, (128, 128))).astype(np.float32)
    c["pid"] = np.ascontiguousarray(np.broadcast_to((NS + np.arange(128, dtype=np.float32))[:, None], (128, 8)))
    return c


CONST_DT = {"ident_bf": BF16, "ldm_hi": BF16, "dmm": BF16, "lcm": BF16, "koh": BF16, "u_bf": BF16, "ones_bf": BF16}


class Prog:
    def __init__(self, nlayers=DEPTH, debug=None):
        self.nc = nc = bass.Bass("TRN2", target_bir_lowering=False)
        self.S = Sched(nc)
        self.nlayers = nlayers
        self.debug = debug
        self.consts_np = make_consts()
        di = lambda name, shape, dt=F32: nc.dram_tensor(name, list(shape), dt, kind="ExternalInput").ap()
        self.x_in = di("x", [T, D])
        self.ab_w_in = di("ab_w_in", [2, D, 3072])
        self.ab_w_out = di("ab_w_out", [2, D, D])
        self.c_w_in = di("c_w_in", [2, D, 4096])
        self.c_w_out = di("c_w_out", [2, D, D])
        self.cng = di("cng", [2, 64, D])
        self.lbl = di("lbl", [2, 64, D])
        self.lng = di("lng", [8, 128, D])
        self.lnb = di("lnb", [8, 128, D])
        self.rw = di("rw", [4, D, 36])
        self.rb = di("rb", [4, 128, 36])
        self.w1 = di("exp_w1", [4, 32, D, 512])
        self.w3 = di("exp_w3", [4, 32, D, 512])
        self.w2 = di("exp_w2", [4, 32, 512, D])
        self.cd = {}
        self.dm_lo_need = [bool(v) for v in self.consts_np.pop("dm_lo_need")]
        for k, v in self.consts_np.items():
            self.cd[k] = di("c_" + k, v.shape, CONST_DT.get(k, F32))
        self.out = nc.dram_tensor("out", [T, D], F32, kind="ExternalOutput").ap()
        dt_ = lambda name, shape, dt: nc.dram_tensor(name, list(shape), dt).ap()
        self.xres = dt_("xres", [T, D], F32)
        self.xres_r = [Reg() for _ in range(64)]
        self.qkt = dt_("qkt", [4, 512, T], BF16)
        self.qkt_r = Reg()
        self.vd = dt_("vd", [T, D], BF16)
        self.vd_r = Reg()
        self.ot = dt_("ot", [D, T], BF16)
        self.ot_r = Reg()
        self.xt = dt_("xt", [D, T], BF16)
        self.xt_r = Reg()
        self.ident_bf = nc.alloc_sbuf_tensor("ident_bf", [128, 128], BF16)
        self.ident_f = nc.alloc_sbuf_tensor("ident_f", [128, 128], F32)
        self.posg = nc.alloc_sbuf_tensor("posg", [128, 32, 2], I32)
        self.g12 = nc.alloc_sbuf_tensor("g12", [128, 32, 2], F32)
        self.pg_r = [Reg() for _ in range(8)]
        self.xs = dt_("xs", [NS + 128, D], BF16)
        self.xs_r = Reg()
        self.yb = dt_("yb", [NS + 128, D], F32)
        self.yb_r = Reg()
        self.cr = Reg()
        S = self.S
        S.dma("sp", self.ident_bf[:], self.cd["ident_bf"], [], [self.cr])
        r2 = Reg()
        S.dma("sp", self.ident_f[:], self.cd["ident_f"], [], [r2])
        self.cr2 = r2

    def xsrc(self, l):
        return self.x_in if l == 0 else self.xres

    def layer_norm_tile(self, P, np_, y, y_r, g_bc, b_bc, gb_r, out_t, out_r, scr):
        nc, S = self.nc, self.S
        st, st_r, mv, mv_r, rs, rs_r = scr
        S.dve(lambda: nc.vector.bn_stats(out=st[:np_, 0:6], in_=y[:np_, 0:512]), [y_r], [st_r])
        S.dve(lambda: nc.vector.bn_stats(out=st[:np_, 6:12], in_=y[:np_, 512:1024]), [y_r], [st_r])
        S.dve(lambda: nc.vector.bn_aggr(out=mv[:np_, :], in_=st[:np_, :]), [st_r], [mv_r])
        S.dve(lambda: nc.vector.tensor_scalar(out=rs[:np_, :], in0=mv[:np_, 1:2], scalar1=LN_EPS, scalar2=None,
                                              op0=ALU.add), [mv_r], [rs_r])
        S.act(lambda: nc.scalar.activation(out=rs[:np_, :], in_=rs[:np_, :], func=AF.Ln), [rs_r], [rs_r])
        S.act(lambda: nc.scalar.activation(out=rs[:np_, :], in_=rs[:np_, :], func=AF.Exp, scale=-0.5), [rs_r], [rs_r])
        S.dve(lambda: nc.vector.tensor_scalar(out=out_t[:np_, :], in0=y[:np_, :], scalar1=mv[:np_, 0:1],
                                              scalar2=rs[:np_, 0:1], op0=ALU.subtract, op1=ALU.mult),
              [y_r, mv_r, rs_r], [out_r])
        S.pool(lambda: nc.gpsimd.tensor_tensor(out=out_t[:np_, :], in0=out_t[:np_, :], in1=g_bc[:np_, :], op=ALU.mult),
               [out_r, gb_r], [out_r])
        S.pool(lambda: nc.gpsimd.tensor_tensor(out=out_t[:np_, :], in0=out_t[:np_, :], in1=b_bc[:np_, :], op=ALU.add),
               [out_r, gb_r], [out_r])

    def phase_attn_proj(self, l):
        nc, S = self.nc, self.S
        j = l // 2
        P = Phase(S)
        W = P.sb([128, 8, 3072], BF16)
        w_r = []
        for k in range(8):
            r = Reg()
            S.dma("pool", W[:, k, :], self.ab_w_in[j, k * 128:(k + 1) * 128, :], [], [r])
            w_r.append(r)
        c2e = P.sb([128, 32, 128], F32)
        s2e = P.sb([128, 32, 128], F32)
        tab_r = Reg()
        tab_r2 = Reg()
        S.dma("sp", c2e[:].rearrange("p a b -> p (a b)"), self.cd["c2e"], [], [tab_r])
        S.dma("sp", s2e[:].rearrange("p a b -> p (a b)"), self.cd["s2e"], [], [tab_r2])
        xs = P.rot_sb(2, [128, D], F32)
        xT = P.rot_sb(2, [128, 8, 128], BF16)
        ps_t = P.rot_ps(2, [128, 512], F32)
        ps_o = P.rot_ps(3, [128, 512], F32)
        ps_tr = P.rot_ps(2, [128, 1024], BF16)
        qs = P.rot_sb(3, [128, 512], BF16)
        t1 = P.rot_sb(2, [128, 8, 16], F32)
        t2 = P.rot_sb(2, [128, 8, 16], F32)
        stg = P.rot_sb(2, [128, 16, 512], BF16)
        vst = P.rot_sb(2, [128, 1024], BF16)
        src = self.xsrc(l)
        for g in range(8):
            stg_t, stg_r = stg.next()
            for tl in range(4):
                t = g * 4 + tl
                x_t, x_r = xs.next()
                S.dma("sp", x_t[:], src[t * 128:(t + 1) * 128, :], [self.xres_r[2 * t], self.xres_r[2 * t + 1]], [x_r])
                xT_t, xT_r = xT.next()
                for hb in range(2):
                    pt, pt_r = ps_t.next()
                    for kk in range(4):
                        k = hb * 4 + kk
                        S.pe(lambda: nc.tensor.transpose(pt[:, kk * 128:(kk + 1) * 128], x_t[:, k * 128:(k + 1) * 128],
                                                         self.ident_f[:]), [x_r, self.cr2], [pt_r])
                    S.act(lambda: nc.scalar.copy(out=xT_t[:, hb * 4:(hb + 1) * 4, :].rearrange("p a b -> p (a b)"),
                                                 in_=pt[:, :]), [pt_r], [xT_r])
                v_t, v_r = vst.next()
                for cg in range(6):
                    if LVL < 2:
                        break
                    po, po_r = ps_o.next()
                    for k in range(8):
                        S.pe(lambda: nc.tensor.matmul(po[:, :], lhsT=xT_t[:, k, :], rhs=W[:, k, cg * 512:(cg + 1) * 512],
                                                      start=(k == 0), stop=(k == 7)), [xT_r, w_r[k]], [po_r])
                    if cg in (2, 5):
                        off = 0 if cg == 2 else 512
                        S.act(lambda: nc.scalar.copy(out=v_t[:, off:off + 512], in_=po[:, :]), [po_r], [v_r])
                        continue
                    cgi = {0: 0, 1: 1, 3: 2, 4: 3}[cg]
                    if LVL < 3:
                        continue
                    q_t, q_r = qs.next()
                    S.act(lambda: nc.scalar.copy(out=q_t[:, :], in_=po[:, :]), [po_r], [q_r])
                    a1, a1_r = t1.next()
                    a2, a2_r = t2.next()
                    pov = po[:, :].rearrange("p (h d) -> p h d", h=8)
                    qv = q_t[:, :].rearrange("p (h d) -> p h d", h=8)
                    cv = c2e[:, t, :].rearrange("p (h d) -> p h d", h=8)
                    sv = s2e[:, t, :].rearrange("p (h d) -> p h d", h=8)
                    S.dve(lambda: nc.vector.tensor_tensor(out=a1[:, :, :], in0=pov[:, :, 0:16], in1=cv, op=ALU.mult),
                          [po_r, tab_r], [a1_r])
                    S.dve(lambda: nc.vector.tensor_tensor(out=a2[:, :, 0:8], in0=pov[:, :, 8:16], in1=sv[:, :, 0:8],
                                                          op=ALU.mult), [po_r, tab_r2], [a2_r])
                    S.dve(lambda: nc.vector.tensor_tensor(out=a2[:, :, 8:16], in0=pov[:, :, 0:8], in1=sv[:, :, 8:16],
                                                          op=ALU.mult), [po_r, tab_r2], [a2_r])
                    S.dve(lambda: nc.vector.tensor_tensor(out=qv[:, :, 0:16], in0=a1[:, :, :], in1=a2[:, :, :], op=ALU.add),
                          [a1_r, a2_r], [q_r])
                    if LVL < 4:
                        continue
                    ptr, ptr_r = ps_tr.next()
                    for pr in range(4):
                        S.pe(lambda: nc.tensor.transpose(ptr[:, pr * 128:(pr + 1) * 128], q_t[:, pr * 128:(pr + 1) * 128],
                                                         self.ident_bf[:]), [q_r, self.cr], [ptr_r])
                    S.dve(lambda: nc.vector.tensor_copy(
                        out=stg_t[:, cgi * 4:(cgi + 1) * 4, tl * 128:(tl + 1) * 128],
                        in_=ptr[:, 0:512].rearrange("p (a b) -> p a b", a=4)), [ptr_r], [stg_r])
                if LVL >= 2:
                    S.dma("act", self.vd[t * 128:(t + 1) * 128, :], v_t[:, :], [v_r], [self.vd_r])
            for cgi in range(4):
                if LVL < 5:
                    break
                S.dma("sp", self.qkt[cgi, :, g * 512:(g + 1) * 512].rearrange("(a p) t -> p a t", p=128),
                      stg_t[:, cgi * 4:(cgi + 1) * 4, :], [stg_r], [self.qkt_r])
        P.close()

    def phase_attn(self, l):
        nc, S = self.nc, self.S
        P = Phase(S)
        dmh = P.sb([128, 20, 512], BF16)
        dmm = P.sb([128, 8, 512], BF16)
        cm = P.sb([128, 4, 512], BF16)
        S.dma("sp", dmh[:].rearrange("p a b -> p (a b)"), self.cd["ldm_hi"], [], [Reg()])
        S.dma("sp", dmm[:].rearrange("p a b -> p (a b)"), self.cd["dmm"], [], [Reg()])
        S.dma("sp", cm[:].rearrange("p a b -> p (a b)"), self.cd["lcm"], [], [Reg()])
        valid = P.sb([128, 512], F32)
        negv = P.sb([128, 512], F32)
        ownm1 = P.sb([128, 512], F32)
        ones_f = P.sb([128, 64], F32)
        k_r = Reg()
        for tl_, nm in ((valid, "valid"), (negv, "negv"), (ownm1, "ownm1"), (ones_f, "ones_f")):
            r = Reg()
            S.dma("sp", tl_[:], self.cd[nm], [], [r])
            k_r = r
        cst_r = Reg()
        QT = [P.sb([128, T], BF16) for _ in range(2)]
        KT = [P.sb([128, T], BF16) for _ in range(2)]
        VA = [P.sb([128, 32, 128], BF16) for _ in range(2)]
        QT_r = [Reg(), Reg()]
        QTb_r = [Reg(), Reg()]
        KT_r = [Reg(), Reg()]
        KTc_r = [Reg(), Reg()]
        VA_r = [[Reg() for _ in range(4)] for _ in range(2)]
        VAo_r = [Reg(), Reg()]
        for b in range(2):
            S.pool(lambda: nc.gpsimd.memset(QT[b][0:64, :], 0.0), [], [QTb_r[b]])
            S.pool(lambda: nc.gpsimd.memset(KT[b][0:64, :], 0.0), [], [KTc_r[b]])
            S.dma("sp", KT[b][0:16, :], self.cd["koh"], [KTc_r[b]], [KTc_r[b]])
            S.pool(lambda: nc.gpsimd.memset(VA[b][:, :, 64:128], 1.0), [], [VAo_r[b]])
        S.barrier()
        ps_s = P.rot_ps(4, [128, 512], F32)
        ps_acc = P.rot_ps(2, [128, 512], F32)
        ps_g = P.ps([128, 512], F32)
        ps_g_r = Reg(True)
        ps_b = P.rot_ps(1, [128, 1024], BF16)
        pb = P.rot_sb(6, [128, 512], BF16)
        km = P.sb([128, 16], F32)
        kmh = P.sb([128, 16], BF16)
        kml = P.sb([128, 16], BF16)
        kmt = P.sb([128, 16], F32)
        km_r = Reg()
        gm = P.sb([128, 32, 16], F32)
        gm_r = Reg()
        m8 = P.sb([128, 32, 8], F32)
        m8_r = Reg()
        sel = P.sb([128, 32, 16], F32)
        sel_r = Reg()
        bia = P.sb([128, 32, 16], BF16)
        bia_r = Reg()
        den = P.rot_sb(2, [128, 512], F32)
        bcs = P.rot_sb(2, [64, 512], F32)
        ost = P.rot_sb(2, [64, 512], BF16)
        def prep_steps(h):
            b = h % 2
            moba = h < 8
            qc, kc = (0, 1) if moba else (2, 3)
            hh = h % 8
            steps = []

            def loads():
                S.dma("sp", QT[b][64:128, :], self.qkt[qc, hh * 64:(hh + 1) * 64, :], [self.qkt_r], [QT_r[b]])
                S.dma("sp", KT[b][64:128, :], self.qkt[kc, hh * 64:(hh + 1) * 64, :], [self.qkt_r], [KT_r[b]])
                for q4 in range(4):
                    S.dma("sp", VA[b][:, q4 * 8:(q4 + 1) * 8, 0:64],
                          self.vd[q4 * 1024:(q4 + 1) * 1024, h * 64:(h + 1) * 64].rearrange("(t p) c -> p t c", p=128),
                          [self.vd_r], [VA_r[b][q4]])
            steps.append(loads)
            if not moba:
                if h in (8, 9):
                    steps.append(lambda: S.pool(lambda: nc.gpsimd.memset(QT[b][0:16, :], 0.0), [], [QTb_r[b]]))
                return steps

            def gate1():
                S.dve(lambda: nc.vector.tensor_reduce(out=km[64:128, :],
                                                      in_=KT[b][64:128, :].rearrange("p (a c) -> p a c", a=16),
                                                      axis=AX.X, op=ALU.add), [KT_r[b]], [km_r])
                S.dve(lambda: nc.vector.tensor_scalar(out=km[64:128, :], in0=km[64:128, :], scalar1=1.0 / 256.0, scalar2=None,
                                                      op0=ALU.mult), [km_r], [km_r])
                S.dve(lambda: nc.vector.tensor_copy(out=kmh[64:128, :], in_=km[64:128, :]), [km_r], [km_r])
                S.dve(lambda: nc.vector.tensor_tensor(out=kmt[64:128, :], in0=km[64:128, :], in1=kmh[64:128, :],
                                                      op=ALU.subtract), [km_r], [km_r])
                S.dve(lambda: nc.vector.tensor_copy(out=kml[64:128, :], in_=kmt[64:128, :]), [km_r], [km_r])
                for t in range(32):
                    S.pe(lambda: nc.tensor.matmul(ps_g[:, t * 16:(t + 1) * 16], lhsT=QT[b][64:128, t * 128:(t + 1) * 128],
                                                  rhs=kmh[64:128, :], start=True, stop=False), [QT_r[b], km_r], [ps_g_r])
                    S.pe(lambda: nc.tensor.matmul(ps_g[:, t * 16:(t + 1) * 16], lhsT=QT[b][64:128, t * 128:(t + 1) * 128],
                                                  rhs=kml[64:128, :], start=False, stop=True), [QT_r[b], km_r], [ps_g_r])
                gmf = gm[:].rearrange("p a b -> p (a b)")
                S.dve(lambda: nc.vector.tensor_tensor(out=gmf, in0=ps_g[:, :], in1=negv[:], op=ALU.add), [ps_g_r], [gm_r])
            steps.append(gate1)

            def gate2():
                for t in range(32):
                    S.dve(lambda: nc.vector.max(out=m8[:, t, :], in_=gm[:, t, :]), [gm_r], [m8_r])
            steps.append(gate2)

            def gate3():
                S.dve(lambda: nc.vector.tensor_tensor(out=sel[:, :, :], in0=gm[:, :, :],
                                                      in1=m8[:, :, 2:3].to_broadcast([128, 32, 16]), op=ALU.is_ge),
                      [gm_r, m8_r], [sel_r])
                self_f = sel[:].rearrange("p a b -> p (a b)")
                S.dve(lambda: nc.vector.tensor_tensor(out=self_f, in0=self_f, in1=valid[:], op=ALU.mult), [sel_r], [sel_r])
                S.dve(lambda: nc.vector.tensor_tensor(out=self_f, in0=self_f, in1=ownm1[:], op=ALU.add), [sel_r], [sel_r])
                S.dve(lambda: nc.vector.tensor_scalar(out=bia[:].rearrange("p a b -> p (a b)"), in0=self_f, scalar1=BIG,
                                                      scalar2=None, op0=ALU.mult), [sel_r], [bia_r])
            steps.append(gate3)

            def mk_tr(g4):
                def tr():
                    pbt, pbt_r = ps_b.next()
                    for tt in range(8):
                        t = g4 * 8 + tt
                        S.pe(lambda: nc.tensor.transpose(pbt[0:16, tt * 128:(tt + 1) * 128], bia[:, t, :], self.ident_bf[:]),
                             [bia_r], [pbt_r])
                    S.act(lambda: nc.scalar.copy(out=QT[b][0:16, g4 * 1024:(g4 + 1) * 1024], in_=pbt[0:16, :]),
                          [pbt_r], [QTb_r[b]])
                return tr
            for g4 in range(4):
                steps.append(mk_tr(g4))
            return steps

        def attend(h, steps):
            b = h % 2
            moba = h < 8
            lo = 0
            blocks = []
            for Q in range(8):
                ms = list(range(0, 4 * Q + 4)) if moba else list(range(max(0, 4 * Q - 16), 4 * Q + 4))
                for mi, m in enumerate(ms):
                    blocks.append((Q, mi, m, len(ms)))
            n = len(blocks)
            every = max(1, n // (len(steps) + 1)) if steps else n + 1
            sq = {}
            LOOK = 3

            def issue_S(i):
                Q, mi, m, nm = blocks[i]
                di_ = (512 * Q - 128 * m + 384) // 128
                extra = []
                if moba:
                    if di_ <= 3:
                        extra.append(cm[:, di_, :])
                elif not self.dm_lo_need[di_]:
                    extra.append(dmh[:, di_, :])
                sp_, sp_r = ps_s.next()
                S.pe(lambda: nc.tensor.matmul(sp_[:, :], lhsT=KT[b][lo:128, m * 128:(m + 1) * 128],
                                              rhs=QT[b][lo:128, Q * 512:(Q + 1) * 512], start=True, stop=(not extra)),
                     [KT_r[b], KTc_r[b], QT_r[b], QTb_r[b]], [sp_r])
                for xi, xm in enumerate(extra):
                    S.pe(lambda: nc.tensor.matmul(sp_[:, :], lhsT=self.ident_bf[:, :], rhs=xm, start=False,
                                                  stop=(xi == len(extra) - 1)), [], [sp_r])
                sq[i] = (sp_, sp_r)

            fin = []

            def fin_pe(Q, acc, acc_r, dn, dn_r):
                bp, bp_r = ps_s.next()
                S.pe(lambda: nc.tensor.matmul(bp[0:64, :], lhsT=ones_f[64:65, 0:64], rhs=dn[64:65, :], start=True, stop=True),
                     [dn_r], [bp_r])
                bc, bc_r = bcs.next()
                S.act(lambda: nc.scalar.copy(out=bc[:, :], in_=bp[0:64, :]), [bp_r], [bc_r])
                o_t, o_r = ost.next()
                S.dve(lambda: nc.vector.tensor_tensor(out=o_t[:, :], in0=acc[0:64, :], in1=bc[:, :], op=ALU.mult),
                      [acc_r, bc_r], [o_r])
                S.dma("sp", self.ot[h * 64:(h + 1) * 64, Q * 512:(Q + 1) * 512], o_t[:, :], [o_r], [self.ot_r])

            for i in range(min(LOOK, n)):
                issue_S(i)
            acc = acc_r = None
            for i in range(n):
                Q, mi, m, nm = blocks[i]
                di_ = (512 * Q - 128 * m + 384) // 128
                sp_, sp_r = sq.pop(i)
                p_t, p_r = pb.next()
                S.act(lambda: nc.scalar.activation(out=p_t[:, :], in_=sp_[:, :], func=AF.Exp, scale=0.125), [sp_r], [p_r])
                if (not moba) and self.dm_lo_need[di_]:
                    S.dve(lambda: nc.vector.tensor_tensor(out=p_t[:, :], in0=p_t[:, :], in1=dmm[:, di_, :], op=ALU.mult),
                          [p_r], [p_r])
                if i + LOOK < n:
                    issue_S(i + LOOK)
                if mi == 0:
                    acc, acc_r = ps_acc.next()
                S.pe(lambda: nc.tensor.matmul(acc[:, :], lhsT=VA[b][:, m, :], rhs=p_t[:, :],
                                              start=(mi == 0), stop=(mi == nm - 1)), [p_r, VA_r[b][m // 8], VAo_r[b]], [acc_r])
                for f in fin:
                    f[0] -= 1
                while fin and fin[0][0] <= 0:
                    f = fin.pop(0)
                    fin_pe(*f[1:])
                if mi == nm - 1:
                    dn, dn_r = den.next()
                    S.act(lambda: nc.scalar.copy(out=dn[64:65, :], in_=acc[64:65, :]), [acc_r], [dn_r])
                    S.dve(lambda: nc.vector.reciprocal(out=dn[64:65, :], in_=dn[64:65, :]), [dn_r], [dn_r])
                    fin.append([2, Q, acc, acc_r, dn, dn_r])
                if steps and (i + 1) % every == 0:
                    steps.pop(0)()
            while fin:
                f = fin.pop(0)
                fin_pe(*f[1:])
            while steps:
                steps.pop(0)()

        for st_ in prep_steps(0):
            st_()
        for h in range(16):
            nxt = prep_steps(h + 1) if h + 1 < 16 else []
            attend(h, nxt)
        P.close()


    def phase_hgrn(self, l):
        nc, S = self.nc, self.S
        j = l // 2
        P = Phase(S)
        V = nc.vector
        G_ = nc.gpsimd
        W = P.sb([128, 8, 4096], BF16)
        w_r = []
        for k in range(8):
            r = Reg()
            S.dma("pool", W[:, k, :], self.c_w_in[j, k * 128:(k + 1) * 128, :], [], [r])
            w_r.append(r)
        mg = P.sb([64, 64], F32)
        ind = P.sb([64, 3], F32)
        cmask8 = P.sb([64, 512], F32)
        cng = P.sb([64, D], F32)
        lb = P.sb([64, D], F32)
        oml = P.sb([64, D], F32)
        l0 = P.sb([64, D], F32)
        cr = Reg()
        for tl_, ap_ in ((mg, self.cd["mg"]), (ind, self.cd["ind"]), (cmask8, self.cd["cmask8"]), (cng, self.cng[j]),
                         (lb, self.lbl[1]), (l0, self.lbl[0])):
            S.dma("sp", tl_[:], ap_, [], [Reg()])
        S.barrier()
        if j == 0:
            S.dve(lambda: V.memset(lb[:], 0.0), [], [cr])
            S.dve(lambda: V.memset(oml[:], 1.0), [], [cr])
        else:
            S.dve(lambda: V.tensor_tensor(out=lb[:], in0=lb[:], in1=l0[:], op=ALU.subtract), [], [cr])
            S.act(lambda: nc.scalar.activation(out=lb[:], in_=lb[:], func=AF.Sigmoid), [cr], [cr])
            S.dve(lambda: V.tensor_scalar(out=oml[:], in0=lb[:], scalar1=-1.0, scalar2=1.0, op0=ALU.mult, op1=ALU.add),
                  [cr], [cr])
        Sst = P.sb([128, 8, 128], F32)
        Sst_r = Reg()
        S.dve(lambda: V.memset(Sst[:].rearrange("p a b -> p (a b)"), 0.0), [], [Sst_r])
        S.barrier()
        banks = P.rot_ps(6, [128, 512], F32)
        pbf = P.rot_ps(2, [128, 1024], BF16)
        xs = P.rot_sb(2, [64, D], F32)
        xT = P.rot_sb(2, [128, 8, 64], BF16)
        fbuf = P.rot_sb(1, [64, D], F32)
        lfb = P.rot_sb(1, [64, D], F32)
        kkb = P.rot_sb(1, [64, D], F32)
        eGb = P.rot_sb(1, [64, D], F32)
        enGb = P.rot_sb(1, [64, D], F32)
        qtb = P.rot_sb(2, [64, D], BF16)
        ktb = P.rot_sb(2, [64, D], BF16)
        vb = P.rot_sb(2, [64, D], BF16)
        sgb = P.rot_sb(2, [64, D], F32)
        abcb = P.rot_sb(2, [128, 8, 3], F32)
        qTb = P.rot_sb(2, [128, 8, 64], BF16)
        kTb = P.rot_sb(2, [128, 8, 64], BF16)
        ATb = P.rot_sb(2, [64, 512], BF16)
        smb = P.rot_sb(2, [128, 8, 128], BF16)
        tmpb = P.rot_sb(1, [128, 8, 128], F32)
        sqb = P.rot_sb(1, [64, D], F32)
        msb = P.rot_sb(2, [64, 16], F32)
        onb = P.rot_sb(1, [64, D], F32)
        ogb = P.rot_sb(2, [64, D], BF16)
        stg = P.rot_sb(2, [128, 8, 512], BF16)
        src = self.xsrc(l)
        idf = self.ident_f
        idb = self.ident_bf
        for g in range(8):
            stg_t, stg_r = stg.next()
            for tl in range(8):
                t = g * 8 + tl
                x_t, x_r = xs.next()
                S.dma("sp", x_t[:], src[t * 64:(t + 1) * 64, :], [self.xres_r[t]], [x_r])
                pt, pt_r = banks.next()
                for k in range(8):
                    S.pe(lambda: nc.tensor.transpose(pt[:, k * 64:(k + 1) * 64], x_t[:, k * 128:(k + 1) * 128], idf[0:64, 0:64]),
                         [x_r], [pt_r])
                xT_t, xT_r = xT.next()
                S.act(lambda: nc.scalar.copy(out=xT_t[:].rearrange("p a b -> p (a b)"), in_=pt[:, :]), [pt_r], [xT_r])

                def proj(cg):
                    po, po_r = banks.next()
                    for k in range(8):
                        S.pe(lambda: nc.tensor.matmul(po[0:64, :], lhsT=xT_t[:, k, :], rhs=W[:, k, cg * 512:(cg + 1) * 512],
                                                      start=(k == 0), stop=(k == 7)), [xT_r, w_r[k]], [po_r])
                    return po, po_r

                f_t, f_r = fbuf.next()
                for hf in range(2):
                    po, po_r = proj(2 + hf)
                    S.act(lambda: nc.scalar.activation(out=f_t[:, hf * 512:(hf + 1) * 512], in_=po[0:64, :], func=AF.Sigmoid),
                          [po_r], [f_r])
                S.dve(lambda: V.tensor_tensor(out=f_t[:], in0=f_t[:], in1=oml[:], op=ALU.mult), [f_r, cr], [f_r])
                S.dve(lambda: V.tensor_tensor(out=f_t[:], in0=f_t[:], in1=lb[:], op=ALU.add), [f_r, cr], [f_r])
                lf_t, lf_r = lfb.next()
                S.act(lambda: nc.scalar.activation(out=lf_t[:], in_=f_t[:], func=AF.Ln), [f_r], [lf_r])
                kk_t, kk_r = kkb.next()
                S.pool(lambda: G_.tensor_scalar(out=kk_t[:], in0=f_t[:], scalar1=-1.0, scalar2=1.0, op0=ALU.mult, op1=ALU.add),
                       [f_r], [kk_r])
                eG_t, eG_r = eGb.next()
                enG_t, enG_r = enGb.next()
                for hf in range(2):
                    pg_, pg_r = banks.next()
                    S.pe(lambda: nc.tensor.matmul(pg_[0:64, :], lhsT=mg[:, :], rhs=lf_t[:, hf * 512:(hf + 1) * 512],
                                                  start=True, stop=True), [lf_r], [pg_r])
                    S.act(lambda: nc.scalar.activation(out=eG_t[:, hf * 512:(hf + 1) * 512], in_=pg_[0:64, :], func=AF.Exp),
                          [pg_r], [eG_r])
                    S.act(lambda: nc.scalar.activation(out=enG_t[:, hf * 512:(hf + 1) * 512], in_=pg_[0:64, :], func=AF.Exp,
                                                       scale=-1.0), [pg_r], [enG_r])
                pst, pst_r = banks.next()
                for h in range(8):
                    S.pe(lambda: nc.tensor.matmul(pst[:, h * 3:(h + 1) * 3], lhsT=lf_t[:, h * 128:(h + 1) * 128], rhs=ind[:, :],
                                                  start=True, stop=True), [lf_r], [pst_r])
                abc, abc_r = abcb.next()
                S.act(lambda: nc.scalar.activation(out=abc[:].rearrange("p a b -> p (a b)"), in_=pst[:, 0:24], func=AF.Exp),
                      [pst_r], [abc_r])
                qt_t, qt_r = qtb.next()
                for hf in range(2):
                    po, po_r = proj(hf)
                    S.dve(lambda: V.tensor_tensor(out=qt_t[:, hf * 512:(hf + 1) * 512], in0=po[0:64, :],
                                                  in1=eG_t[:, hf * 512:(hf + 1) * 512], op=ALU.mult), [po_r, eG_r], [qt_r])
                kt_t, kt_r = ktb.next()
                S.pool(lambda: G_.tensor_tensor(out=kt_t[:], in0=kk_t[:], in1=enG_t[:], op=ALU.mult), [kk_r, enG_r], [kt_r])
                v_t, v_r = vb.next()
                for hf in range(2):
                    po, po_r = proj(4 + hf)
                    S.act(lambda: nc.scalar.copy(out=v_t[:, hf * 512:(hf + 1) * 512], in_=po[0:64, :]), [po_r], [v_r])
                sg_t, sg_r = sgb.next()
                for hf in range(2):
                    po, po_r = proj(6 + hf)
                    S.act(lambda: nc.scalar.activation(out=sg_t[:, hf * 512:(hf + 1) * 512], in_=po[0:64, :], func=AF.Silu),
                          [po_r], [sg_r])
                qT_t, qT_r = qTb.next()
                kT_t, kT_r = kTb.next()
                for (src_t, src_r, dst_t, dst_r) in ((qt_t, qt_r, qT_t, qT_r), (kt_t, kt_r, kT_t, kT_r)):
                    pb_, pb_r = pbf.next()
                    for h in range(8):
                        S.pe(lambda: nc.tensor.transpose(pb_[:, h * 64:(h + 1) * 64], src_t[:, h * 128:(h + 1) * 128],
                                                         idb[0:64, 0:64]), [src_r], [pb_r])
                    S.dve(lambda: V.tensor_copy(out=dst_t[:].rearrange("p a b -> p (a b)"), in_=pb_[:, 0:512]), [pb_r], [dst_r])
                pat, pat_r = banks.next()
                for h in range(8):
                    S.pe(lambda: nc.tensor.matmul(pat[0:64, h * 64:(h + 1) * 64], lhsT=kT_t[:, h, :], rhs=qT_t[:, h, :],
                                                  start=True, stop=True), [kT_r, qT_r], [pat_r])
                AT_t, AT_r = ATb.next()
                S.dve(lambda: V.tensor_tensor(out=AT_t[:, :], in0=pat[0:64, :], in1=cmask8[:, :], op=ALU.mult), [pat_r], [AT_r])
                sm_t, sm_r = smb.next()
                S.pool(lambda: G_.tensor_tensor(out=sm_t[:, :, :], in0=Sst[:, :, :],
                                                in1=abc[:, :, 0:1].to_broadcast([128, 8, 128]), op=ALU.mult),
                       [Sst_r, abc_r], [sm_r])
                po0, po0_r = banks.next()
                po1, po1_r = banks.next()
                for h in range(8):
                    ob, ob_r = (po0, po0_r) if h < 4 else (po1, po1_r)
                    c0 = (h % 4) * 128
                    S.pe(lambda: nc.tensor.matmul(ob[0:64, c0:c0 + 128], lhsT=qT_t[:, h, :], rhs=sm_t[:, h, :],
                                                  start=True, stop=False), [qT_r, sm_r], [ob_r])
                    S.pe(lambda: nc.tensor.matmul(ob[0:64, c0:c0 + 128], lhsT=AT_t[:, h * 64:(h + 1) * 64],
                                                  rhs=v_t[:, h * 128:(h + 1) * 128], start=False, stop=True),
                         [AT_r, v_r], [ob_r])
                pk0, pk0_r = banks.next()
                pk1, pk1_r = banks.next()
                for h in range(8):
                    kb, kb_r = (pk0, pk0_r) if h < 4 else (pk1, pk1_r)
                    c0 = (h % 4) * 128
                    S.pe(lambda: nc.tensor.matmul(kb[:, c0:c0 + 128], lhsT=kt_t[:, h * 128:(h + 1) * 128],
                                                  rhs=v_t[:, h * 128:(h + 1) * 128], start=True, stop=True),
                         [kt_r, v_r], [kb_r])
                tmp_t, tmp_r = tmpb.next()
                for hb, (kb, kb_r) in enumerate(((pk0, pk0_r), (pk1, pk1_r))):
                    S.dve(lambda: V.tensor_tensor(out=tmp_t[:, hb * 4:(hb + 1) * 4, :],
                                                  in0=kb[:, :].rearrange("p (a b) -> p a b", a=4),
                                                  in1=abc[:, hb * 4:(hb + 1) * 4, 2:3].to_broadcast([128, 4, 128]), op=ALU.mult),
                          [kb_r, abc_r], [tmp_r])
                S.pool(lambda: G_.tensor_tensor(out=Sst[:, :, :], in0=Sst[:, :, :],
                                                in1=abc[:, :, 1:2].to_broadcast([128, 8, 128]), op=ALU.mult),
                       [Sst_r, abc_r], [Sst_r])
                S.pool(lambda: G_.tensor_tensor(out=Sst[:, :, :], in0=Sst[:, :, :], in1=tmp_t[:, :, :], op=ALU.add),
                       [Sst_r, tmp_r], [Sst_r])
                sq_t, sq_r = sqb.next()
                for hb, (ob, ob_r) in enumerate(((po0, po0_r), (po1, po1_r))):
                    S.act(lambda: nc.scalar.activation(out=sq_t[:, hb * 512:(hb + 1) * 512], in_=ob[0:64, :], func=AF.Square),
                          [ob_r], [sq_r])
                ms_t, ms_r = msb.next()
                S.dve(lambda: V.tensor_reduce(out=ms_t[:, 0:8], in_=sq_t[:].rearrange("p (a b) -> p a b", a=8), axis=AX.X,
                                              op=ALU.add), [sq_r], [ms_r])
                S.dve(lambda: V.tensor_scalar(out=ms_t[:, 0:8], in0=ms_t[:, 0:8], scalar1=1.0 / 128.0, scalar2=RMS_EPS,
                                              op0=ALU.mult, op1=ALU.add), [ms_r], [ms_r])
                S.act(lambda: nc.scalar.activation(out=ms_t[:, 0:8], in_=ms_t[:, 0:8], func=AF.Ln), [ms_r], [ms_r])
                S.act(lambda: nc.scalar.activation(out=ms_t[:, 8:16], in_=ms_t[:, 0:8], func=AF.Exp, scale=-0.5), [ms_r], [ms_r])
                on_t, on_r = onb.next()
                for hb, (ob, ob_r) in enumerate(((po0, po0_r), (po1, po1_r))):
                    S.dve(lambda: V.tensor_tensor(out=on_t[:, hb * 512:(hb + 1) * 512].rearrange("p (a b) -> p a b", a=4),
                                                  in0=ob[0:64, :].rearrange("p (a b) -> p a b", a=4),
                                                  in1=ms_t[:, 8 + hb * 4:8 + (hb + 1) * 4].unsqueeze(2).to_broadcast([64, 4, 128]),
                                                  op=ALU.mult), [ob_r, ms_r], [on_r])
                S.pool(lambda: G_.tensor_tensor(out=on_t[:], in0=on_t[:], in1=cng[:], op=ALU.mult), [on_r], [on_r])
                og_t, og_r = ogb.next()
                S.pool(lambda: G_.tensor_tensor(out=og_t[:], in0=on_t[:], in1=sg_t[:], op=ALU.mult), [on_r, sg_r], [og_r])
                pb_, pb_r = pbf.next()
                for h in range(8):
                    S.pe(lambda: nc.tensor.transpose(pb_[:, h * 64:(h + 1) * 64], og_t[:, h * 128:(h + 1) * 128], idb[0:64, 0:64]),
                         [og_r], [pb_r])
                S.act(lambda: nc.scalar.copy(out=stg_t[:, :, tl * 64:(tl + 1) * 64],
                                             in_=pb_[:, 0:512].rearrange("p (a b) -> p a b", a=8)), [pb_r], [stg_r])
            for k in range(8):
                S.dma("act", self.ot[k * 128:(k + 1) * 128, g * 512:(g + 1) * 512], stg_t[:, k, :], [stg_r], [self.ot_r])
        P.close()

    def phase_out_ln_router(self, l, w_out_ap):
        nc, S = self.nc, self.S
        V = nc.vector
        P = Phase(S)
        W = P.sb([128, 8, D], BF16)
        w_r = []
        for k in range(8):
            r = Reg()
            S.dma("pool", W[:, k, :], w_out_ap[k * 128:(k + 1) * 128, :], [], [r])
            w_r.append(r)
        g_bc = P.sb([128, D], F32)
        b_bc = P.sb([128, D], F32)
        rw = P.sb([128, 8, 36], F32)
        rb = P.sb([128, 36], F32)
        u_bf = P.sb([128, 128], BF16)
        ones_bf = P.sb([128, 128], BF16)
        ecb = P.sb([128, 128], F32)
        carry = P.sb([128, 32], F32)
        pid = P.sb([128, 8], F32)
        S.dma("sp", pid[:], self.cd["pid"], [], [Reg()])
        for tl_, ap_ in ((g_bc, self.lng[l * 2]), (b_bc, self.lnb[l * 2]), (rw, self.rw[l].rearrange("(k p) n -> p k n", p=128)),
                         (rb, self.rb[l]), (u_bf, self.cd["u_bf"]), (ones_bf, self.cd["ones_bf"]), (ecb, self.cd["ecb"])):
            S.dma("sp", tl_[:], ap_, [], [Reg()])
        car_r = Reg()
        S.dve(lambda: V.memset(carry[:], 0.0), [], [car_r])
        S.barrier()
        gb_r = Reg()
        oT = Rot([(P.sb([128, 8, 512], BF16), [Reg() for _ in range(8)]) for _ in range(2)])
        xs = P.rot_sb(2, [128, D], F32)
        ys = P.rot_sb(2, [128, D], F32)
        xa = P.rot_sb(2, [128, D], F32)
        xbf = P.rot_sb(8, [128, D], BF16)
        ps_m = P.rot_ps(2, [128, 1024], F32)
        ps_t = P.rot_ps(1, [128, 1024], F32)
        ps_r = P.rot_ps(2, [128, 512], F32)
        xTf = P.rot_sb(2, [128, 8, 128], F32)
        st = P.sb([128, 12], F32)
        mv = P.sb([128, 2], F32)
        rs = P.sb([128, 1], F32)
        scr = (st, Reg(), mv, Reg(), rs, Reg())
        lgs = P.rot_sb(2, [128, 4, 36], F32)
        sm = P.sb([128, 16, 4], F32)
        oh = P.sb([128, 4, 4], F32)
        eg = P.sb([128, 4, 4], F32)
        lem = P.sb([128, 4, 32], F32)
        o1 = P.sb([128, 4, 32], F32)
        o2 = P.sb([128, 4, 32], F32)
        mbf = P.sb([128, 4, 32], BF16)
        rf = P.sb([128, 4, 32], F32)
        tmp = P.sb([128, 4, 32], F32)
        rk = P.sb([128, 4, 2], F32)
        bs = P.sb([128, 4, 2], F32)
        ov = P.sb([128, 4, 2], F32)
        ps_ = P.sb([128, 4, 2], F32)
        pd = P.sb([128, 4, 2], F32)
        possi = P.rot_sb(2, [128, 4, 2], I32)
        rr = Reg()
        src = self.xsrc(l)
        f3 = lambda t: t[:].rearrange("p a b -> p (a b)")
        for g in range(8):
            oT_t, o_regs = oT.next()
            for k in range(8):
                S.dma("sp", oT_t[:, k, :], self.ot[k * 128:(k + 1) * 128, g * 512:(g + 1) * 512], [self.ot_r], [o_regs[k]])
            lg, lg_r = lgs.next()
            xb_list = []
            for tl in range(4):
                t = g * 4 + tl
                x_t, x_r = xs.next()
                S.dma("sp", x_t[:], src[t * 128:(t + 1) * 128, :], [self.xres_r[2 * t], self.xres_r[2 * t + 1]], [x_r])
                pm, pm_r = ps_m.next()
                for hf in range(2):
                    for k in range(8):
                        S.pe(lambda: nc.tensor.matmul(pm[:, hf * 512:(hf + 1) * 512], lhsT=oT_t[:, k, tl * 128:(tl + 1) * 128],
                                                      rhs=W[:, k, hf * 512:(hf + 1) * 512], start=(k == 0), stop=(k == 7)),
                             [o_regs[k], w_r[k]], [pm_r])
                y_t, y_r = ys.next()
                for hf in range(2):
                    S.dve(lambda: V.scalar_tensor_tensor(out=y_t[:, hf * 512:(hf + 1) * 512],
                                                         in0=x_t[:, hf * 512:(hf + 1) * 512], scalar=ALPHA,
                                                         in1=pm[:, hf * 512:(hf + 1) * 512], op0=ALU.mult, op1=ALU.add),
                          [x_r, pm_r], [y_r])
                xa_t, xa_r = xa.next()
                self.layer_norm_tile(P, 128, y_t, y_r, g_bc, b_bc, gb_r, xa_t, xa_r, scr)
                S.dma("pool", self.xres[t * 128:(t + 1) * 128, :], xa_t[:, :], [xa_r],
                      [self.xres_r[2 * t], self.xres_r[2 * t + 1]])
                xb_t, xb_r = xbf.next()
                S.act(lambda: nc.scalar.copy(out=xb_t[:, :], in_=xa_t[:, :]), [xa_r], [xb_r])
                xb_list.append((xb_t, xb_r))
                pt, pt_r = ps_t.next()
                for k in range(8):
                    S.pe(lambda: nc.tensor.transpose(pt[:, k * 128:(k + 1) * 128], xa_t[:, k * 128:(k + 1) * 128],
                                                     self.ident_f[:]), [xa_r], [pt_r])
                xf, xf_r = xTf.next()
                S.act(lambda: nc.scalar.copy(out=xf[:, 0:4, :], in_=pt[:, 0:512].rearrange("p (a b) -> p a b", a=4)),
                      [pt_r], [xf_r])
                S.dve(lambda: V.tensor_copy(out=xf[:, 4:8, :], in_=pt[:, 512:1024].rearrange("p (a b) -> p a b", a=4)),
                      [pt_r], [xf_r])
                pr, pr_r = ps_r.next()
                for k in range(8):
                    S.pe(lambda: nc.tensor.matmul(pr[:, 0:36], lhsT=xf[:, k, :], rhs=rw[:, k, :], start=(k == 0), stop=(k == 7)),
                         [xf_r], [pr_r])
                S.dve(lambda: V.tensor_tensor(out=lg[:, tl, :], in0=pr[:, 0:36], in1=rb[:, :], op=ALU.add), [pr_r], [lg_r])
            R_ = [lg_r, rr]
            LG = lg[:, :, 0:4]
            LE = lg[:, :, 4:36]
            bc4 = lambda j: sm[:, j, :].unsqueeze(2).to_broadcast([128, 4, 4])
            bc32 = lambda j: sm[:, j, :].unsqueeze(2).to_broadcast([128, 4, 32])
            S.dve(lambda: V.tensor_reduce(out=sm[:, 0, :], in_=LG, axis=AX.X, op=ALU.max), R_, [rr])
            S.dve(lambda: V.tensor_tensor(out=oh[:, :, :], in0=LG, in1=bc4(0), op=ALU.is_ge), R_, [rr])
            S.dve(lambda: V.tensor_tensor(out=eg[:, :, :], in0=LG, in1=bc4(0), op=ALU.subtract), R_, [rr])
            S.act(lambda: nc.scalar.activation(out=f3(eg), in_=f3(eg), func=AF.Exp), [rr], [rr])
            S.dve(lambda: V.tensor_reduce(out=sm[:, 1, :], in_=eg[:, :, :], axis=AX.X, op=ALU.add), [rr], [rr])
            S.dve(lambda: V.reciprocal(out=sm[:, 2, :], in_=sm[:, 1, :]), [rr], [rr])
            S.dve(lambda: V.tensor_scalar(out=f3(oh), in0=f3(oh), scalar1=-1.0, scalar2=BIG, op0=ALU.add, op1=ALU.mult),
                  [rr], [rr])
            S.dve(lambda: V.tensor_tensor(out=lem[:].rearrange("p a (g e) -> p (a g) e", g=4),
                                          in0=LE.rearrange("p a (g e) -> p a g e", g=4),
                                          in1=oh[:, :, :].unsqueeze(3).to_broadcast([128, 4, 4, 8]), op=ALU.add)
                  if False else
                  V.tensor_tensor(out=lem[:, :, :].rearrange("p a (g e) -> p a g e", g=4),
                                  in0=LE.rearrange("p a (g e) -> p a g e", g=4),
                                  in1=oh[:, :, :].unsqueeze(3).to_broadcast([128, 4, 4, 8]), op=ALU.add), R_, [rr])
            S.dve(lambda: V.tensor_reduce(out=sm[:, 3, :], in_=lem[:, :, :], axis=AX.X, op=ALU.max), [rr], [rr])
            S.dve(lambda: V.tensor_tensor(out=o1[:, :, :], in0=lem[:, :, :], in1=bc32(3), op=ALU.is_ge), [rr], [rr])
            S.dve(lambda: V.scalar_tensor_tensor(out=f3(lem), in0=f3(o1), scalar=-BIG, in1=f3(lem), op0=ALU.mult, op1=ALU.add),
                  [rr], [rr])
            S.dve(lambda: V.tensor_reduce(out=sm[:, 4, :], in_=lem[:, :, :], axis=AX.X, op=ALU.max), [rr], [rr])
            S.dve(lambda: V.tensor_tensor(out=o2[:, :, :], in0=lem[:, :, :], in1=bc32(4), op=ALU.is_ge), [rr], [rr])
            S.dve(lambda: V.tensor_tensor(out=sm[:, 5, :], in0=sm[:, 4, :], in1=sm[:, 3, :], op=ALU.subtract), [rr], [rr])
            S.act(lambda: nc.scalar.activation(out=sm[:, 6, :], in_=sm[:, 5, :], func=AF.Exp), [rr], [rr])
            S.dve(lambda: V.tensor_scalar(out=sm[:, 7, :], in0=sm[:, 6, :], scalar1=1.0, scalar2=None, op0=ALU.add), [rr], [rr])
            S.dve(lambda: V.reciprocal(out=sm[:, 8, :], in_=sm[:, 7, :]), [rr], [rr])
            pgr = self.pg_r[g]
            S.dve(lambda: V.tensor_tensor(out=self.g12[:, g * 4:(g + 1) * 4, 0], in0=sm[:, 8, :], in1=sm[:, 2, :], op=ALU.mult),
                  [rr], [pgr])
            S.dve(lambda: V.tensor_tensor(out=self.g12[:, g * 4:(g + 1) * 4, 1], in0=self.g12[:, g * 4:(g + 1) * 4, 0],
                                          in1=sm[:, 6, :], op=ALU.mult), [rr, pgr], [pgr])
            S.dve(lambda: V.tensor_tensor(out=f3(mbf), in0=f3(o1), in1=f3(o2), op=ALU.add), [rr], [rr])
            pp, pp_r = ps_r.next()
            S.pe(lambda: nc.tensor.matmul(pp[:, 0:128], lhsT=u_bf[:, :], rhs=f3(mbf), start=True, stop=True), [rr], [pp_r])
            S.pe(lambda: nc.tensor.matmul(pp[:, 128:256], lhsT=ones_bf[:, :], rhs=f3(mbf), start=True, stop=True), [rr], [pp_r])
            for tl in range(4):
                S.dve(lambda: V.tensor_tensor(out=rf[:, tl, :], in0=pp[:, tl * 32:(tl + 1) * 32], in1=carry[:, :], op=ALU.add),
                      [pp_r, car_r], [rr])
                S.dve(lambda: V.tensor_tensor(out=carry[:, :], in0=carry[:, :], in1=pp[:, 128 + tl * 32:128 + (tl + 1) * 32],
                                              op=ALU.add), [pp_r, car_r], [car_r])
            for kk, ok in enumerate((o1, o2)):
                S.dve(lambda: V.tensor_tensor(out=f3(tmp), in0=f3(ok), in1=f3(rf), op=ALU.mult), [rr], [rr])
                S.dve(lambda: V.tensor_reduce(out=rk[:, :, kk], in_=tmp[:, :, :], axis=AX.X, op=ALU.add), [rr], [rr])
                S.dve(lambda: V.tensor_tensor(out=f3(tmp), in0=f3(ok), in1=ecb[:, :], op=ALU.mult), [rr], [rr])
                S.dve(lambda: V.tensor_reduce(out=bs[:, :, kk], in_=tmp[:, :, :], axis=AX.X, op=ALU.add), [rr], [rr])
            S.dve(lambda: V.tensor_scalar(out=f3(ov), in0=f3(rk), scalar1=float(CAP), scalar2=None, op0=ALU.is_ge), [rr], [rr])
            S.dve(lambda: V.tensor_tensor(out=f3(ps_), in0=f3(rk), in1=f3(bs), op=ALU.add), [rr], [rr])
            S.dve(lambda: V.tensor_tensor(out=f3(pd), in0=pid[:, :], in1=f3(ps_), op=ALU.subtract), [rr], [rr])
            S.dve(lambda: V.tensor_tensor(out=f3(pd), in0=f3(pd), in1=f3(ov), op=ALU.mult), [rr], [rr])
            S.dve(lambda: V.tensor_tensor(out=f3(ps_), in0=f3(ps_), in1=f3(pd), op=ALU.add), [rr], [rr])
            S.dve(lambda: V.tensor_copy(out=self.posg[:, g * 4:(g + 1) * 4, :].rearrange("p a b -> p (a b)"), in_=f3(ps_)),
                  [rr], [pgr])
            for tl in range(4):
                xb_t, xb_r = xb_list[tl]
                for kk in range(2):
                    S.idma(self.xs[:, :], bass.IndirectOffsetOnAxis(ap=self.posg[:, g * 4 + tl, kk:kk + 1], axis=0), xb_t[:, :],
                           None, None, [xb_r, pgr], [self.xs_r])
        P.close()

    def phase_moe(self, l, last):
        nc, S = self.nc, self.S
        V = nc.vector
        P = Phase(S)
        g_bc = P.sb([128, D], F32)
        b_bc = P.sb([128, D], F32)
        S.dma("sp", g_bc[:], self.lng[l * 2 + 1], [], [Reg()])
        S.dma("sp", b_bc[:], self.lnb[l * 2 + 1], [], [Reg()])
        S.barrier()
        gb_r = Reg()
        W1 = P.rot_sb(2, [128, 8, 512], BF16)
        W3 = P.rot_sb(2, [128, 8, 512], BF16)
        W2 = P.rot_sb(2, [128, 4, D], BF16)
        xsl = P.rot_sb(2, [128, 4, D], BF16)
        xgT = P.rot_sb(2, [128, 8, 512], BF16)
        ps_tr = P.rot_ps(2, [128, 1024], BF16)
        ps_h1 = P.rot_ps(2, [128, 512], F32)
        ps_h3 = P.rot_ps(2, [128, 512], F32)
        ps_y = P.rot_ps(2, [128, 512], F32)
        sl = P.rot_sb(2, [128, 512], F32)
        hT = P.rot_sb(2, [128, 4, 512], BF16)
        ysb = P.rot_sb(3, [128, D], F32)
        NB = CAP // 128
        for e in range(32):
            w1_t, w1_r = W1.next()
            w3_t, w3_r = W3.next()
            w2_t, w2_r = W2.next()
            S.dma("pool", w1_t[:], self.w1[l, e].rearrange("(k p) f -> p k f", p=128), [], [w1_r])
            S.dma("pool", w3_t[:], self.w3[l, e].rearrange("(k p) f -> p k f", p=128), [], [w3_r])
            S.dma("pool", w2_t[:], self.w2[l, e].rearrange("(k p) f -> p k f", p=128), [], [w2_r])
            xs_t, xs_r = xsl.next()
            S.dma("sp", xs_t[:, 0:NB, :], self.xs[e * CAP:(e + 1) * CAP, :].rearrange("(b p) d -> p b d", p=128),
                  [self.xs_r], [xs_r])
            xg, xg_r = xgT.next()
            for k2 in range(4):
                ptr, ptr_r = ps_tr.next()
                for kk in range(2):
                    k = k2 * 2 + kk
                    for bb in range(NB):
                        S.pe(lambda: nc.tensor.transpose(ptr[:, kk * 512 + bb * 128:kk * 512 + (bb + 1) * 128],
                                                         xs_t[:, bb, k * 128:(k + 1) * 128], self.ident_bf[:]), [xs_r], [ptr_r])
                if k2 % 2 == 0:
                    S.act(lambda: nc.scalar.copy(out=xg[:, k2 * 2:k2 * 2 + 2, :].rearrange("p a b -> p (a b)"), in_=ptr[:, :]),
                          [ptr_r], [xg_r])
                else:
                    S.dve(lambda: V.tensor_copy(out=xg[:, k2 * 2:k2 * 2 + 2, :].rearrange("p a b -> p (a b)"), in_=ptr[:, :]),
                          [ptr_r], [xg_r])
            h_t, h_r = hT.next()
            for fc in range(4):
                p1, p1_r = ps_h1.next()
                p3, p3_r = ps_h3.next()
                for k in range(8):
                    S.pe(lambda: nc.tensor.matmul(p1[:, :], lhsT=w1_t[:, k, fc * 128:(fc + 1) * 128], rhs=xg[:, k, :],
                                                  start=(k == 0), stop=(k == 7)), [w1_r, xg_r], [p1_r])
                for k in range(8):
                    S.pe(lambda: nc.tensor.matmul(p3[:, :], lhsT=w3_t[:, k, fc * 128:(fc + 1) * 128], rhs=xg[:, k, :],
                                                  start=(k == 0), stop=(k == 7)), [w3_r, xg_r], [p3_r])
                s_t, s_r = sl.next()
                S.act(lambda: nc.scalar.activation(out=s_t[:, :], in_=p1[:, :], func=AF.Silu), [p1_r], [s_r])
                S.dve(lambda: V.tensor_tensor(out=h_t[:, fc, :], in0=p3[:, :], in1=s_t[:, :], op=ALU.mult), [p3_r, s_r], [h_r])
            for st_ in range(NB):
                y_t, y_r = ysb.next()
                for dh in range(2):
                    py, py_r = ps_y.next()
                    for fc in range(4):
                        S.pe(lambda: nc.tensor.matmul(py[:, :], lhsT=h_t[:, fc, st_ * 128:(st_ + 1) * 128],
                                                      rhs=w2_t[:, fc, dh * 512:(dh + 1) * 512], start=(fc == 0), stop=(fc == 3)),
                             [h_r, w2_r], [py_r])
                    if dh == 0:
                        S.act(lambda: nc.scalar.copy(out=y_t[:, 0:512], in_=py[:, :]), [py_r], [y_r])
                    else:
                        S.dve(lambda: V.tensor_copy(out=y_t[:, 512:1024], in_=py[:, :]), [py_r], [y_r])
                S.dma("sp", self.yb[e * CAP + st_ * 128:e * CAP + (st_ + 1) * 128, :], y_t[:, :], [y_r], [self.yb_r])
        S.barrier()
        xs2 = P.rot_sb(2, [128, D], F32)
        ya = P.rot_sb(2, [128, D], F32)
        ybb = P.rot_sb(2, [128, D], F32)
        ys = P.rot_sb(2, [128, D], F32)
        xo = P.rot_sb(2, [128, D], F32)
        st = P.sb([128, 12], F32)
        mv = P.sb([128, 2], F32)
        rs = P.sb([128, 1], F32)
        scr = (st, Reg(), mv, Reg(), rs, Reg())
        dst = self.out if last else self.xres
        for t in range(32):
            pgr = self.pg_r[t // 4]
            x_t, x_r = xs2.next()
            S.dma("sp", x_t[:], self.xres[t * 128:(t + 1) * 128, :], [self.xres_r[2 * t], self.xres_r[2 * t + 1]], [x_r])
            a_t, a_r = ya.next()
            b_t, b_r = ybb.next()
            S.idma(a_t[:, :], None, self.yb[:, :], bass.IndirectOffsetOnAxis(ap=self.posg[:, t, 0:1], axis=0), NS + 127,
                   [self.yb_r, pgr], [a_r])
            S.idma(b_t[:, :], None, self.yb[:, :], bass.IndirectOffsetOnAxis(ap=self.posg[:, t, 1:2], axis=0), NS + 127,
                   [self.yb_r, pgr], [b_r])
            y_t, y_r = ys.next()
            S.act(lambda: nc.scalar.activation(out=y_t[:, :], in_=x_t[:, :], func=AF.Copy, scale=ALPHA), [x_r], [y_r])
            S.dve(lambda: V.scalar_tensor_tensor(out=y_t[:, :], in0=a_t[:, :], scalar=self.g12[:, t, 0:1], in1=y_t[:, :],
                                                 op0=ALU.mult, op1=ALU.add), [a_r, pgr, y_r], [y_r])
            S.dve(lambda: V.scalar_tensor_tensor(out=y_t[:, :], in0=b_t[:, :], scalar=self.g12[:, t, 1:2], in1=y_t[:, :],
                                                 op0=ALU.mult, op1=ALU.add), [b_r, pgr, y_r], [y_r])
            xo_t, xo_r = xo.next()
            self.layer_norm_tile(P, 128, y_t, y_r, g_bc, b_bc, gb_r, xo_t, xo_r, scr)
            S.dma("pool", dst[t * 128:(t + 1) * 128, :], xo_t[:, :], [xo_r], [self.xres_r[2 * t], self.xres_r[2 * t + 1]])
        P.close()

    def dump(self, src_ap, shape, dt):
        nc, S = self.nc, self.S
        dbg = nc.dram_tensor("dbg", list(shape), dt, kind="ExternalOutput").ap()
        S.barrier()
        n = shape[0] // 128
        for i in range(n):
            S.dma("sp", dbg[i * 128:(i + 1) * 128, :], src_ap[i * 128:(i + 1) * 128, :], [], [Reg()])
        S.barrier()

    def init_scratch(self):
        nc, S = self.nc, self.S
        P = Phase(S)
        zb = P.sb([128, 8, D], BF16)
        zf = P.sb([128, D], F32)
        r = Reg()
        S.dve(lambda: nc.vector.memset(zb[:].rearrange("p a b -> p (a b)"), 0.0), [], [r])
        S.dve(lambda: nc.vector.memset(zf[:], 0.0), [], [r])
        for i in range(NS // 1024):
            S.dma("sp", self.xs[i * 1024:(i + 1) * 1024, :].rearrange("(p a) d -> p a d", p=128), zb[:, :, :], [r], [Reg()])
        S.dma("sp", self.xs[NS:NS + 128, :], zb[:, 0, :], [r], [Reg()])
        S.dma("sp", self.yb[NS:NS + 128, :], zf[:, :], [r], [Reg()])
        P.close()

    def build(self):
        dbg = self.debug
        self.init_scratch()
        for l in range(self.nlayers):
            if l % 2 == 0:
                self.phase_attn_proj(l)
                if dbg == "qkt%d" % l:
                    self.dump(self.qkt.rearrange("a b c -> (a b) c"), [2048, T], BF16)
                    return self.nc
                self.phase_attn(l)
                if dbg == "ot%d" % l:
                    self.dump(self.ot, [D, T], BF16)
                    return self.nc
                self.phase_out_ln_router(l, self.ab_w_out[l // 2])
            else:
                self.phase_hgrn(l)
                if dbg == "ot%d" % l:
                    self.dump(self.ot, [D, T], BF16)
                    return self.nc
                self.phase_out_ln_router(l, self.c_w_out[l // 2])
            if dbg == "xa%d" % l:
                self.dump(self.xres, [T, D], F32)
                return self.nc
            self.phase_moe(l, last=(l == self.nlayers - 1))
        self.S.barrier()
        return self.nc


def host_inputs(inputs):
    f = lambda a: np.ascontiguousarray(np.asarray(a, dtype=np.float32))
    d = {}
    d["ab_w_in"] = f(inputs["ab_w_in"])
    d["ab_w_out"] = f(inputs["ab_w_out"])
    d["c_w_in"] = f(inputs["c_w_in"])
    d["c_w_out"] = f(inputs["c_w_out"])
    d["exp_w1"] = f(inputs["exp_w1"])
    d["exp_w3"] = f(inputs["exp_w3"])
    d["exp_w2"] = f(inputs["exp_w2"])
    cng = np.tile(f(inputs["c_norm_g"]), (1, 8))
    d["cng"] = np.ascontiguousarray(np.broadcast_to(cng[:, None, :], (2, 64, D)))
    d["lbl"] = np.ascontiguousarray(np.broadcast_to(f(inputs["hgrn_lb_logits"])[:, None, :], (2, 64, D)))
    d["lng"] = np.ascontiguousarray(np.broadcast_to(f(inputs["ln_g"]).reshape(8, 1, D), (8, 128, D)))
    d["lnb"] = np.ascontiguousarray(np.broadcast_to(f(inputs["ln_b"]).reshape(8, 1, D), (8, 128, D)))
    d["rw"] = np.ascontiguousarray(np.concatenate([f(inputs["router_g_w"]), f(inputs["router_e_w"])], axis=2))
    rb = np.concatenate([f(inputs["router_g_b"]), f(inputs["router_e_b"])], axis=1)
    d["rb"] = np.ascontiguousarray(np.broadcast_to(rb[:, None, :], (4, 128, 36)))
    return d


_CACHE = {}


def kernel(**inputs):
    x = np.ascontiguousarray(np.asarray(inputs["x"], dtype=np.float32))
    if "prog" not in _CACHE:
        p = Prog()
        p.build()
        _CACHE["prog"] = p
    p = _CACHE["prog"]
    shared = host_inputs(inputs)
    for k, v in p.consts_np.items():
        shared["c_" + k] = v
    in_maps = []
    for c in range(8):
        m = dict(shared)
        m["x"] = x[c]
        in_maps.append(m)
    res = run_bass_kernel_spmd(p.nc, in_maps, core_ids=list(range(8)))
    return np.stack([np.asarray(r["out"], dtype=np.float32) for r in res.results], axis=0)
```

```python
import contextlib
import os
LVL = int(os.environ.get('DBG_LVL', '9'))
import numpy as np
import ml_dtypes
import concourse.bass as bass
import concourse.mybir as mybir
from concourse.bass_utils import run_bass_kernel_spmd

F32 = mybir.dt.float32
BF16 = mybir.dt.bfloat16
AF = mybir.ActivationFunctionType
ALU = mybir.AluOpType
AX = mybir.AxisListType

T = 4096
D = 1024
DEPTH = 4
ALPHA = float((2.0 * DEPTH) ** 0.25)
LN_EPS = 1e-5
RMS_EPS = 1e-6
BIG = 30000.0
EPOCH = 30000
NSLOT = 8
CAP = 512
NS = 32 * CAP
I32 = mybir.dt.int32


class Reg:
    __slots__ = ("w", "r", "x")

    def __init__(self, x=False):
        self.w = {}
        self.r = {}
        self.x = x


class Eng:
    def __init__(self, name, e, is_pe=False):
        self.name = name
        self.e = e
        self.count = 0
        self.known = {}
        self.is_pe = is_pe
        self.dcount = 0


class Sched:
    def __init__(self, nc):
        self.nc = nc
        self.E = {
            "pe": Eng("pe", nc.tensor, True),
            "act": Eng("act", nc.scalar),
            "dve": Eng("dve", nc.vector),
            "pool": Eng("pool", nc.gpsimd),
            "sp": Eng("sp", nc.sync),
        }
        self.semh = {}
        self.nsem = 0

    def _sem(self, key):
        h = self.semh.get(key)
        if h is None:
            h = self.nc.alloc_semaphore("s_%s_%s_%d" % key)
            self.semh[key] = h
            self.nsem += 1
        return h

    def _waits(self, E, reads, writes):
        deps = {}
        for r in reads:
            for k, v in r.w.items():
                if deps.get(k, 0) < v:
                    deps[k] = v
            if r.x:
                for k, v in r.r.items():
                    if deps.get(k, 0) < v:
                        deps[k] = v
        for w in writes:
            for k, v in w.w.items():
                if deps.get(k, 0) < v:
                    deps[k] = v
            for k, v in w.r.items():
                if deps.get(k, 0) < v:
                    deps[k] = v
        for k, v in deps.items():
            if E.is_pe and k[0] == "pe" and k[1] == "c":
                continue
            if E.known.get(k, 0) < v:
                E.e.wait_ge(self._sem(k), v)
                E.known[k] = v

    def op(self, en, fn, reads, writes):
        E = self.E[en]
        self._waits(E, reads, writes)
        ins = fn()
        key = (en, "c", E.count // EPOCH)
        val = E.count % EPOCH + 1
        ins.then_inc(self._sem(key), 1)
        E.count += 1
        for r in reads:
            if r.x:
                r.w = {key: val}
                r.r = {}
            else:
                r.r[key] = val
        for w in writes:
            w.w = {key: val}
            w.r = {}

    def pe(self, fn, reads, writes):
        self.op("pe", fn, reads, writes)

    def act(self, fn, reads, writes):
        self.op("act", fn, reads, writes)

    def dve(self, fn, reads, writes):
        self.op("dve", fn, reads, writes)

    def pool(self, fn, reads, writes):
        self.op("pool", fn, reads, writes)

    def dma(self, qn, out, in_, reads, writes):
        Q = self.E[qn]
        self._waits(Q, reads, writes)
        i = Q.dcount
        slot = i % NSLOT
        tgt = 16 * (i // NSLOT + 1)
        assert tgt < 60000
        key = (qn, "d", slot)
        if i >= NSLOT and Q.known.get(key, 0) < tgt - 16:
            Q.e.wait_ge(self._sem(key), tgt - 16)
            Q.known[key] = tgt - 16
        Q.e.dma_start(out=out, in_=in_).then_inc(self._sem(key), 16)
        Q.dcount += 1
        for r in reads:
            if r.r.get(key, 0) < tgt:
                r.r[key] = tgt
        for w in writes:
            w.w = {key: tgt}
            w.r = {}

    def idma(self, out, out_off, in_, in_off, bound, reads, writes):
        Q = self.E["pool"]
        self._waits(Q, reads, writes)
        i = Q.dcount
        slot = i % NSLOT
        tgt = 16 * (i // NSLOT + 1)
        assert tgt < 60000
        key = ("pool", "d", slot)
        if i >= NSLOT and Q.known.get(key, 0) < tgt - 16:
            Q.e.wait_ge(self._sem(key), tgt - 16)
            Q.known[key] = tgt - 16
        Q.e.indirect_dma_start(out=out, out_offset=out_off, in_=in_, in_offset=in_off).then_inc(self._sem(key), 16)
        Q.dcount += 1
        for r in reads:
            if r.r.get(key, 0) < tgt:
                r.r[key] = tgt
        for w in writes:
            w.w = {key: tgt}
            w.r = {}

    def barrier(self):
        latest = {}
        for E in self.E.values():
            if E.count > 0:
                latest[(E.name, "c", (E.count - 1) // EPOCH)] = (E.count - 1) % EPOCH + 1
            for slot in range(min(NSLOT, E.dcount)):
                n = (E.dcount - slot + NSLOT - 1) // NSLOT
                latest[(E.name, "d", slot)] = 16 * n
        for E in self.E.values():
            for k, v in latest.items():
                if E.is_pe and k[0] == "pe" and k[1] == "c":
                    continue
                if E.known.get(k, 0) < v:
                    E.e.wait_ge(self._sem(k), v)
                    E.known[k] = v


class Phase:
    cnt = 0

    def __init__(self, S):
        self.S = S
        self.nc = S.nc
        self.stack = contextlib.ExitStack()

    def sb(self, shape, dt):
        Phase.cnt += 1
        return self.stack.enter_context(self.nc.sbuf_tensor("sb%d" % Phase.cnt, list(shape), dt))

    def ps(self, shape, dt=F32):
        Phase.cnt += 1
        return self.stack.enter_context(self.nc.psum_tensor("ps%d" % Phase.cnt, list(shape), dt))

    def rot_sb(self, n, shape, dt):
        return Rot([(self.sb(shape, dt), Reg()) for _ in range(n)])

    def rot_ps(self, n, shape, dt=F32):
        return Rot([(self.ps(shape, dt), Reg(True)) for _ in range(n)])

    def close(self):
        self.S.barrier()
        self.stack.close()


class Rot:
    def __init__(self, items):
        self.items = items
        self.i = 0

    def next(self):
        it = self.items[self.i % len(self.items)]
        self.i += 1
        return it


def make_consts():
    bf = ml_dtypes.bfloat16
    c = {}
    c["ident_bf"] = np.eye(128, dtype=np.float32).astype(bf)
    c["ident_f"] = np.eye(128, dtype=np.float32)
    half = 8
    inv = (np.float32(500000.0) ** (-np.arange(half, dtype=np.float32) / np.float32(half))).astype(np.float32)
    ang = (np.arange(T, dtype=np.float32)[:, None] * inv[None, :]).astype(np.float32)
    cos = np.cos(ang.astype(np.float64)).astype(np.float32)
    sin = np.sin(ang.astype(np.float64)).astype(np.float32)
    c2 = np.concatenate([cos, cos], axis=1)
    s2 = np.concatenate([-sin, sin], axis=1)
    c2e = np.tile(c2, (1, 8))
    s2e = np.tile(s2, (1, 8))
    c["c2e"] = np.ascontiguousarray(c2e.reshape(32, 128, 128).transpose(1, 0, 2).reshape(128, 4096))
    c["s2e"] = np.ascontiguousarray(s2e.reshape(32, 128, 128).transpose(1, 0, 2).reshape(128, 4096))

    def mult(d):
        m = np.zeros_like(d, dtype=np.float32)
        m += ((d >= 0) & (d <= 128))
        m += ((d >= 0) & (d % 4 == 0) & (d <= 512))
        m += ((d >= 0) & (d % 16 == 0) & (d <= 2048))
        return m

    kl = np.arange(128)[:, None]
    ql = np.arange(512)[None, :]
    dm = np.zeros((128, 20, 512), np.float32)
    for di in range(20):
        delta = -384 + 128 * di
        dm[:, di, :] = mult(delta + ql - kl)
    with np.errstate(divide="ignore"):
        ldm = np.where(dm > 0, 8.0 * np.log(np.maximum(dm, 1e-30).astype(np.float64)), -BIG)
    ldm_hi = ldm.astype(np.float32).astype(bf)
    ldm_lo = (ldm - ldm_hi.astype(np.float64)).astype(np.float32)
    ldm_lo = np.where(dm > 0, ldm_lo, 0.0).astype(bf)
    c["ldm_hi"] = ldm_hi.reshape(128, 20 * 512)
    c["dmm"] = dm[:, 0:8, :].reshape(128, 8 * 512).astype(bf)
    c["dm_lo_need"] = np.array([float(np.any(ldm_lo[:, di, :].astype(np.float32) != 0)) for di in range(20)], np.float32)
    cm = np.zeros((128, 4, 512), np.float32)
    for ci in range(4):
        delta = -384 + 128 * ci
        cm[:, ci, :] = (delta + ql - kl >= 0)
    c["lcm"] = ((cm - 1.0) * BIG).reshape(128, 4 * 512).astype(bf)
    koh = np.zeros((16, T), np.float32)
    for b in range(16):
        koh[b, b * 256:(b + 1) * 256] = 1.0
    c["koh"] = koh.astype(bf)
    tt = np.arange(32)[:, None]
    bb = np.arange(16)[None, :]
    own = tt // 2
    valid = (bb < own).astype(np.float32)
    negv = (valid - 1.0) * BIG
    ownm1 = (bb == own).astype(np.float32) - 1.0
    c["valid"] = np.ascontiguousarray(np.broadcast_to(valid.reshape(1, 512), (128, 512))).astype(np.float32)
    c["negv"] = np.ascontiguousarray(np.broadcast_to(negv.reshape(1, 512), (128, 512))).astype(np.float32)
    c["ownm1"] = np.ascontiguousarray(np.broadcast_to(ownm1.reshape(1, 512), (128, 512))).astype(np.float32)
    j = np.arange(64)[:, None]
    i = np.arange(64)[None, :]
    c["mg"] = ((j <= i).astype(np.float32) - (j <= 31).astype(np.float32)).astype(np.float32)
    ind = np.zeros((64, 3), np.float32)
    ind[:, 0] = (np.arange(64) <= 31)
    ind[:, 1] = 1.0
    ind[:, 2] = (np.arange(64) > 31)
    c["ind"] = ind
    c["cmask8"] = np.ascontiguousarray(np.tile((i >= j).astype(np.float32), (1, 8)))
    c["ones_f"] = np.ones((128, 64), np.float32)
    tp = np.arange(128)[:, None]
    tq = np.arange(128)[None, :]
    c["u_bf"] = (tp < tq).astype(np.float32).astype(bf)
    c["ones_bf"] = np.ones((128, 128), np.float32).astype(bf)
    ecb = np.tile((np.arange(32, dtype=np.float32) * CAP)[None, :], (1, 4))
    c["ecb"] = np.ascontiguousarray(np.broadcast_to(ecb, (128, 128))).astype(np.float32)
    c["pid"] = np.ascontiguousarray(np.broadcast_to((NS + np.arange(128, dtype=np.float32))[:, None], (128, 8)))
    return c


CONST_DT = {"ident_bf": BF16, "ldm_hi": BF16, "dmm": BF16, "lcm": BF16, "koh": BF16, "u_bf": BF16, "ones_bf": BF16}


class Prog:
    def __init__(self, nlayers=DEPTH, debug=None):
        self.nc = nc = bass.Bass("TRN2", target_bir_lowering=False)
        self.S = Sched(nc)
        self.nlayers = nlayers
        self.debug = debug
        self.consts_np = make_consts()
        di = lambda name, shape, dt=F32: nc.dram_tensor(name, list(shape), dt, kind="ExternalInput").ap()
        self.x_in = di("x", [T, D])
        self.ab_w_in = di("ab_w_in", [2, D, 3072])
        self.ab_w_out = di("ab_w_out", [2, D, D])
        self.c_w_in = di("c_w_in", [2, D, 4096])
        self.c_w_out = di("c_w_out", [2, D, D])
        self.cng = di("cng", [2, 64, D])
        self.lbl = di("lbl", [2, 64, D])
        self.lng = di("lng", [8, 128, D])
        self.lnb = di("lnb", [8, 128, D])
        self.rw = di("rw", [4, D, 36])
        self.rb = di("rb", [4, 128, 36])
        self.w1 = di("exp_w1", [4, 32, D, 512])
        self.w3 = di("exp_w3", [4, 32, D, 512])
        self.w2 = di("exp_w2", [4, 32, 512, D])
        self.cd = {}
        self.dm_lo_need = [bool(v) for v in self.consts_np.pop("dm_lo_need")]
        for k, v in self.consts_np.items():
            self.cd[k] = di("c_" + k, v.shape, CONST_DT.get(k, F32))
        self.out = nc.dram_tensor("out", [T, D], F32, kind="ExternalOutput").ap()
        dt_ = lambda name, shape, dt: nc.dram_tensor(name, list(shape), dt).ap()
        self.xres = dt_("xres", [T, D], F32)
        self.xres_r = [Reg() for _ in range(64)]
        self.qkt = dt_("qkt", [4, 512, T], BF16)
        self.qkt_r = Reg()
        self.vd = dt_("vd", [T, D], BF16)
        self.vd_r = Reg()
        self.ot = dt_("ot", [D, T], BF16)
        self.ot_r = Reg()
        self.xt = dt_("xt", [D, T], BF16)
        self.xt_r = Reg()
        self.ident_bf = nc.alloc_sbuf_tensor("ident_bf", [128, 128], BF16)
        self.ident_f = nc.alloc_sbuf_tensor("ident_f", [128, 128], F32)
        self.posg = nc.alloc_sbuf_tensor("posg", [128, 32, 2], I32)
        self.g12 = nc.alloc_sbuf_tensor("g12", [128, 32, 2], F32)
        self.pg_r = [Reg() for _ in range(8)]
        self.xs = dt_("xs", [NS + 128, D], BF16)
        self.xs_r = Reg()
        self.yb = dt_("yb", [NS + 128, D], F32)
        self.yb_r = Reg()
        self.cr = Reg()
        S = self.S
        S.dma("sp", self.ident_bf[:], self.cd["ident_bf"], [], [self.cr])
        r2 = Reg()
        S.dma("sp", self.ident_f[:], self.cd["ident_f"], [], [r2])
        self.cr2 = r2

    def xsrc(self, l):
        return self.x_in if l == 0 else self.xres

    def layer_norm_tile(self, P, np_, y, y_r, g_bc, b_bc, gb_r, out_t, out_r, scr, tail="pool"):
        nc, S = self.nc, self.S
        st, st_r, mv, mv_r, rs, rs_r = scr
        S.dve(lambda: nc.vector.bn_stats(out=st[:np_, 0:6], in_=y[:np_, 0:512]), [y_r], [st_r])
        S.dve(lambda: nc.vector.bn_stats(out=st[:np_, 6:12], in_=y[:np_, 512:1024]), [y_r], [st_r])
        S.dve(lambda: nc.vector.bn_aggr(out=mv[:np_, :], in_=st[:np_, :]), [st_r], [mv_r])
        S.dve(lambda: nc.vector.tensor_scalar(out=rs[:np_, :], in0=mv[:np_, 1:2], scalar1=LN_EPS, scalar2=None,
                                              op0=ALU.add), [mv_r], [rs_r])
        S.act(lambda: nc.scalar.activation(out=rs[:np_, :], in_=rs[:np_, :], func=AF.Ln), [rs_r], [rs_r])
        S.act(lambda: nc.scalar.activation(out=rs[:np_, :], in_=rs[:np_, :], func=AF.Exp, scale=-0.5), [rs_r], [rs_r])
        S.dve(lambda: nc.vector.tensor_scalar(out=out_t[:np_, :], in0=y[:np_, :], scalar1=mv[:np_, 0:1],
                                              scalar2=rs[:np_, 0:1], op0=ALU.subtract, op1=ALU.mult),
              [y_r, mv_r, rs_r], [out_r])
        TE = nc.gpsimd if tail == "pool" else nc.vector
        S.op(tail, lambda: TE.tensor_tensor(out=out_t[:np_, :], in0=out_t[:np_, :], in1=g_bc[:np_, :], op=ALU.mult),
             [out_r, gb_r], [out_r])
        S.op(tail, lambda: TE.tensor_tensor(out=out_t[:np_, :], in0=out_t[:np_, :], in1=b_bc[:np_, :], op=ALU.add),
             [out_r, gb_r], [out_r])

    def phase_attn_proj(self, l):
        nc, S = self.nc, self.S
        j = l // 2
        P = Phase(S)
        W = P.sb([128, 8, 3072], BF16)
        w_r = []
        for k in range(8):
            r = Reg()
            S.dma("pool", W[:, k, :], self.ab_w_in[j, k * 128:(k + 1) * 128, :], [], [r])
            w_r.append(r)
        c2e = P.sb([128, 32, 128], F32)
        s2e = P.sb([128, 32, 128], F32)
        tab_r = Reg()
        tab_r2 = Reg()
        S.dma("sp", c2e[:].rearrange("p a b -> p (a b)"), self.cd["c2e"], [], [tab_r])
        S.dma("sp", s2e[:].rearrange("p a b -> p (a b)"), self.cd["s2e"], [], [tab_r2])
        xs = P.rot_sb(2, [128, D], F32)
        xT = P.rot_sb(2, [128, 8, 128], BF16)
        ps_t = P.rot_ps(2, [128, 512], F32)
        ps_o = P.rot_ps(3, [128, 512], F32)
        ps_tr = P.rot_ps(2, [128, 1024], BF16)
        qs = P.rot_sb(3, [128, 512], BF16)
        t1 = P.rot_sb(2, [128, 8, 16], F32)
        t2 = P.rot_sb(2, [128, 8, 16], F32)
        stg = P.rot_sb(2, [128, 16, 512], BF16)
        vst = P.rot_sb(2, [128, 1024], BF16)
        src = self.xsrc(l)
        for g in range(8):
            stg_t, stg_r = stg.next()
            for tl in range(4):
                t = g * 4 + tl
                x_t, x_r = xs.next()
                S.dma("sp", x_t[:], src[t * 128:(t + 1) * 128, :], [self.xres_r[2 * t], self.xres_r[2 * t + 1]], [x_r])
                xT_t, xT_r = xT.next()
                for hb in range(2):
                    pt, pt_r = ps_t.next()
                    for kk in range(4):
                        k = hb * 4 + kk
                        S.pe(lambda: nc.tensor.transpose(pt[:, kk * 128:(kk + 1) * 128], x_t[:, k * 128:(k + 1) * 128],
                                                         self.ident_f[:]), [x_r, self.cr2], [pt_r])
                    S.act(lambda: nc.scalar.copy(out=xT_t[:, hb * 4:(hb + 1) * 4, :].rearrange("p a b -> p (a b)"),
                                                 in_=pt[:, :]), [pt_r], [xT_r])
                v_t, v_r = vst.next()
                for cg in range(6):
                    if LVL < 2:
                        break
                    po, po_r = ps_o.next()
                    for k in range(8):
                        S.pe(lambda: nc.tensor.matmul(po[:, :], lhsT=xT_t[:, k, :], rhs=W[:, k, cg * 512:(cg + 1) * 512],
                                                      start=(k == 0), stop=(k == 7)), [xT_r, w_r[k]], [po_r])
                    if cg in (2, 5):
                        off = 0 if cg == 2 else 512
                        S.act(lambda: nc.scalar.copy(out=v_t[:, off:off + 512], in_=po[:, :]), [po_r], [v_r])
                        continue
                    cgi = {0: 0, 1: 1, 3: 2, 4: 3}[cg]
                    if LVL < 3:
                        continue
                    q_t, q_r = qs.next()
                    S.act(lambda: nc.scalar.copy(out=q_t[:, :], in_=po[:, :]), [po_r], [q_r])
                    a1, a1_r = t1.next()
                    a2, a2_r = t2.next()
                    pov = po[:, :].rearrange("p (h d) -> p h d", h=8)
                    qv = q_t[:, :].rearrange("p (h d) -> p h d", h=8)
                    cv = c2e[:, t, :].rearrange("p (h d) -> p h d", h=8)
                    sv = s2e[:, t, :].rearrange("p (h d) -> p h d", h=8)
                    S.dve(lambda: nc.vector.tensor_tensor(out=a1[:, :, :], in0=pov[:, :, 0:16], in1=cv, op=ALU.mult),
                          [po_r, tab_r], [a1_r])
                    S.dve(lambda: nc.vector.tensor_tensor(out=a2[:, :, 0:8], in0=pov[:, :, 8:16], in1=sv[:, :, 0:8],
                                                          op=ALU.mult), [po_r, tab_r2], [a2_r])
                    S.dve(lambda: nc.vector.tensor_tensor(out=a2[:, :, 8:16], in0=pov[:, :, 0:8], in1=sv[:, :, 8:16],
                                                          op=ALU.mult), [po_r, tab_r2], [a2_r])
                    S.dve(lambda: nc.vector.tensor_tensor(out=qv[:, :, 0:16], in0=a1[:, :, :], in1=a2[:, :, :], op=ALU.add),
                          [a1_r, a2_r], [q_r])
                    if LVL < 4:
                        continue
                    ptr, ptr_r = ps_tr.next()
                    for pr in range(4):
                        S.pe(lambda: nc.tensor.transpose(ptr[:, pr * 128:(pr + 1) * 128], q_t[:, pr * 128:(pr + 1) * 128],
                                                         self.ident_bf[:]), [q_r, self.cr], [ptr_r])
                    S.dve(lambda: nc.vector.tensor_copy(
                        out=stg_t[:, cgi * 4:(cgi + 1) * 4, tl * 128:(tl + 1) * 128],
                        in_=ptr[:, 0:512].rearrange("p (a b) -> p a b", a=4)), [ptr_r], [stg_r])
                if LVL >= 2:
                    S.dma("act", self.vd[t * 128:(t + 1) * 128, :], v_t[:, :], [v_r], [self.vd_r])
            for cgi in range(4):
                if LVL < 5:
                    break
                S.dma("sp", self.qkt[cgi, :, g * 512:(g + 1) * 512].rearrange("(a p) t -> p a t", p=128),
                      stg_t[:, cgi * 4:(cgi + 1) * 4, :], [stg_r], [self.qkt_r])
        P.close()

    def phase_attn(self, l):
        nc, S = self.nc, self.S
        P = Phase(S)
        dmh = P.sb([128, 20, 512], BF16)
        dmm = P.sb([128, 8, 512], BF16)
        cm = P.sb([128, 4, 512], BF16)
        S.dma("sp", dmh[:].rearrange("p a b -> p (a b)"), self.cd["ldm_hi"], [], [Reg()])
        S.dma("sp", dmm[:].rearrange("p a b -> p (a b)"), self.cd["dmm"], [], [Reg()])
        S.dma("sp", cm[:].rearrange("p a b -> p (a b)"), self.cd["lcm"], [], [Reg()])
        valid = P.sb([128, 512], F32)
        negv = P.sb([128, 512], F32)
        ownm1 = P.sb([128, 512], F32)
        ones_f = P.sb([128, 64], F32)
        k_r = Reg()
        for tl_, nm in ((valid, "valid"), (negv, "negv"), (ownm1, "ownm1"), (ones_f, "ones_f")):
            r = Reg()
            S.dma("sp", tl_[:], self.cd[nm], [], [r])
            k_r = r
        cst_r = Reg()
        QT = [P.sb([128, T], BF16) for _ in range(2)]
        KT = [P.sb([128, T], BF16) for _ in range(2)]
        VA = [P.sb([128, 32, 128], BF16) for _ in range(2)]
        QT_r = [Reg(), Reg()]
        QTb_r = [Reg(), Reg()]
        KT_r = [Reg(), Reg()]
        KTc_r = [Reg(), Reg()]
        VA_r = [[Reg() for _ in range(4)] for _ in range(2)]
        VAo_r = [Reg(), Reg()]
        for b in range(2):
            S.pool(lambda: nc.gpsimd.memset(QT[b][0:64, :], 0.0), [], [QTb_r[b]])
            S.pool(lambda: nc.gpsimd.memset(KT[b][0:64, :], 0.0), [], [KTc_r[b]])
            S.dma("sp", KT[b][0:16, :], self.cd["koh"], [KTc_r[b]], [KTc_r[b]])
            S.pool(lambda: nc.gpsimd.memset(VA[b][:, :, 64:128], 1.0), [], [VAo_r[b]])
        S.barrier()
        ps_s = P.rot_ps(4, [128, 512], F32)
        ps_acc = P.rot_ps(2, [128, 512], F32)
        ps_g = P.ps([128, 512], F32)
        ps_g_r = Reg(True)
        ps_b = P.rot_ps(1, [128, 1024], BF16)
        pb = P.rot_sb(6, [128, 512], BF16)
        km = P.sb([128, 16], F32)
        kmh = P.sb([128, 16], BF16)
        kml = P.sb([128, 16], BF16)
        kmt = P.sb([128, 16], F32)
        km_r = Reg()
        gm = P.sb([128, 32, 16], F32)
        gm_r = Reg()
        m8 = P.sb([128, 32, 8], F32)
        m8_r = Reg()
        sel = P.sb([128, 32, 16], F32)
        sel_r = Reg()
        bia = P.sb([128, 32, 16], BF16)
        bia_r = Reg()
        den = P.rot_sb(2, [128, 512], F32)
        bcs = P.rot_sb(2, [64, 512], F32)
        ost = P.rot_sb(2, [64, 512], BF16)
        def prep_steps(h):
            b = h % 2
            moba = h < 8
            qc, kc = (0, 1) if moba else (2, 3)
            hh = h % 8
            steps = []

            def loads():
                S.dma("sp", QT[b][64:128, :], self.qkt[qc, hh * 64:(hh + 1) * 64, :], [self.qkt_r], [QT_r[b]])
                S.dma("sp", KT[b][64:128, :], self.qkt[kc, hh * 64:(hh + 1) * 64, :], [self.qkt_r], [KT_r[b]])
                for q4 in range(4):
                    S.dma("sp", VA[b][:, q4 * 8:(q4 + 1) * 8, 0:64],
                          self.vd[q4 * 1024:(q4 + 1) * 1024, h * 64:(h + 1) * 64].rearrange("(t p) c -> p t c", p=128),
                          [self.vd_r], [VA_r[b][q4]])
            steps.append(loads)
            if not moba:
                if h in (8, 9):
                    steps.append(lambda: S.pool(lambda: nc.gpsimd.memset(QT[b][0:16, :], 0.0), [], [QTb_r[b]]))
                return steps

            def gate1():
                S.dve(lambda: nc.vector.tensor_reduce(out=km[64:128, :],
                                                      in_=KT[b][64:128, :].rearrange("p (a c) -> p a c", a=16),
                                                      axis=AX.X, op=ALU.add), [KT_r[b]], [km_r])
                S.dve(lambda: nc.vector.tensor_scalar(out=km[64:128, :], in0=km[64:128, :], scalar1=1.0 / 256.0, scalar2=None,
                                                      op0=ALU.mult), [km_r], [km_r])
                S.dve(lambda: nc.vector.tensor_copy(out=kmh[64:128, :], in_=km[64:128, :]), [km_r], [km_r])
                S.dve(lambda: nc.vector.tensor_tensor(out=kmt[64:128, :], in0=km[64:128, :], in1=kmh[64:128, :],
                                                      op=ALU.subtract), [km_r], [km_r])
                S.dve(lambda: nc.vector.tensor_copy(out=kml[64:128, :], in_=kmt[64:128, :]), [km_r], [km_r])
                for t in range(32):
                    S.pe(lambda: nc.tensor.matmul(ps_g[:, t * 16:(t + 1) * 16], lhsT=QT[b][64:128, t * 128:(t + 1) * 128],
                                                  rhs=kmh[64:128, :], start=True, stop=False), [QT_r[b], km_r], [ps_g_r])
                    S.pe(lambda: nc.tensor.matmul(ps_g[:, t * 16:(t + 1) * 16], lhsT=QT[b][64:128, t * 128:(t + 1) * 128],
                                                  rhs=kml[64:128, :], start=False, stop=True), [QT_r[b], km_r], [ps_g_r])
                gmf = gm[:].rearrange("p a b -> p (a b)")
                S.dve(lambda: nc.vector.tensor_tensor(out=gmf, in0=ps_g[:, :], in1=negv[:], op=ALU.add), [ps_g_r], [gm_r])
            steps.append(gate1)

            def gate2():
                for t in range(32):
                    S.dve(lambda: nc.vector.max(out=m8[:, t, :], in_=gm[:, t, :]), [gm_r], [m8_r])
            steps.append(gate2)

            def gate3():
                S.dve(lambda: nc.vector.tensor_tensor(out=sel[:, :, :], in0=gm[:, :, :],
                                                      in1=m8[:, :, 2:3].to_broadcast([128, 32, 16]), op=ALU.is_ge),
                      [gm_r, m8_r], [sel_r])
                self_f = sel[:].rearrange("p a b -> p (a b)")
                S.dve(lambda: nc.vector.tensor_tensor(out=self_f, in0=self_f, in1=valid[:], op=ALU.mult), [sel_r], [sel_r])
                S.dve(lambda: nc.vector.tensor_tensor(out=self_f, in0=self_f, in1=ownm1[:], op=ALU.add), [sel_r], [sel_r])
                S.dve(lambda: nc.vector.tensor_scalar(out=bia[:].rearrange("p a b -> p (a b)"), in0=self_f, scalar1=BIG,
                                                      scalar2=None, op0=ALU.mult), [sel_r], [bia_r])
            steps.append(gate3)

            def mk_tr(g4):
                def tr():
                    pbt, pbt_r = ps_b.next()
                    for tt in range(8):
                        t = g4 * 8 + tt
                        S.pe(lambda: nc.tensor.transpose(pbt[0:16, tt * 128:(tt + 1) * 128], bia[:, t, :], self.ident_bf[:]),
                             [bia_r], [pbt_r])
                    S.act(lambda: nc.scalar.copy(out=QT[b][0:16, g4 * 1024:(g4 + 1) * 1024], in_=pbt[0:16, :]),
                          [pbt_r], [QTb_r[b]])
                return tr
            for g4 in range(4):
                steps.append(mk_tr(g4))
            return steps

        def attend(h, steps):
            b = h % 2
            moba = h < 8
            lo = 0
            blocks = []
            for Q in range(8):
                ms = list(range(0, 4 * Q + 4)) if moba else list(range(max(0, 4 * Q - 16), 4 * Q + 4))
                for mi, m in enumerate(ms):
                    blocks.append((Q, mi, m, len(ms)))
            n = len(blocks)
            every = max(1, n // (len(steps) + 1)) if steps else n + 1
            sq = {}
            LOOK = 3

            def issue_S(i):
                Q, mi, m, nm = blocks[i]
                di_ = (512 * Q - 128 * m + 384) // 128
                extra = []
                if moba:
                    if di_ <= 3:
                        extra.append(cm[:, di_, :])
                elif not self.dm_lo_need[di_]:
                    extra.append(dmh[:, di_, :])
                sp_, sp_r = ps_s.next()
                S.pe(lambda: nc.tensor.matmul(sp_[:, :], lhsT=KT[b][lo:128, m * 128:(m + 1) * 128],
                                              rhs=QT[b][lo:128, Q * 512:(Q + 1) * 512], start=True, stop=(not extra)),
                     [KT_r[b], KTc_r[b], QT_r[b], QTb_r[b]], [sp_r])
                for xi, xm in enumerate(extra):
                    S.pe(lambda: nc.tensor.matmul(sp_[:, :], lhsT=self.ident_bf[:, :], rhs=xm, start=False,
                                                  stop=(xi == len(extra) - 1)), [], [sp_r])
                sq[i] = (sp_, sp_r)

            fin = []

            def fin_pe(Q, acc, acc_r, dn, dn_r):
                bp, bp_r = ps_s.next()
                S.pe(lambda: nc.tensor.matmul(bp[0:64, :], lhsT=ones_f[64:65, 0:64], rhs=dn[64:65, :], start=True, stop=True),
                     [dn_r], [bp_r])
                bc, bc_r = bcs.next()
                S.act(lambda: nc.scalar.copy(out=bc[:, :], in_=bp[0:64, :]), [bp_r], [bc_r])
                o_t, o_r = ost.next()
                S.dve(lambda: nc.vector.tensor_tensor(out=o_t[:, :], in0=acc[0:64, :], in1=bc[:, :], op=ALU.mult),
                      [acc_r, bc_r], [o_r])
                S.dma("sp", self.ot[h * 64:(h + 1) * 64, Q * 512:(Q + 1) * 512], o_t[:, :], [o_r], [self.ot_r])

            for i in range(min(LOOK, n)):
                issue_S(i)
            acc = acc_r = None
            for i in range(n):
                Q, mi, m, nm = blocks[i]
                di_ = (512 * Q - 128 * m + 384) // 128
                sp_, sp_r = sq.pop(i)
                p_t, p_r = pb.next()
                S.act(lambda: nc.scalar.activation(out=p_t[:, :], in_=sp_[:, :], func=AF.Exp, scale=0.125), [sp_r], [p_r])
                if (not moba) and self.dm_lo_need[di_]:
                    S.dve(lambda: nc.vector.tensor_tensor(out=p_t[:, :], in0=p_t[:, :], in1=dmm[:, di_, :], op=ALU.mult),
                          [p_r], [p_r])
                if i + LOOK < n:
                    issue_S(i + LOOK)
                if mi == 0:
                    acc, acc_r = ps_acc.next()
                S.pe(lambda: nc.tensor.matmul(acc[:, :], lhsT=VA[b][:, m, :], rhs=p_t[:, :],
                                              start=(mi == 0), stop=(mi == nm - 1)), [p_r, VA_r[b][m // 8], VAo_r[b]], [acc_r])
                for f in fin:
                    f[0] -= 1
                while fin and fin[0][0] <= 0:
                    f = fin.pop(0)
                    fin_pe(*f[1:])
                if mi == nm - 1:
                    dn, dn_r = den.next()
                    S.act(lambda: nc.scalar.copy(out=dn[64:65, :], in_=acc[64:65, :]), [acc_r], [dn_r])
                    S.dve(lambda: nc.vector.reciprocal(out=dn[64:65, :], in_=dn[64:65, :]), [dn_r], [dn_r])
                    fin.append([2, Q, acc, acc_r, dn, dn_r])
                if steps and (i + 1) % every == 0:
                    steps.pop(0)()
            while fin:
                f = fin.pop(0)
                fin_pe(*f[1:])
            while steps:
                steps.pop(0)()

        for st_ in prep_steps(0):
            st_()
        for h in range(16):
            nxt = prep_steps(h + 1) if h + 1 < 16 else []
            attend(h, nxt)
        P.close()


    def phase_hgrn(self, l):
        nc, S = self.nc, self.S
        j = l // 2
        P = Phase(S)
        V = nc.vector
        G_ = nc.gpsimd
        W = P.sb([128, 8, 4096], BF16)
        w_r = []
        for k in range(8):
            r = Reg()
            S.dma("pool", W[:, k, :], self.c_w_in[j, k * 128:(k + 1) * 128, :], [], [r])
            w_r.append(r)
        mg = P.sb([64, 64], F32)
        ind = P.sb([64, 3], F32)
        cmask8 = P.sb([64, 512], F32)
        cng = P.sb([64, D], F32)
        lb = P.sb([64, D], F32)
        oml = P.sb([64, D], F32)
        l0 = P.sb([64, D], F32)
        cr = Reg()
        for tl_, ap_ in ((mg, self.cd["mg"]), (ind, self.cd["ind"]), (cmask8, self.cd["cmask8"]), (cng, self.cng[j]),
                         (lb, self.lbl[1]), (l0, self.lbl[0])):
            S.dma("sp", tl_[:], ap_, [], [Reg()])
        S.barrier()
        if j == 0:
            S.dve(lambda: V.memset(lb[:], 0.0), [], [cr])
            S.dve(lambda: V.memset(oml[:], 1.0), [], [cr])
        else:
            S.dve(lambda: V.tensor_tensor(out=lb[:], in0=lb[:], in1=l0[:], op=ALU.subtract), [], [cr])
            S.act(lambda: nc.scalar.activation(out=lb[:], in_=lb[:], func=AF.Sigmoid), [cr], [cr])
            S.dve(lambda: V.tensor_scalar(out=oml[:], in0=lb[:], scalar1=-1.0, scalar2=1.0, op0=ALU.mult, op1=ALU.add),
                  [cr], [cr])
        Sst = P.sb([128, 8, 128], F32)
        Sst_r = Reg()
        S.dve(lambda: V.memset(Sst[:].rearrange("p a b -> p (a b)"), 0.0), [], [Sst_r])
        S.barrier()
        banks = P.rot_ps(6, [128, 512], F32)
        pbf = P.rot_ps(2, [128, 1024], BF16)
        xs = P.rot_sb(2, [64, D], F32)
        xT = P.rot_sb(2, [128, 8, 64], BF16)
        fbuf = P.rot_sb(1, [64, D], F32)
        lfb = P.rot_sb(1, [64, D], F32)
        kkb = P.rot_sb(1, [64, D], F32)
        eGb = P.rot_sb(1, [64, D], F32)
        enGb = P.rot_sb(1, [64, D], F32)
        qtb = P.rot_sb(2, [64, D], BF16)
        ktb = P.rot_sb(2, [64, D], BF16)
        vb = P.rot_sb(2, [64, D], BF16)
        sgb = P.rot_sb(2, [64, D], F32)
        abcb = P.rot_sb(2, [128, 8, 3], F32)
        qTb = P.rot_sb(2, [128, 8, 64], BF16)
        kTb = P.rot_sb(2, [128, 8, 64], BF16)
        ATb = P.rot_sb(2, [64, 512], BF16)
        smb = P.rot_sb(2, [128, 8, 128], BF16)
        tmpb = P.rot_sb(1, [128, 8, 128], F32)
        sqb = P.rot_sb(1, [64, D], F32)
        msb = P.rot_sb(2, [64, 16], F32)
        onb = P.rot_sb(1, [64, D], F32)
        ogb = P.rot_sb(2, [64, D], BF16)
        stg = P.rot_sb(2, [128, 8, 512], BF16)
        src = self.xsrc(l)
        idf = self.ident_f
        idb = self.ident_bf
        for g in range(8):
            stg_t, stg_r = stg.next()
            for tl in range(8):
                t = g * 8 + tl
                x_t, x_r = xs.next()
                S.dma("sp", x_t[:], src[t * 64:(t + 1) * 64, :], [self.xres_r[t]], [x_r])
                pt, pt_r = banks.next()
                for k in range(8):
                    S.pe(lambda: nc.tensor.transpose(pt[:, k * 64:(k + 1) * 64], x_t[:, k * 128:(k + 1) * 128], idf[0:64, 0:64]),
                         [x_r], [pt_r])
                xT_t, xT_r = xT.next()
                S.act(lambda: nc.scalar.copy(out=xT_t[:].rearrange("p a b -> p (a b)"), in_=pt[:, :]), [pt_r], [xT_r])

                def proj(cg):
                    po, po_r = banks.next()
                    for k in range(8):
                        S.pe(lambda: nc.tensor.matmul(po[0:64, :], lhsT=xT_t[:, k, :], rhs=W[:, k, cg * 512:(cg + 1) * 512],
                                                      start=(k == 0), stop=(k == 7)), [xT_r, w_r[k]], [po_r])
                    return po, po_r

                f_t, f_r = fbuf.next()
                for hf in range(2):
                    po, po_r = proj(2 + hf)
                    S.act(lambda: nc.scalar.activation(out=f_t[:, hf * 512:(hf + 1) * 512], in_=po[0:64, :], func=AF.Sigmoid),
                          [po_r], [f_r])
                S.dve(lambda: V.tensor_tensor(out=f_t[:], in0=f_t[:], in1=oml[:], op=ALU.mult), [f_r, cr], [f_r])
                S.dve(lambda: V.tensor_tensor(out=f_t[:], in0=f_t[:], in1=lb[:], op=ALU.add), [f_r, cr], [f_r])
                lf_t, lf_r = lfb.next()
                S.act(lambda: nc.scalar.activation(out=lf_t[:], in_=f_t[:], func=AF.Ln), [f_r], [lf_r])
                kk_t, kk_r = kkb.next()
                S.pool(lambda: G_.tensor_scalar(out=kk_t[:], in0=f_t[:], scalar1=-1.0, scalar2=1.0, op0=ALU.mult, op1=ALU.add),
                       [f_r], [kk_r])
                eG_t, eG_r = eGb.next()
                enG_t, enG_r = enGb.next()
                for hf in range(2):
                    pg_, pg_r = banks.next()
                    S.pe(lambda: nc.tensor.matmul(pg_[0:64, :], lhsT=mg[:, :], rhs=lf_t[:, hf * 512:(hf + 1) * 512],
                                                  start=True, stop=True), [lf_r], [pg_r])
                    S.act(lambda: nc.scalar.activation(out=eG_t[:, hf * 512:(hf + 1) * 512], in_=pg_[0:64, :], func=AF.Exp),
                          [pg_r], [eG_r])
                    S.act(lambda: nc.scalar.activation(out=enG_t[:, hf * 512:(hf + 1) * 512], in_=pg_[0:64, :], func=AF.Exp,
                                                       scale=-1.0), [pg_r], [enG_r])
                pst, pst_r = banks.next()
                for h in range(8):
                    S.pe(lambda: nc.tensor.matmul(pst[:, h * 3:(h + 1) * 3], lhsT=lf_t[:, h * 128:(h + 1) * 128], rhs=ind[:, :],
                                                  start=True, stop=True), [lf_r], [pst_r])
                abc, abc_r = abcb.next()
                S.act(lambda: nc.scalar.activation(out=abc[:].rearrange("p a b -> p (a b)"), in_=pst[:, 0:24], func=AF.Exp),
                      [pst_r], [abc_r])
                qt_t, qt_r = qtb.next()
                for hf in range(2):
                    po, po_r = proj(hf)
                    S.dve(lambda: V.tensor_tensor(out=qt_t[:, hf * 512:(hf + 1) * 512], in0=po[0:64, :],
                                                  in1=eG_t[:, hf * 512:(hf + 1) * 512], op=ALU.mult), [po_r, eG_r], [qt_r])
                kt_t, kt_r = ktb.next()
                S.pool(lambda: G_.tensor_tensor(out=kt_t[:], in0=kk_t[:], in1=enG_t[:], op=ALU.mult), [kk_r, enG_r], [kt_r])
                v_t, v_r = vb.next()
                for hf in range(2):
                    po, po_r = proj(4 + hf)
                    S.act(lambda: nc.scalar.copy(out=v_t[:, hf * 512:(hf + 1) * 512], in_=po[0:64, :]), [po_r], [v_r])
                sg_t, sg_r = sgb.next()
                for hf in range(2):
                    po, po_r = proj(6 + hf)
                    S.act(lambda: nc.scalar.activation(out=sg_t[:, hf * 512:(hf + 1) * 512], in_=po[0:64, :], func=AF.Silu),
                          [po_r], [sg_r])
                qT_t, qT_r = qTb.next()
                kT_t, kT_r = kTb.next()
                for (src_t, src_r, dst_t, dst_r) in ((qt_t, qt_r, qT_t, qT_r), (kt_t, kt_r, kT_t, kT_r)):
                    pb_, pb_r = pbf.next()
                    for h in range(8):
                        S.pe(lambda: nc.tensor.transpose(pb_[:, h * 64:(h + 1) * 64], src_t[:, h * 128:(h + 1) * 128],
                                                         idb[0:64, 0:64]), [src_r], [pb_r])
                    S.dve(lambda: V.tensor_copy(out=dst_t[:].rearrange("p a b -> p (a b)"), in_=pb_[:, 0:512]), [pb_r], [dst_r])
                pat, pat_r = banks.next()
                for h in range(8):
                    S.pe(lambda: nc.tensor.matmul(pat[0:64, h * 64:(h + 1) * 64], lhsT=kT_t[:, h, :], rhs=qT_t[:, h, :],
                                                  start=True, stop=True), [kT_r, qT_r], [pat_r])
                AT_t, AT_r = ATb.next()
                S.dve(lambda: V.tensor_tensor(out=AT_t[:, :], in0=pat[0:64, :], in1=cmask8[:, :], op=ALU.mult), [pat_r], [AT_r])
                sm_t, sm_r = smb.next()
                S.pool(lambda: G_.tensor_tensor(out=sm_t[:, :, :], in0=Sst[:, :, :],
                                                in1=abc[:, :, 0:1].to_broadcast([128, 8, 128]), op=ALU.mult),
                       [Sst_r, abc_r], [sm_r])
                po0, po0_r = banks.next()
                po1, po1_r = banks.next()
                for h in range(8):
                    ob, ob_r = (po0, po0_r) if h < 4 else (po1, po1_r)
                    c0 = (h % 4) * 128
                    S.pe(lambda: nc.tensor.matmul(ob[0:64, c0:c0 + 128], lhsT=qT_t[:, h, :], rhs=sm_t[:, h, :],
                                                  start=True, stop=False), [qT_r, sm_r], [ob_r])
                    S.pe(lambda: nc.tensor.matmul(ob[0:64, c0:c0 + 128], lhsT=AT_t[:, h * 64:(h + 1) * 64],
                                                  rhs=v_t[:, h * 128:(h + 1) * 128], start=False, stop=True),
                         [AT_r, v_r], [ob_r])
                pk0, pk0_r = banks.next()
                pk1, pk1_r = banks.next()
                for h in range(8):
                    kb, kb_r = (pk0, pk0_r) if h < 4 else (pk1, pk1_r)
                    c0 = (h % 4) * 128
                    S.pe(lambda: nc.tensor.matmul(kb[:, c0:c0 + 128], lhsT=kt_t[:, h * 128:(h + 1) * 128],
                                                  rhs=v_t[:, h * 128:(h + 1) * 128], start=True, stop=True),
                         [kt_r, v_r], [kb_r])
                tmp_t, tmp_r = tmpb.next()
                for hb, (kb, kb_r) in enumerate(((pk0, pk0_r), (pk1, pk1_r))):
                    S.dve(lambda: V.tensor_tensor(out=tmp_t[:, hb * 4:(hb + 1) * 4, :],
                                                  in0=kb[:, :].rearrange("p (a b) -> p a b", a=4),
                                                  in1=abc[:, hb * 4:(hb + 1) * 4, 2:3].to_broadcast([128, 4, 128]), op=ALU.mult),
                          [kb_r, abc_r], [tmp_r])
                S.pool(lambda: G_.tensor_tensor(out=Sst[:, :, :], in0=Sst[:, :, :],
                                                in1=abc[:, :, 1:2].to_broadcast([128, 8, 128]), op=ALU.mult),
                       [Sst_r, abc_r], [Sst_r])
                S.pool(lambda: G_.tensor_tensor(out=Sst[:, :, :], in0=Sst[:, :, :], in1=tmp_t[:, :, :], op=ALU.add),
                       [Sst_r, tmp_r], [Sst_r])
                sq_t, sq_r = sqb.next()
                for hb, (ob, ob_r) in enumerate(((po0, po0_r), (po1, po1_r))):
                    S.act(lambda: nc.scalar.activation(out=sq_t[:, hb * 512:(hb + 1) * 512], in_=ob[0:64, :], func=AF.Square),
                          [ob_r], [sq_r])
                ms_t, ms_r = msb.next()
                S.dve(lambda: V.tensor_reduce(out=ms_t[:, 0:8], in_=sq_t[:].rearrange("p (a b) -> p a b", a=8), axis=AX.X,
                                              op=ALU.add), [sq_r], [ms_r])
                S.dve(lambda: V.tensor_scalar(out=ms_t[:, 0:8], in0=ms_t[:, 0:8], scalar1=1.0 / 128.0, scalar2=RMS_EPS,
                                              op0=ALU.mult, op1=ALU.add), [ms_r], [ms_r])
                S.act(lambda: nc.scalar.activation(out=ms_t[:, 0:8], in_=ms_t[:, 0:8], func=AF.Ln), [ms_r], [ms_r])
                S.act(lambda: nc.scalar.activation(out=ms_t[:, 8:16], in_=ms_t[:, 0:8], func=AF.Exp, scale=-0.5), [ms_r], [ms_r])
                on_t, on_r = onb.next()
                for hb, (ob, ob_r) in enumerate(((po0, po0_r), (po1, po1_r))):
                    S.dve(lambda: V.tensor_tensor(out=on_t[:, hb * 512:(hb + 1) * 512].rearrange("p (a b) -> p a b", a=4),
                                                  in0=ob[0:64, :].rearrange("p (a b) -> p a b", a=4),
                                                  in1=ms_t[:, 8 + hb * 4:8 + (hb + 1) * 4].unsqueeze(2).to_broadcast([64, 4, 128]),
                                                  op=ALU.mult), [ob_r, ms_r], [on_r])
                S.pool(lambda: G_.tensor_tensor(out=on_t[:], in0=on_t[:], in1=cng[:], op=ALU.mult), [on_r], [on_r])
                og_t, og_r = ogb.next()
                S.pool(lambda: G_.tensor_tensor(out=og_t[:], in0=on_t[:], in1=sg_t[:], op=ALU.mult), [on_r, sg_r], [og_r])
                pb_, pb_r = pbf.next()
                for h in range(8):
                    S.pe(lambda: nc.tensor.transpose(pb_[:, h * 64:(h + 1) * 64], og_t[:, h * 128:(h + 1) * 128], idb[0:64, 0:64]),
                         [og_r], [pb_r])
                S.act(lambda: nc.scalar.copy(out=stg_t[:, :, tl * 64:(tl + 1) * 64],
                                             in_=pb_[:, 0:512].rearrange("p (a b) -> p a b", a=8)), [pb_r], [stg_r])
            for k in range(8):
                S.dma("act", self.ot[k * 128:(k + 1) * 128, g * 512:(g + 1) * 512], stg_t[:, k, :], [stg_r], [self.ot_r])
        P.close()

    def phase_out_ln_router(self, l, w_out_ap):
        nc, S = self.nc, self.S
        V = nc.vector
        P = Phase(S)
        W = P.sb([128, 8, D], BF16)
        w_r = []
        for k in range(8):
            r = Reg()
            S.dma("pool", W[:, k, :], w_out_ap[k * 128:(k + 1) * 128, :], [], [r])
            w_r.append(r)
        g_bc = P.sb([128, D], F32)
        b_bc = P.sb([128, D], F32)
        rw = P.sb([128, 8, 36], F32)
        rb = P.sb([128, 36], F32)
        u_bf = P.sb([128, 128], BF16)
        ones_bf = P.sb([128, 128], BF16)
        ecb = P.sb([128, 128], F32)
        carry = P.sb([128, 32], F32)
        pid = P.sb([128, 8], F32)
        S.dma("sp", pid[:], self.cd["pid"], [], [Reg()])
        for tl_, ap_ in ((g_bc, self.lng[l * 2]), (b_bc, self.lnb[l * 2]), (rw, self.rw[l].rearrange("(k p) n -> p k n", p=128)),
                         (rb, self.rb[l]), (u_bf, self.cd["u_bf"]), (ones_bf, self.cd["ones_bf"]), (ecb, self.cd["ecb"])):
            S.dma("sp", tl_[:], ap_, [], [Reg()])
        car_r = Reg()
        S.dve(lambda: V.memset(carry[:], 0.0), [], [car_r])
        S.barrier()
        gb_r = Reg()
        oT = Rot([(P.sb([128, 8, 512], BF16), [Reg() for _ in range(8)]) for _ in range(2)])
        xs = P.rot_sb(2, [128, D], F32)
        ys = P.rot_sb(2, [128, D], F32)
        xa = P.rot_sb(2, [128, D], F32)
        xbf = P.rot_sb(8, [128, D], BF16)
        ps_m = P.rot_ps(2, [128, 1024], F32)
        ps_t = P.rot_ps(1, [128, 1024], F32)
        ps_r = P.rot_ps(2, [128, 512], F32)
        xTf = P.rot_sb(2, [128, 8, 128], F32)
        st = P.sb([128, 12], F32)
        mv = P.sb([128, 2], F32)
        rs = P.sb([128, 1], F32)
        scr = (st, Reg(), mv, Reg(), rs, Reg())
        lgs = P.rot_sb(2, [128, 4, 36], F32)
        sm = P.sb([128, 16, 4], F32)
        oh = P.sb([128, 4, 4], F32)
        eg = P.sb([128, 4, 4], F32)
        lem = P.sb([128, 4, 32], F32)
        o1 = P.sb([128, 4, 32], F32)
        o2 = P.sb([128, 4, 32], F32)
        mbf = P.sb([128, 4, 32], BF16)
        rf = P.sb([128, 4, 32], F32)
        tmp = P.sb([128, 4, 32], F32)
        rk = P.sb([128, 4, 2], F32)
        bs = P.sb([128, 4, 2], F32)
        ov = P.sb([128, 4, 2], F32)
        ps_ = P.sb([128, 4, 2], F32)
        pd = P.sb([128, 4, 2], F32)
        possi = P.rot_sb(2, [128, 4, 2], I32)
        rr = Reg()
        src = self.xsrc(l)
        f3 = lambda t: t[:].rearrange("p a b -> p (a b)")
        for g in range(8):
            oT_t, o_regs = oT.next()
            for k in range(8):
                S.dma("sp", oT_t[:, k, :], self.ot[k * 128:(k + 1) * 128, g * 512:(g + 1) * 512], [self.ot_r], [o_regs[k]])
            lg, lg_r = lgs.next()
            xb_list = []
            for tl in range(4):
                t = g * 4 + tl
                x_t, x_r = xs.next()
                S.dma("sp", x_t[:], src[t * 128:(t + 1) * 128, :], [self.xres_r[2 * t], self.xres_r[2 * t + 1]], [x_r])
                pm, pm_r = ps_m.next()
                for hf in range(2):
                    for k in range(8):
                        S.pe(lambda: nc.tensor.matmul(pm[:, hf * 512:(hf + 1) * 512], lhsT=oT_t[:, k, tl * 128:(tl + 1) * 128],
                                                      rhs=W[:, k, hf * 512:(hf + 1) * 512], start=(k == 0), stop=(k == 7)),
                             [o_regs[k], w_r[k]], [pm_r])
                y_t, y_r = ys.next()
                for hf in range(2):
                    S.dve(lambda: V.scalar_tensor_tensor(out=y_t[:, hf * 512:(hf + 1) * 512],
                                                         in0=x_t[:, hf * 512:(hf + 1) * 512], scalar=ALPHA,
                                                         in1=pm[:, hf * 512:(hf + 1) * 512], op0=ALU.mult, op1=ALU.add),
                          [x_r, pm_r], [y_r])
                xa_t, xa_r = xa.next()
                self.layer_norm_tile(P, 128, y_t, y_r, g_bc, b_bc, gb_r, xa_t, xa_r, scr)
                S.dma("pool", self.xres[t * 128:(t + 1) * 128, :], xa_t[:, :], [xa_r],
                      [self.xres_r[2 * t], self.xres_r[2 * t + 1]])
                xb_t, xb_r = xbf.next()
                S.act(lambda: nc.scalar.copy(out=xb_t[:, :], in_=xa_t[:, :]), [xa_r], [xb_r])
                xb_list.append((xb_t, xb_r))
                pt, pt_r = ps_t.next()
                for k in range(8):
                    S.pe(lambda: nc.tensor.transpose(pt[:, k * 128:(k + 1) * 128], xa_t[:, k * 128:(k + 1) * 128],
                                                     self.ident_f[:]), [xa_r], [pt_r])
                xf, xf_r = xTf.next()
                S.act(lambda: nc.scalar.copy(out=xf[:, 0:4, :], in_=pt[:, 0:512].rearrange("p (a b) -> p a b", a=4)),
                      [pt_r], [xf_r])
                S.dve(lambda: V.tensor_copy(out=xf[:, 4:8, :], in_=pt[:, 512:1024].rearrange("p (a b) -> p a b", a=4)),
                      [pt_r], [xf_r])
                pr, pr_r = ps_r.next()
                for k in range(8):
                    S.pe(lambda: nc.tensor.matmul(pr[:, 0:36], lhsT=xf[:, k, :], rhs=rw[:, k, :], start=(k == 0), stop=(k == 7)),
                         [xf_r], [pr_r])
                S.dve(lambda: V.tensor_tensor(out=lg[:, tl, :], in0=pr[:, 0:36], in1=rb[:, :], op=ALU.add), [pr_r], [lg_r])
            R_ = [lg_r, rr]
            LG = lg[:, :, 0:4]
            LE = lg[:, :, 4:36]
            bc4 = lambda j: sm[:, j, :].unsqueeze(2).to_broadcast([128, 4, 4])
            bc32 = lambda j: sm[:, j, :].unsqueeze(2).to_broadcast([128, 4, 32])
            S.dve(lambda: V.tensor_reduce(out=sm[:, 0, :], in_=LG, axis=AX.X, op=ALU.max), R_, [rr])
            S.dve(lambda: V.tensor_tensor(out=oh[:, :, :], in0=LG, in1=bc4(0), op=ALU.is_ge), R_, [rr])
            S.dve(lambda: V.tensor_tensor(out=eg[:, :, :], in0=LG, in1=bc4(0), op=ALU.subtract), R_, [rr])
            S.act(lambda: nc.scalar.activation(out=f3(eg), in_=f3(eg), func=AF.Exp), [rr], [rr])
            S.dve(lambda: V.tensor_reduce(out=sm[:, 1, :], in_=eg[:, :, :], axis=AX.X, op=ALU.add), [rr], [rr])
            S.dve(lambda: V.reciprocal(out=sm[:, 2, :], in_=sm[:, 1, :]), [rr], [rr])
            S.dve(lambda: V.tensor_scalar(out=f3(oh), in0=f3(oh), scalar1=-1.0, scalar2=BIG, op0=ALU.add, op1=ALU.mult),
                  [rr], [rr])
            S.dve(lambda: V.tensor_tensor(out=lem[:].rearrange("p a (g e) -> p (a g) e", g=4),
                                          in0=LE.rearrange("p a (g e) -> p a g e", g=4),
                                          in1=oh[:, :, :].unsqueeze(3).to_broadcast([128, 4, 4, 8]), op=ALU.add)
                  if False else
                  V.tensor_tensor(out=lem[:, :, :].rearrange("p a (g e) -> p a g e", g=4),
                                  in0=LE.rearrange("p a (g e) -> p a g e", g=4),
                                  in1=oh[:, :, :].unsqueeze(3).to_broadcast([128, 4, 4, 8]), op=ALU.add), R_, [rr])
            S.dve(lambda: V.tensor_reduce(out=sm[:, 3, :], in_=lem[:, :, :], axis=AX.X, op=ALU.max), [rr], [rr])
            S.dve(lambda: V.tensor_tensor(out=o1[:, :, :], in0=lem[:, :, :], in1=bc32(3), op=ALU.is_ge), [rr], [rr])
            S.dve(lambda: V.scalar_tensor_tensor(out=f3(lem), in0=f3(o1), scalar=-BIG, in1=f3(lem), op0=ALU.mult, op1=ALU.add),
                  [rr], [rr])
            S.dve(lambda: V.tensor_reduce(out=sm[:, 4, :], in_=lem[:, :, :], axis=AX.X, op=ALU.max), [rr], [rr])
            S.dve(lambda: V.tensor_tensor(out=o2[:, :, :], in0=lem[:, :, :], in1=bc32(4), op=ALU.is_ge), [rr], [rr])
            S.dve(lambda: V.tensor_tensor(out=sm[:, 5, :], in0=sm[:, 4, :], in1=sm[:, 3, :], op=ALU.subtract), [rr], [rr])
            S.act(lambda: nc.scalar.activation(out=sm[:, 6, :], in_=sm[:, 5, :], func=AF.Exp), [rr], [rr])
            S.dve(lambda: V.tensor_scalar(out=sm[:, 7, :], in0=sm[:, 6, :], scalar1=1.0, scalar2=None, op0=ALU.add), [rr], [rr])
            S.dve(lambda: V.reciprocal(out=sm[:, 8, :], in_=sm[:, 7, :]), [rr], [rr])
            pgr = self.pg_r[g]
            S.dve(lambda: V.tensor_tensor(out=self.g12[:, g * 4:(g + 1) * 4, 0], in0=sm[:, 8, :], in1=sm[:, 2, :], op=ALU.mult),
                  [rr], [pgr])
            S.dve(lambda: V.tensor_tensor(out=self.g12[:, g * 4:(g + 1) * 4, 1], in0=self.g12[:, g * 4:(g + 1) * 4, 0],
                                          in1=sm[:, 6, :], op=ALU.mult), [rr, pgr], [pgr])
            S.dve(lambda: V.tensor_tensor(out=f3(mbf), in0=f3(o1), in1=f3(o2), op=ALU.add), [rr], [rr])
            pp, pp_r = ps_r.next()
            S.pe(lambda: nc.tensor.matmul(pp[:, 0:128], lhsT=u_bf[:, :], rhs=f3(mbf), start=True, stop=True), [rr], [pp_r])
            S.pe(lambda: nc.tensor.matmul(pp[:, 128:256], lhsT=ones_bf[:, :], rhs=f3(mbf), start=True, stop=True), [rr], [pp_r])
            for tl in range(4):
                S.dve(lambda: V.tensor_tensor(out=rf[:, tl, :], in0=pp[:, tl * 32:(tl + 1) * 32], in1=carry[:, :], op=ALU.add),
                      [pp_r, car_r], [rr])
                S.dve(lambda: V.tensor_tensor(out=carry[:, :], in0=carry[:, :], in1=pp[:, 128 + tl * 32:128 + (tl + 1) * 32],
                                              op=ALU.add), [pp_r, car_r], [car_r])
            for kk, ok in enumerate((o1, o2)):
                S.dve(lambda: V.tensor_tensor(out=f3(tmp), in0=f3(ok), in1=f3(rf), op=ALU.mult), [rr], [rr])
                S.dve(lambda: V.tensor_reduce(out=rk[:, :, kk], in_=tmp[:, :, :], axis=AX.X, op=ALU.add), [rr], [rr])
                S.dve(lambda: V.tensor_tensor(out=f3(tmp), in0=f3(ok), in1=ecb[:, :], op=ALU.mult), [rr], [rr])
                S.dve(lambda: V.tensor_reduce(out=bs[:, :, kk], in_=tmp[:, :, :], axis=AX.X, op=ALU.add), [rr], [rr])
            S.dve(lambda: V.tensor_scalar(out=f3(ov), in0=f3(rk), scalar1=float(CAP), scalar2=None, op0=ALU.is_ge), [rr], [rr])
            S.dve(lambda: V.tensor_tensor(out=f3(ps_), in0=f3(rk), in1=f3(bs), op=ALU.add), [rr], [rr])
            S.dve(lambda: V.tensor_tensor(out=f3(pd), in0=pid[:, :], in1=f3(ps_), op=ALU.subtract), [rr], [rr])
            S.dve(lambda: V.tensor_tensor(out=f3(pd), in0=f3(pd), in1=f3(ov), op=ALU.mult), [rr], [rr])
            S.dve(lambda: V.tensor_tensor(out=f3(ps_), in0=f3(ps_), in1=f3(pd), op=ALU.add), [rr], [rr])
            S.dve(lambda: V.tensor_copy(out=self.posg[:, g * 4:(g + 1) * 4, :].rearrange("p a b -> p (a b)"), in_=f3(ps_)),
                  [rr], [pgr])
            for tl in range(4):
                xb_t, xb_r = xb_list[tl]
                for kk in range(2):
                    S.idma(self.xs[:, :], bass.IndirectOffsetOnAxis(ap=self.posg[:, g * 4 + tl, kk:kk + 1], axis=0), xb_t[:, :],
                           None, None, [xb_r, pgr], [self.xs_r])
        P.close()

    def phase_moe(self, l, last):
        nc, S = self.nc, self.S
        V = nc.vector
        P = Phase(S)
        g_bc = P.sb([128, D], F32)
        b_bc = P.sb([128, D], F32)
        S.dma("sp", g_bc[:], self.lng[l * 2 + 1], [], [Reg()])
        S.dma("sp", b_bc[:], self.lnb[l * 2 + 1], [], [Reg()])
        S.barrier()
        gb_r = Reg()
        W1 = P.rot_sb(2, [128, 8, 512], BF16)
        W3 = P.rot_sb(2, [128, 8, 512], BF16)
        W2 = P.rot_sb(2, [128, 4, D], BF16)
        S3 = P.rot_sb(1, [128, 8, 512], F32)
        S2 = P.rot_sb(1, [128, 4, D], F32)
        xsl = P.rot_sb(2, [128, 4, D], BF16)
        xgT = P.rot_sb(2, [128, 8, 512], BF16)
        ps_tr = P.rot_ps(2, [128, 1024], BF16)
        ps_h1 = P.rot_ps(2, [128, 512], F32)
        ps_h3 = P.rot_ps(2, [128, 512], F32)
        ps_y = P.rot_ps(2, [128, 512], F32)
        sl = P.rot_sb(2, [128, 512], F32)
        hT = P.rot_sb(2, [128, 4, 512], BF16)
        ysb = P.rot_sb(3, [128, D], F32)
        NB = CAP // 128
        fl = lambda t_: t_[:].rearrange("p a b -> p (a b)")
        wq = {}

        def fetch(e):
            w1_t, w1_r = W1.next()
            w3_t, w3_r = W3.next()
            w2_t, w2_r = W2.next()
            s3_t, s3_r = S3.next()
            s2_t, s2_r = S2.next()
            S.dma("pool", w1_t[:], self.w1[l, e].rearrange("(k p) f -> p k f", p=128), [], [w1_r])
            S.dma("sp", s3_t[:], self.w3[l, e].rearrange("(k p) f -> p k f", p=128), [], [s3_r])
            S.dma("sp", s2_t[:], self.w2[l, e].rearrange("(k p) f -> p k f", p=128), [], [s2_r])
            xs_t, xs_r = xsl.next()
            S.dma("sp", xs_t[:, 0:NB, :], self.xs[e * CAP:(e + 1) * CAP, :].rearrange("(b p) d -> p b d", p=128),
                  [self.xs_r], [xs_r])
            wq[e] = (w1_t, w1_r, w3_t, w3_r, w2_t, w2_r, s3_t, s3_r, s2_t, s2_r, xs_t, xs_r)

        def cast(e):
            (w1_t, w1_r, w3_t, w3_r, w2_t, w2_r, s3_t, s3_r, s2_t, s2_r, xs_t, xs_r) = wq[e]
            S.act(lambda: nc.scalar.copy(out=fl(w3_t), in_=fl(s3_t)), [s3_r], [w3_r])
            S.dve(lambda: V.tensor_copy(out=fl(w2_t), in_=fl(s2_t)), [s2_r], [w2_r])

        fetch(0)
        cast(0)
        for e in range(32):
            (w1_t, w1_r, w3_t, w3_r, w2_t, w2_r, s3_t, s3_r, s2_t, s2_r, xs_t, xs_r) = wq.pop(e)
            if e + 1 < 32:
                fetch(e + 1)
            xg, xg_r = xgT.next()
            for k2 in range(4):
                ptr, ptr_r = ps_tr.next()
                for kk in range(2):
                    k = k2 * 2 + kk
                    for bb in range(NB):
                        S.pe(lambda: nc.tensor.transpose(ptr[:, kk * 512 + bb * 128:kk * 512 + (bb + 1) * 128],
                                                         xs_t[:, bb, k * 128:(k + 1) * 128], self.ident_bf[:]), [xs_r], [ptr_r])
                if k2 % 2 == 0:
                    S.act(lambda: nc.scalar.copy(out=xg[:, k2 * 2:k2 * 2 + 2, :].rearrange("p a b -> p (a b)"), in_=ptr[:, :]),
                          [ptr_r], [xg_r])
                else:
                    S.dve(lambda: V.tensor_copy(out=xg[:, k2 * 2:k2 * 2 + 2, :].rearrange("p a b -> p (a b)"), in_=ptr[:, :]),
                          [ptr_r], [xg_r])
            h_t, h_r = hT.next()
            for fc in range(4):
                p1, p1_r = ps_h1.next()
                p3, p3_r = ps_h3.next()
                for k in range(8):
                    S.pe(lambda: nc.tensor.matmul(p1[:, :], lhsT=w1_t[:, k, fc * 128:(fc + 1) * 128], rhs=xg[:, k, :],
                                                  start=(k == 0), stop=(k == 7)), [w1_r, xg_r], [p1_r])
                for k in range(8):
                    S.pe(lambda: nc.tensor.matmul(p3[:, :], lhsT=w3_t[:, k, fc * 128:(fc + 1) * 128], rhs=xg[:, k, :],
                                                  start=(k == 0), stop=(k == 7)), [w3_r, xg_r], [p3_r])
                s_t, s_r = sl.next()
                S.act(lambda: nc.scalar.activation(out=s_t[:, :], in_=p1[:, :], func=AF.Silu), [p1_r], [s_r])
                S.dve(lambda: V.tensor_tensor(out=h_t[:, fc, :], in0=p3[:, :], in1=s_t[:, :], op=ALU.mult), [p3_r, s_r], [h_r])
            for st_ in range(NB):
                y_t, y_r = ysb.next()
                for dh in range(2):
                    py, py_r = ps_y.next()
                    for fc in range(4):
                        S.pe(lambda: nc.tensor.matmul(py[:, :], lhsT=h_t[:, fc, st_ * 128:(st_ + 1) * 128],
                                                      rhs=w2_t[:, fc, dh * 512:(dh + 1) * 512], start=(fc == 0), stop=(fc == 3)),
                             [h_r, w2_r], [py_r])
                    if dh == 0:
                        S.act(lambda: nc.scalar.copy(out=y_t[:, 0:512], in_=py[:, :]), [py_r], [y_r])
                    else:
                        S.dve(lambda: V.tensor_copy(out=y_t[:, 512:1024], in_=py[:, :]), [py_r], [y_r])
                S.dma("sp", self.yb[e * CAP + st_ * 128:e * CAP + (st_ + 1) * 128, :], y_t[:, :], [y_r], [self.yb_r])
            if e + 1 < 32:
                cast(e + 1)
        P.close()
        P = Phase(S)
        g_bc = P.sb([128, D], F32)
        b_bc = P.sb([128, D], F32)
        S.dma("sp", g_bc[:], self.lng[l * 2 + 1], [], [Reg()])
        S.dma("sp", b_bc[:], self.lnb[l * 2 + 1], [], [Reg()])
        S.barrier()
        xs2 = P.rot_sb(3, [128, D], F32)
        ya = P.rot_sb(4, [128, D], F32)
        ybb = P.rot_sb(4, [128, D], F32)
        ys = P.rot_sb(2, [128, D], F32)
        xo = P.rot_sb(2, [128, D], F32)
        st = P.sb([128, 12], F32)
        mv = P.sb([128, 2], F32)
        rs = P.sb([128, 1], F32)
        scr = (st, Reg(), mv, Reg(), rs, Reg())
        dst = self.out if last else self.xres
        for t in range(32):
            pgr = self.pg_r[t // 4]
            x_t, x_r = xs2.next()
            S.dma("sp", x_t[:], self.xres[t * 128:(t + 1) * 128, :], [self.xres_r[2 * t], self.xres_r[2 * t + 1]], [x_r])
            a_t, a_r = ya.next()
            b_t, b_r = ybb.next()
            S.idma(a_t[:, :], None, self.yb[:, :], bass.IndirectOffsetOnAxis(ap=self.posg[:, t, 0:1], axis=0), NS + 127,
                   [self.yb_r, pgr], [a_r])
            S.idma(b_t[:, :], None, self.yb[:, :], bass.IndirectOffsetOnAxis(ap=self.posg[:, t, 1:2], axis=0), NS + 127,
                   [self.yb_r, pgr], [b_r])
            y_t, y_r = ys.next()
            S.act(lambda: nc.scalar.activation(out=y_t[:, :], in_=x_t[:, :], func=AF.Copy, scale=ALPHA), [x_r], [y_r])
            S.dve(lambda: V.scalar_tensor_tensor(out=y_t[:, :], in0=a_t[:, :], scalar=self.g12[:, t, 0:1], in1=y_t[:, :],
                                                 op0=ALU.mult, op1=ALU.add), [a_r, pgr, y_r], [y_r])
            S.dve(lambda: V.scalar_tensor_tensor(out=y_t[:, :], in0=b_t[:, :], scalar=self.g12[:, t, 1:2], in1=y_t[:, :],
                                                 op0=ALU.mult, op1=ALU.add), [b_r, pgr, y_r], [y_r])
            xo_t, xo_r = xo.next()
            self.layer_norm_tile(P, 128, y_t, y_r, g_bc, b_bc, gb_r, xo_t, xo_r, scr, tail="dve")
            S.dma("sp", dst[t * 128:(t + 1) * 128, :], xo_t[:, :], [xo_r], [self.xres_r[2 * t], self.xres_r[2 * t + 1]])
        P.close()

    def dump(self, src_ap, shape, dt):
        nc, S = self.nc, self.S
        dbg = nc.dram_tensor("dbg", list(shape), dt, kind="ExternalOutput").ap()
        S.barrier()
        n = shape[0] // 128
        for i in range(n):
            S.dma("sp", dbg[i * 128:(i + 1) * 128, :], src_ap[i * 128:(i + 1) * 128, :], [], [Reg()])
        S.barrier()

    def init_scratch(self):
        nc, S = self.nc, self.S
        P = Phase(S)
        zb = P.sb([128, 8, D], BF16)
        zf = P.sb([128, D], F32)
        r = Reg()
        S.dve(lambda: nc.vector.memset(zb[:].rearrange("p a b -> p (a b)"), 0.0), [], [r])
        S.dve(lambda: nc.vector.memset(zf[:], 0.0), [], [r])
        for i in range(NS // 1024):
            S.dma("sp", self.xs[i * 1024:(i + 1) * 1024, :].rearrange("(p a) d -> p a d", p=128), zb[:, :, :], [r], [Reg()])
        S.dma("sp", self.xs[NS:NS + 128, :], zb[:, 0, :], [r], [Reg()])
        S.dma("sp", self.yb[NS:NS + 128, :], zf[:, :], [r], [Reg()])
        P.close()

    def build(self):
        dbg = self.debug
        self.init_scratch()
        for l in range(self.nlayers):
            if l % 2 == 0:
                self.phase_attn_proj(l)
                if dbg == "qkt%d" % l:
                    self.dump(self.qkt.rearrange("a b c -> (a b) c"), [2048, T], BF16)
                    return self.nc
                self.phase_attn(l)
                if dbg == "ot%d" % l:
                    self.dump(self.ot, [D, T], BF16)
                    return self.nc
                self.phase_out_ln_router(l, self.ab_w_out[l // 2])
            else:
                self.phase_hgrn(l)
                if dbg == "ot%d" % l:
                    self.dump(self.ot, [D, T], BF16)
                    return self.nc
                self.phase_out_ln_router(l, self.c_w_out[l // 2])
            if dbg == "xa%d" % l:
                self.dump(self.xres, [T, D], F32)
                return self.nc
            self.phase_moe(l, last=(l == self.nlayers - 1))
        self.S.barrier()
        return self.nc


def host_inputs(inputs):
    f = lambda a: np.ascontiguousarray(np.asarray(a, dtype=np.float32))
    d = {}
    d["ab_w_in"] = f(inputs["ab_w_in"])
    d["ab_w_out"] = f(inputs["ab_w_out"])
    d["c_w_in"] = f(inputs["c_w_in"])
    d["c_w_out"] = f(inputs["c_w_out"])
    d["exp_w1"] = f(inputs["exp_w1"])
    d["exp_w3"] = f(inputs["exp_w3"])
    d["exp_w2"] = f(inputs["exp_w2"])
    cng = np.tile(f(inputs["c_norm_g"]), (1, 8))
    d["cng"] = np.ascontiguousarray(np.broadcast_to(cng[:, None, :], (2, 64, D)))
    d["lbl"] = np.ascontiguousarray(np.broadcast_to(f(inputs["hgrn_lb_logits"])[:, None, :], (2, 64, D)))
    d["lng"] = np.ascontiguousarray(np.broadcast_to(f(inputs["ln_g"]).reshape(8, 1, D), (8, 128, D)))
    d["lnb"] = np.ascontiguousarray(np.broadcast_to(f(inputs["ln_b"]).reshape(8, 1, D), (8, 128, D)))
    d["rw"] = np.ascontiguousarray(np.concatenate([f(inputs["router_g_w"]), f(inputs["router_e_w"])], axis=2))
    rb = np.concatenate([f(inputs["router_g_b"]), f(inputs["router_e_b"])], axis=1)
    d["rb"] = np.ascontiguousarray(np.broadcast_to(rb[:, None, :], (4, 128, 36)))
    return d


_CACHE = {}


def kernel(**inputs):
    x = np.ascontiguousarray(np.asarray(inputs["x"], dtype=np.float32))
    if "prog" not in _CACHE:
        p = Prog()
        p.build()
        _CACHE["prog"] = p
    p = _CACHE["prog"]
    shared = host_inputs(inputs)
    for k, v in p.consts_np.items():
        shared["c_" + k] = v
    in_maps = []
    for c in range(8):
        m = dict(shared)
        m["x"] = x[c]
        in_maps.append(m)
    res = run_bass_kernel_spmd(p.nc, in_maps, core_ids=list(range(8)))
    return np.stack([np.asarray(r["out"], dtype=np.float32) for r in res.results], axis=0)
```

```python
import contextlib
import os
LVL = int(os.environ.get('DBG_LVL', '9'))
import numpy as np
import ml_dtypes
import concourse.bass as bass
import concourse.mybir as mybir
from concourse.bass_utils import run_bass_kernel_spmd

F32 = mybir.dt.float32
BF16 = mybir.dt.bfloat16
AF = mybir.ActivationFunctionType
ALU = mybir.AluOpType
AX = mybir.AxisListType

T = 4096
D = 1024
DEPTH = 4
ALPHA = float((2.0 * DEPTH) ** 0.25)
LN_EPS = 1e-5
RMS_EPS = 1e-6
BIG = 30000.0
EPOCH = 30000
NSLOT = 8
CAP = 512
NS = 32 * CAP
I32 = mybir.dt.int32


class Reg:
    __slots__ = ("w", "r", "x")

    def __init__(self, x=False):
        self.w = {}
        self.r = {}
        self.x = x


class Eng:
    def __init__(self, name, e, is_pe=False):
        self.name = name
        self.e = e
        self.count = 0
        self.known = {}
        self.is_pe = is_pe
        self.dcount = 0


class Sched:
    def __init__(self, nc):
        self.nc = nc
        self.E = {
            "pe": Eng("pe", nc.tensor, True),
            "act": Eng("act", nc.scalar),
            "dve": Eng("dve", nc.vector),
            "pool": Eng("pool", nc.gpsimd),
            "sp": Eng("sp", nc.sync),
        }
        self.semh = {}
        self.nsem = 0

    def _sem(self, key):
        h = self.semh.get(key)
        if h is None:
            h = self.nc.alloc_semaphore("s_%s_%s_%d" % key)
            self.semh[key] = h
            self.nsem += 1
        return h

    def _waits(self, E, reads, writes):
        deps = {}
        for r in reads:
            for k, v in r.w.items():
                if deps.get(k, 0) < v:
                    deps[k] = v
            if r.x:
                for k, v in r.r.items():
                    if deps.get(k, 0) < v:
                        deps[k] = v
        for w in writes:
            for k, v in w.w.items():
                if deps.get(k, 0) < v:
                    deps[k] = v
            for k, v in w.r.items():
                if deps.get(k, 0) < v:
                    deps[k] = v
        for k, v in deps.items():
            if E.is_pe and k[0] == "pe" and k[1] == "c":
                continue
            if E.known.get(k, 0) < v:
                E.e.wait_ge(self._sem(k), v)
                E.known[k] = v

    def op(self, en, fn, reads, writes):
        E = self.E[en]
        self._waits(E, reads, writes)
        ins = fn()
        key = (en, "c", E.count // EPOCH)
        val = E.count % EPOCH + 1
        ins.then_inc(self._sem(key), 1)
        E.count += 1
        for r in reads:
            if r.x:
                r.w = {key: val}
                r.r = {}
            else:
                r.r[key] = val
        for w in writes:
            w.w = {key: val}
            w.r = {}

    def pe(self, fn, reads, writes):
        self.op("pe", fn, reads, writes)

    def act(self, fn, reads, writes):
        self.op("act", fn, reads, writes)

    def dve(self, fn, reads, writes):
        self.op("dve", fn, reads, writes)

    def pool(self, fn, reads, writes):
        self.op("pool", fn, reads, writes)

    def dma(self, qn, out, in_, reads, writes):
        Q = self.E[qn]
        self._waits(Q, reads, writes)
        i = Q.dcount
        slot = i % NSLOT
        tgt = 16 * (i // NSLOT + 1)
        assert tgt < 60000
        key = (qn, "d", slot)
        if i >= NSLOT and Q.known.get(key, 0) < tgt - 16:
            Q.e.wait_ge(self._sem(key), tgt - 16)
            Q.known[key] = tgt - 16
        Q.e.dma_start(out=out, in_=in_).then_inc(self._sem(key), 16)
        Q.dcount += 1
        for r in reads:
            if r.r.get(key, 0) < tgt:
                r.r[key] = tgt
        for w in writes:
            w.w = {key: tgt}
            w.r = {}

    def idma(self, out, out_off, in_, in_off, bound, reads, writes):
        Q = self.E["pool"]
        self._waits(Q, reads, writes)
        i = Q.dcount
        slot = i % NSLOT
        tgt = 16 * (i // NSLOT + 1)
        assert tgt < 60000
        key = ("pool", "d", slot)
        if i >= NSLOT and Q.known.get(key, 0) < tgt - 16:
            Q.e.wait_ge(self._sem(key), tgt - 16)
            Q.known[key] = tgt - 16
        Q.e.indirect_dma_start(out=out, out_offset=out_off, in_=in_, in_offset=in_off).then_inc(self._sem(key), 16)
        Q.dcount += 1
        for r in reads:
            if r.r.get(key, 0) < tgt:
                r.r[key] = tgt
        for w in writes:
            w.w = {key: tgt}
            w.r = {}

    def barrier(self):
        latest = {}
        for E in self.E.values():
            if E.count > 0:
                latest[(E.name, "c", (E.count - 1) // EPOCH)] = (E.count - 1) % EPOCH + 1
            for slot in range(min(NSLOT, E.dcount)):
                n = (E.dcount - slot + NSLOT - 1) // NSLOT
                latest[(E.name, "d", slot)] = 16 * n
        for E in self.E.values():
            for k, v in latest.items():
                if E.is_pe and k[0] == "pe" and k[1] == "c":
                    continue
                if E.known.get(k, 0) < v:
                    E.e.wait_ge(self._sem(k), v)
                    E.known[k] = v


class Phase:
    cnt = 0

    def __init__(self, S):
        self.S = S
        self.nc = S.nc
        self.stack = contextlib.ExitStack()

    def sb(self, shape, dt):
        Phase.cnt += 1
        return self.stack.enter_context(self.nc.sbuf_tensor("sb%d" % Phase.cnt, list(shape), dt))

    def ps(self, shape, dt=F32):
        Phase.cnt += 1
        return self.stack.enter_context(self.nc.psum_tensor("ps%d" % Phase.cnt, list(shape), dt))

    def rot_sb(self, n, shape, dt):
        return Rot([(self.sb(shape, dt), Reg()) for _ in range(n)])

    def rot_ps(self, n, shape, dt=F32):
        return Rot([(self.ps(shape, dt), Reg(True)) for _ in range(n)])

    def close(self):
        self.S.barrier()
        self.stack.close()


class Rot:
    def __init__(self, items):
        self.items = items
        self.i = 0

    def next(self):
        it = self.items[self.i % len(self.items)]
        self.i += 1
        return it


def make_consts():
    bf = ml_dtypes.bfloat16
    c = {}
    c["ident_bf"] = np.eye(128, dtype=np.float32).astype(bf)
    c["ident_f"] = np.eye(128, dtype=np.float32)
    half = 8
    inv = (np.float32(500000.0) ** (-np.arange(half, dtype=np.float32) / np.float32(half))).astype(np.float32)
    ang = (np.arange(T, dtype=np.float32)[:, None] * inv[None, :]).astype(np.float32)
    cos = np.cos(ang.astype(np.float64)).astype(np.float32)
    sin = np.sin(ang.astype(np.float64)).astype(np.float32)
    c2 = np.concatenate([cos, cos], axis=1)
    s2 = np.concatenate([-sin, sin], axis=1)
    c2e = np.tile(c2, (1, 8))
    s2e = np.tile(s2, (1, 8))
    c["c2e"] = np.ascontiguousarray(c2e.reshape(32, 128, 128).transpose(1, 0, 2).reshape(128, 4096))
    c["s2e"] = np.ascontiguousarray(s2e.reshape(32, 128, 128).transpose(1, 0, 2).reshape(128, 4096))

    def mult(d):
        m = np.zeros_like(d, dtype=np.float32)
        m += ((d >= 0) & (d <= 128))
        m += ((d >= 0) & (d % 4 == 0) & (d <= 512))
        m += ((d >= 0) & (d % 16 == 0) & (d <= 2048))
        return m

    kl = np.arange(128)[:, None]
    ql = np.arange(512)[None, :]
    dm = np.zeros((128, 20, 512), np.float32)
    for di in range(20):
        delta = -384 + 128 * di
        dm[:, di, :] = mult(delta + ql - kl)
    with np.errstate(divide="ignore"):
        ldm = np.where(dm > 0, 8.0 * np.log(np.maximum(dm, 1e-30).astype(np.float64)), -BIG)
    ldm_hi = ldm.astype(np.float32).astype(bf)
    ldm_lo = (ldm - ldm_hi.astype(np.float64)).astype(np.float32)
    ldm_lo = np.where(dm > 0, ldm_lo, 0.0).astype(bf)
    c["ldm_hi"] = ldm_hi.reshape(128, 20 * 512)
    c["dmm"] = dm[:, 0:8, :].reshape(128, 8 * 512).astype(bf)
    c["dm_lo_need"] = np.array([float(np.any(ldm_lo[:, di, :].astype(np.float32) != 0)) for di in range(20)], np.float32)
    cm = np.zeros((128, 4, 512), np.float32)
    for ci in range(4):
        delta = -384 + 128 * ci
        cm[:, ci, :] = (delta + ql - kl >= 0)
    c["lcm"] = ((cm - 1.0) * BIG).reshape(128, 4 * 512).astype(bf)
    koh = np.zeros((16, T), np.float32)
    for b in range(16):
        koh[b, b * 256:(b + 1) * 256] = 1.0
    c["koh"] = koh.astype(bf)
    tt = np.arange(32)[:, None]
    bb = np.arange(16)[None, :]
    own = tt // 2
    valid = (bb < own).astype(np.float32)
    negv = (valid - 1.0) * BIG
    ownm1 = (bb == own).astype(np.float32) - 1.0
    c["valid"] = np.ascontiguousarray(np.broadcast_to(valid.reshape(1, 512), (128, 512))).astype(np.float32)
    c["negv"] = np.ascontiguousarray(np.broadcast_to(negv.reshape(1, 512), (128, 512))).astype(np.float32)
    c["ownm1"] = np.ascontiguousarray(np.broadcast_to(ownm1.reshape(1, 512), (128, 512))).astype(np.float32)
    j = np.arange(64)[:, None]
    i = np.arange(64)[None, :]
    c["mg"] = ((j <= i).astype(np.float32) - (j <= 31).astype(np.float32)).astype(np.float32)
    ind = np.zeros((64, 3), np.float32)
    ind[:, 0] = (np.arange(64) <= 31)
    ind[:, 1] = 1.0
    ind[:, 2] = (np.arange(64) > 31)
    c["ind"] = ind
    c["cmask8"] = np.ascontiguousarray(np.tile((i >= j).astype(np.float32), (1, 8)))
    c["ones_f"] = np.ones((128, 64), np.float32)
    tp = np.arange(128)[:, None]
    tq = np.arange(128)[None, :]
    c["u_bf"] = (tp < tq).astype(np.float32).astype(bf)
    c["ones_bf"] = np.ones((128, 128), np.float32).astype(bf)
    ecb = np.tile((np.arange(32, dtype=np.float32) * CAP)[None, :], (1, 4))
    c["ecb"] = np.ascontiguousarray(np.broadcast_to(ecb, (128, 128))).astype(np.float32)
    c["pid"] = np.ascontiguousarray(np.broadcast_to((NS + np.arange(128, dtype=np.float32))[:, None], (128, 8)))
    return c


CONST_DT = {"ident_bf": BF16, "ldm_hi": BF16, "dmm": BF16, "lcm": BF16, "koh": BF16, "u_bf": BF16, "ones_bf": BF16}


class Prog:
    def __init__(self, nlayers=DEPTH, debug=None):
        self.nc = nc = bass.Bass("TRN2", target_bir_lowering=False)
        self.S = Sched(nc)
        self.nlayers = nlayers
        self.debug = debug
        self.consts_np = make_consts()
        di = lambda name, shape, dt=F32: nc.dram_tensor(name, list(shape), dt, kind="ExternalInput").ap()
        self.x_in = di("x", [T, D])
        self.ab_w_in = di("ab_w_in", [2, D, 3072])
        self.ab_w_out = di("ab_w_out", [2, D, D])
        self.c_w_in = di("c_w_in", [2, D, 4096])
        self.c_w_out = di("c_w_out", [2, D, D])
        self.cng = di("cng", [2, 64, D])
        self.lbl = di("lbl", [2, 64, D])
        self.lng = di("lng", [8, 128, D])
        self.lnb = di("lnb", [8, 128, D])
        self.rw = di("rw", [4, D, 36])
        self.rb = di("rb", [4, 128, 36])
        self.w1 = di("exp_w1", [4, 32, D, 512])
        self.w3 = di("exp_w3", [4, 32, D, 512])
        self.w2 = di("exp_w2", [4, 32, 512, D])
        self.cd = {}
        self.dm_lo_need = [bool(v) for v in self.consts_np.pop("dm_lo_need")]
        for k, v in self.consts_np.items():
            self.cd[k] = di("c_" + k, v.shape, CONST_DT.get(k, F32))
        self.out = nc.dram_tensor("out", [T, D], F32, kind="ExternalOutput").ap()
        dt_ = lambda name, shape, dt: nc.dram_tensor(name, list(shape), dt).ap()
        self.xres = dt_("xres", [T, D], F32)
        self.xres_r = [Reg() for _ in range(64)]
        self.qkt = dt_("qkt", [4, 512, T], BF16)
        self.qkt_r = Reg()
        self.vd = dt_("vd", [T, D], BF16)
        self.vd_r = Reg()
        self.ot = dt_("ot", [D, T], BF16)
        self.ot_r = Reg()
        self.xt = dt_("xt", [D, T], BF16)
        self.xt_r = Reg()
        self.ident_bf = nc.alloc_sbuf_tensor("ident_bf", [128, 128], BF16)
        self.ident_f = nc.alloc_sbuf_tensor("ident_f", [128, 128], F32)
        self.posg = nc.alloc_sbuf_tensor("posg", [128, 32, 2], I32)
        self.g12 = nc.alloc_sbuf_tensor("g12", [128, 32, 2], F32)
        self.pg_r = [Reg() for _ in range(8)]
        self.xs = dt_("xs", [NS + 128, D], BF16)
        self.xs_r = Reg()
        self.yb = dt_("yb", [NS + 128, D], F32)
        self.yb_r = Reg()
        self.cr = Reg()
        S = self.S
        S.dma("sp", self.ident_bf[:], self.cd["ident_bf"], [], [self.cr])
        r2 = Reg()
        S.dma("sp", self.ident_f[:], self.cd["ident_f"], [], [r2])
        self.cr2 = r2

    def xsrc(self, l):
        return self.x_in if l == 0 else self.xres

    def layer_norm_tile(self, P, np_, y, y_r, g_bc, b_bc, gb_r, out_t, out_r, scr, tail="pool"):
        nc, S = self.nc, self.S
        st, st_r, mv, mv_r, rs, rs_r = scr
        S.dve(lambda: nc.vector.bn_stats(out=st[:np_, 0:6], in_=y[:np_, 0:512]), [y_r], [st_r])
        S.dve(lambda: nc.vector.bn_stats(out=st[:np_, 6:12], in_=y[:np_, 512:1024]), [y_r], [st_r])
        S.dve(lambda: nc.vector.bn_aggr(out=mv[:np_, :], in_=st[:np_, :]), [st_r], [mv_r])
        S.dve(lambda: nc.vector.tensor_scalar(out=rs[:np_, :], in0=mv[:np_, 1:2], scalar1=LN_EPS, scalar2=None,
                                              op0=ALU.add), [mv_r], [rs_r])
        S.act(lambda: nc.scalar.activation(out=rs[:np_, :], in_=rs[:np_, :], func=AF.Ln), [rs_r], [rs_r])
        S.act(lambda: nc.scalar.activation(out=rs[:np_, :], in_=rs[:np_, :], func=AF.Exp, scale=-0.5), [rs_r], [rs_r])
        S.dve(lambda: nc.vector.tensor_scalar(out=out_t[:np_, :], in0=y[:np_, :], scalar1=mv[:np_, 0:1],
                                              scalar2=rs[:np_, 0:1], op0=ALU.subtract, op1=ALU.mult),
              [y_r, mv_r, rs_r], [out_r])
        TE = nc.gpsimd if tail == "pool" else nc.vector
        S.op(tail, lambda: TE.tensor_tensor(out=out_t[:np_, :], in0=out_t[:np_, :], in1=g_bc[:np_, :], op=ALU.mult),
             [out_r, gb_r], [out_r])
        S.op(tail, lambda: TE.tensor_tensor(out=out_t[:np_, :], in0=out_t[:np_, :], in1=b_bc[:np_, :], op=ALU.add),
             [out_r, gb_r], [out_r])

    def phase_attn_proj(self, l):
        nc, S = self.nc, self.S
        j = l // 2
        P = Phase(S)
        W = P.sb([128, 8, 3072], BF16)
        w_r = []
        for k in range(8):
            r = Reg()
            S.dma("pool", W[:, k, :], self.ab_w_in[j, k * 128:(k + 1) * 128, :], [], [r])
            w_r.append(r)
        c2e = P.sb([128, 32, 128], F32)
        s2e = P.sb([128, 32, 128], F32)
        tab_r = Reg()
        tab_r2 = Reg()
        S.dma("sp", c2e[:].rearrange("p a b -> p (a b)"), self.cd["c2e"], [], [tab_r])
        S.dma("sp", s2e[:].rearrange("p a b -> p (a b)"), self.cd["s2e"], [], [tab_r2])
        xs = P.rot_sb(2, [128, D], F32)
        xT = P.rot_sb(2, [128, 8, 128], BF16)
        ps_t = P.rot_ps(2, [128, 512], F32)
        ps_o = P.rot_ps(3, [128, 512], F32)
        ps_tr = P.rot_ps(2, [128, 1024], BF16)
        qs = P.rot_sb(3, [128, 512], BF16)
        t1 = P.rot_sb(2, [128, 8, 16], F32)
        t2 = P.rot_sb(2, [128, 8, 16], F32)
        stg = P.rot_sb(2, [128, 16, 512], BF16)
        vst = P.rot_sb(2, [128, 1024], BF16)
        src = self.xsrc(l)
        for g in range(8):
            stg_t, stg_r = stg.next()
            for tl in range(4):
                t = g * 4 + tl
                x_t, x_r = xs.next()
                S.dma("sp", x_t[:], src[t * 128:(t + 1) * 128, :], [self.xres_r[2 * t], self.xres_r[2 * t + 1]], [x_r])
                xT_t, xT_r = xT.next()
                for hb in range(2):
                    pt, pt_r = ps_t.next()
                    for kk in range(4):
                        k = hb * 4 + kk
                        S.pe(lambda: nc.tensor.transpose(pt[:, kk * 128:(kk + 1) * 128], x_t[:, k * 128:(k + 1) * 128],
                                                         self.ident_f[:]), [x_r, self.cr2], [pt_r])
                    S.act(lambda: nc.scalar.copy(out=xT_t[:, hb * 4:(hb + 1) * 4, :].rearrange("p a b -> p (a b)"),
                                                 in_=pt[:, :]), [pt_r], [xT_r])
                v_t, v_r = vst.next()
                for cg in range(6):
                    if LVL < 2:
                        break
                    po, po_r = ps_o.next()
                    for k in range(8):
                        S.pe(lambda: nc.tensor.matmul(po[:, :], lhsT=xT_t[:, k, :], rhs=W[:, k, cg * 512:(cg + 1) * 512],
                                                      start=(k == 0), stop=(k == 7)), [xT_r, w_r[k]], [po_r])
                    if cg in (2, 5):
                        off = 0 if cg == 2 else 512
                        S.act(lambda: nc.scalar.copy(out=v_t[:, off:off + 512], in_=po[:, :]), [po_r], [v_r])
                        continue
                    cgi = {0: 0, 1: 1, 3: 2, 4: 3}[cg]
                    if LVL < 3:
                        continue
                    q_t, q_r = qs.next()
                    S.act(lambda: nc.scalar.copy(out=q_t[:, :], in_=po[:, :]), [po_r], [q_r])
                    a1, a1_r = t1.next()
                    a2, a2_r = t2.next()
                    pov = po[:, :].rearrange("p (h d) -> p h d", h=8)
                    qv = q_t[:, :].rearrange("p (h d) -> p h d", h=8)
                    cv = c2e[:, t, :].rearrange("p (h d) -> p h d", h=8)
                    sv = s2e[:, t, :].rearrange("p (h d) -> p h d", h=8)
                    S.dve(lambda: nc.vector.tensor_tensor(out=a1[:, :, :], in0=pov[:, :, 0:16], in1=cv, op=ALU.mult),
                          [po_r, tab_r], [a1_r])
                    S.dve(lambda: nc.vector.tensor_tensor(out=a2[:, :, 0:8], in0=pov[:, :, 8:16], in1=sv[:, :, 0:8],
                                                          op=ALU.mult), [po_r, tab_r2], [a2_r])
                    S.dve(lambda: nc.vector.tensor_tensor(out=a2[:, :, 8:16], in0=pov[:, :, 0:8], in1=sv[:, :, 8:16],
                                                          op=ALU.mult), [po_r, tab_r2], [a2_r])
                    S.dve(lambda: nc.vector.tensor_tensor(out=qv[:, :, 0:16], in0=a1[:, :, :], in1=a2[:, :, :], op=ALU.add),
                          [a1_r, a2_r], [q_r])
                    if LVL < 4:
                        continue
                    ptr, ptr_r = ps_tr.next()
                    for pr in range(4):
                        S.pe(lambda: nc.tensor.transpose(ptr[:, pr * 128:(pr + 1) * 128], q_t[:, pr * 128:(pr + 1) * 128],
                                                         self.ident_bf[:]), [q_r, self.cr], [ptr_r])
                    S.dve(lambda: nc.vector.tensor_copy(
                        out=stg_t[:, cgi * 4:(cgi + 1) * 4, tl * 128:(tl + 1) * 128],
                        in_=ptr[:, 0:512].rearrange("p (a b) -> p a b", a=4)), [ptr_r], [stg_r])
                if LVL >= 2:
                    S.dma("act", self.vd[t * 128:(t + 1) * 128, :], v_t[:, :], [v_r], [self.vd_r])
            for cgi in range(4):
                if LVL < 5:
                    break
                S.dma("sp", self.qkt[cgi, :, g * 512:(g + 1) * 512].rearrange("(a p) t -> p a t", p=128),
                      stg_t[:, cgi * 4:(cgi + 1) * 4, :], [stg_r], [self.qkt_r])
        P.close()

    def phase_attn(self, l):
        nc, S = self.nc, self.S
        P = Phase(S)
        dmh = P.sb([128, 20, 512], BF16)
        dmm = P.sb([128, 8, 512], BF16)
        cm = P.sb([128, 4, 512], BF16)
        S.dma("sp", dmh[:].rearrange("p a b -> p (a b)"), self.cd["ldm_hi"], [], [Reg()])
        S.dma("sp", dmm[:].rearrange("p a b -> p (a b)"), self.cd["dmm"], [], [Reg()])
        S.dma("sp", cm[:].rearrange("p a b -> p (a b)"), self.cd["lcm"], [], [Reg()])
        valid = P.sb([128, 512], F32)
        negv = P.sb([128, 512], F32)
        ownm1 = P.sb([128, 512], F32)
        ones_f = P.sb([128, 64], F32)
        k_r = Reg()
        for tl_, nm in ((valid, "valid"), (negv, "negv"), (ownm1, "ownm1"), (ones_f, "ones_f")):
            r = Reg()
            S.dma("sp", tl_[:], self.cd[nm], [], [r])
            k_r = r
        cst_r = Reg()
        QT = [P.sb([128, T], BF16) for _ in range(2)]
        KT = [P.sb([128, T], BF16) for _ in range(2)]
        VA = [P.sb([128, 32, 128], BF16) for _ in range(2)]
        QT_r = [Reg(), Reg()]
        QTb_r = [Reg(), Reg()]
        KT_r = [Reg(), Reg()]
        KTc_r = [Reg(), Reg()]
        VA_r = [[Reg() for _ in range(4)] for _ in range(2)]
        VAo_r = [Reg(), Reg()]
        for b in range(2):
            S.pool(lambda: nc.gpsimd.memset(QT[b][0:64, :], 0.0), [], [QTb_r[b]])
            S.pool(lambda: nc.gpsimd.memset(KT[b][0:64, :], 0.0), [], [KTc_r[b]])
            S.dma("sp", KT[b][0:16, :], self.cd["koh"], [KTc_r[b]], [KTc_r[b]])
            S.pool(lambda: nc.gpsimd.memset(VA[b][:, :, 64:128], 1.0), [], [VAo_r[b]])
        S.barrier()
        ps_s = P.rot_ps(4, [128, 512], F32)
        ps_acc = P.rot_ps(2, [128, 512], F32)
        ps_g = P.ps([128, 512], F32)
        ps_g_r = Reg(True)
        ps_b = P.rot_ps(1, [128, 1024], BF16)
        pb = P.rot_sb(6, [128, 512], BF16)
        km = P.sb([128, 16], F32)
        kmh = P.sb([128, 16], BF16)
        kml = P.sb([128, 16], BF16)
        kmt = P.sb([128, 16], F32)
        km_r = Reg()
        gm = P.sb([128, 32, 16], F32)
        gm_r = Reg()
        m8 = P.sb([128, 32, 8], F32)
        m8_r = Reg()
        sel = P.sb([128, 32, 16], F32)
        sel_r = Reg()
        bia = P.sb([128, 32, 16], BF16)
        bia_r = Reg()
        den = P.rot_sb(2, [128, 512], F32)
        bcs = P.rot_sb(2, [64, 512], F32)
        ost = P.rot_sb(2, [64, 512], BF16)
        def prep_steps(h):
            b = h % 2
            moba = h < 8
            qc, kc = (0, 1) if moba else (2, 3)
            hh = h % 8
            steps = []

            def loads():
                S.dma("sp", QT[b][64:128, :], self.qkt[qc, hh * 64:(hh + 1) * 64, :], [self.qkt_r], [QT_r[b]])
                S.dma("sp", KT[b][64:128, :], self.qkt[kc, hh * 64:(hh + 1) * 64, :], [self.qkt_r], [KT_r[b]])
                for q4 in range(4):
                    S.dma("sp", VA[b][:, q4 * 8:(q4 + 1) * 8, 0:64],
                          self.vd[q4 * 1024:(q4 + 1) * 1024, h * 64:(h + 1) * 64].rearrange("(t p) c -> p t c", p=128),
                          [self.vd_r], [VA_r[b][q4]])
            steps.append(loads)
            if not moba:
                if h in (8, 9):
                    steps.append(lambda: S.pool(lambda: nc.gpsimd.memset(QT[b][0:16, :], 0.0), [], [QTb_r[b]]))
                return steps

            def gate1():
                S.dve(lambda: nc.vector.tensor_reduce(out=km[64:128, :],
                                                      in_=KT[b][64:128, :].rearrange("p (a c) -> p a c", a=16),
                                                      axis=AX.X, op=ALU.add), [KT_r[b]], [km_r])
                S.dve(lambda: nc.vector.tensor_scalar(out=km[64:128, :], in0=km[64:128, :], scalar1=1.0 / 256.0, scalar2=None,
                                                      op0=ALU.mult), [km_r], [km_r])
                S.dve(lambda: nc.vector.tensor_copy(out=kmh[64:128, :], in_=km[64:128, :]), [km_r], [km_r])
                S.dve(lambda: nc.vector.tensor_tensor(out=kmt[64:128, :], in0=km[64:128, :], in1=kmh[64:128, :],
                                                      op=ALU.subtract), [km_r], [km_r])
                S.dve(lambda: nc.vector.tensor_copy(out=kml[64:128, :], in_=kmt[64:128, :]), [km_r], [km_r])
                for t in range(32):
                    S.pe(lambda: nc.tensor.matmul(ps_g[:, t * 16:(t + 1) * 16], lhsT=QT[b][64:128, t * 128:(t + 1) * 128],
                                                  rhs=kmh[64:128, :], start=True, stop=False), [QT_r[b], km_r], [ps_g_r])
                    S.pe(lambda: nc.tensor.matmul(ps_g[:, t * 16:(t + 1) * 16], lhsT=QT[b][64:128, t * 128:(t + 1) * 128],
                                                  rhs=kml[64:128, :], start=False, stop=True), [QT_r[b], km_r], [ps_g_r])
                gmf = gm[:].rearrange("p a b -> p (a b)")
                S.dve(lambda: nc.vector.tensor_tensor(out=gmf, in0=ps_g[:, :], in1=negv[:], op=ALU.add), [ps_g_r], [gm_r])
            steps.append(gate1)

            def gate2():
                for t in range(32):
                    S.dve(lambda: nc.vector.max(out=m8[:, t, :], in_=gm[:, t, :]), [gm_r], [m8_r])
            steps.append(gate2)

            def gate3():
                S.dve(lambda: nc.vector.tensor_tensor(out=sel[:, :, :], in0=gm[:, :, :],
                                                      in1=m8[:, :, 2:3].to_broadcast([128, 32, 16]), op=ALU.is_ge),
                      [gm_r, m8_r], [sel_r])
                self_f = sel[:].rearrange("p a b -> p (a b)")
                S.dve(lambda: nc.vector.tensor_tensor(out=self_f, in0=self_f, in1=valid[:], op=ALU.mult), [sel_r], [sel_r])
                S.dve(lambda: nc.vector.tensor_tensor(out=self_f, in0=self_f, in1=ownm1[:], op=ALU.add), [sel_r], [sel_r])
                S.dve(lambda: nc.vector.tensor_scalar(out=bia[:].rearrange("p a b -> p (a b)"), in0=self_f, scalar1=BIG,
                                                      scalar2=None, op0=ALU.mult), [sel_r], [bia_r])
            steps.append(gate3)

            def mk_tr(g4):
                def tr():
                    pbt, pbt_r = ps_b.next()
                    for tt in range(8):
                        t = g4 * 8 + tt
                        S.pe(lambda: nc.tensor.transpose(pbt[0:16, tt * 128:(tt + 1) * 128], bia[:, t, :], self.ident_bf[:]),
                             [bia_r], [pbt_r])
                    S.act(lambda: nc.scalar.copy(out=QT[b][0:16, g4 * 1024:(g4 + 1) * 1024], in_=pbt[0:16, :]),
                          [pbt_r], [QTb_r[b]])
                return tr
            for g4 in range(4):
                steps.append(mk_tr(g4))
            return steps

        def attend(h, steps):
            b = h % 2
            moba = h < 8
            lo = 0
            blocks = []
            for Q in range(8):
                ms = list(range(0, 4 * Q + 4)) if moba else list(range(max(0, 4 * Q - 16), 4 * Q + 4))
                for mi, m in enumerate(ms):
                    blocks.append((Q, mi, m, len(ms)))
            n = len(blocks)
            every = max(1, n // (len(steps) + 1)) if steps else n + 1
            sq = {}
            LOOK = 3

            def issue_S(i):
                Q, mi, m, nm = blocks[i]
                di_ = (512 * Q - 128 * m + 384) // 128
                extra = []
                if moba:
                    if di_ <= 3:
                        extra.append(cm[:, di_, :])
                elif not self.dm_lo_need[di_]:
                    extra.append(dmh[:, di_, :])
                sp_, sp_r = ps_s.next()
                S.pe(lambda: nc.tensor.matmul(sp_[:, :], lhsT=KT[b][lo:128, m * 128:(m + 1) * 128],
                                              rhs=QT[b][lo:128, Q * 512:(Q + 1) * 512], start=True, stop=(not extra)),
                     [KT_r[b], KTc_r[b], QT_r[b], QTb_r[b]], [sp_r])
                for xi, xm in enumerate(extra):
                    S.pe(lambda: nc.tensor.matmul(sp_[:, :], lhsT=self.ident_bf[:, :], rhs=xm, start=False,
                                                  stop=(xi == len(extra) - 1)), [], [sp_r])
                sq[i] = (sp_, sp_r)

            fin = []

            def fin_pe(Q, acc, acc_r, dn, dn_r):
                bp, bp_r = ps_s.next()
                S.pe(lambda: nc.tensor.matmul(bp[0:64, :], lhsT=ones_f[64:65, 0:64], rhs=dn[64:65, :], start=True, stop=True),
                     [dn_r], [bp_r])
                bc, bc_r = bcs.next()
                S.act(lambda: nc.scalar.copy(out=bc[:, :], in_=bp[0:64, :]), [bp_r], [bc_r])
                o_t, o_r = ost.next()
                S.dve(lambda: nc.vector.tensor_tensor(out=o_t[:, :], in0=acc[0:64, :], in1=bc[:, :], op=ALU.mult),
                      [acc_r, bc_r], [o_r])
                S.dma("sp", self.ot[h * 64:(h + 1) * 64, Q * 512:(Q + 1) * 512], o_t[:, :], [o_r], [self.ot_r])

            for i in range(min(LOOK, n)):
                issue_S(i)
            acc = acc_r = None
            for i in range(n):
                Q, mi, m, nm = blocks[i]
                di_ = (512 * Q - 128 * m + 384) // 128
                sp_, sp_r = sq.pop(i)
                p_t, p_r = pb.next()
                S.act(lambda: nc.scalar.activation(out=p_t[:, :], in_=sp_[:, :], func=AF.Exp, scale=0.125), [sp_r], [p_r])
                if (not moba) and self.dm_lo_need[di_]:
                    S.dve(lambda: nc.vector.tensor_tensor(out=p_t[:, :], in0=p_t[:, :], in1=dmm[:, di_, :], op=ALU.mult),
                          [p_r], [p_r])
                if i + LOOK < n:
                    issue_S(i + LOOK)
                if mi == 0:
                    acc, acc_r = ps_acc.next()
                S.pe(lambda: nc.tensor.matmul(acc[:, :], lhsT=VA[b][:, m, :], rhs=p_t[:, :],
                                              start=(mi == 0), stop=(mi == nm - 1)), [p_r, VA_r[b][m // 8], VAo_r[b]], [acc_r])
                for f in fin:
                    f[0] -= 1
                while fin and fin[0][0] <= 0:
                    f = fin.pop(0)
                    fin_pe(*f[1:])
                if mi == nm - 1:
                    dn, dn_r = den.next()
                    S.act(lambda: nc.scalar.copy(out=dn[64:65, :], in_=acc[64:65, :]), [acc_r], [dn_r])
                    S.dve(lambda: nc.vector.reciprocal(out=dn[64:65, :], in_=dn[64:65, :]), [dn_r], [dn_r])
                    fin.append([2, Q, acc, acc_r, dn, dn_r])
                if steps and (i + 1) % every == 0:
                    steps.pop(0)()
            while fin:
                f = fin.pop(0)
                fin_pe(*f[1:])
            while steps:
                steps.pop(0)()

        for st_ in prep_steps(0):
            st_()
        for h in range(16):
            nxt = prep_steps(h + 1) if h + 1 < 16 else []
            attend(h, nxt)
        P.close()


    def phase_hgrn(self, l):
        nc, S = self.nc, self.S
        j = l // 2
        P = Phase(S)
        V = nc.vector
        G_ = nc.gpsimd
        W = P.sb([128, 8, 4096], BF16)
        w_r = []
        for k in range(8):
            r = Reg()
            S.dma("pool", W[:, k, :], self.c_w_in[j, k * 128:(k + 1) * 128, :], [], [r])
            w_r.append(r)
        mg = P.sb([64, 64], F32)
        ind = P.sb([64, 3], F32)
        cmask8 = P.sb([64, 512], F32)
        cng = P.sb([64, D], F32)
        lb = P.sb([64, D], F32)
        oml = P.sb([64, D], F32)
        l0 = P.sb([64, D], F32)
        cr = Reg()
        for tl_, ap_ in ((mg, self.cd["mg"]), (ind, self.cd["ind"]), (cmask8, self.cd["cmask8"]), (cng, self.cng[j]),
                         (lb, self.lbl[1]), (l0, self.lbl[0])):
            S.dma("sp", tl_[:], ap_, [], [Reg()])
        S.barrier()
        if j == 0:
            S.dve(lambda: V.memset(lb[:], 0.0), [], [cr])
            S.dve(lambda: V.memset(oml[:], 1.0), [], [cr])
        else:
            S.dve(lambda: V.tensor_tensor(out=lb[:], in0=lb[:], in1=l0[:], op=ALU.subtract), [], [cr])
            S.act(lambda: nc.scalar.activation(out=lb[:], in_=lb[:], func=AF.Sigmoid), [cr], [cr])
            S.dve(lambda: V.tensor_scalar(out=oml[:], in0=lb[:], scalar1=-1.0, scalar2=1.0, op0=ALU.mult, op1=ALU.add),
                  [cr], [cr])
        Sst = P.sb([128, 8, 128], F32)
        Sst_r = Reg()
        S.dve(lambda: V.memset(Sst[:].rearrange("p a b -> p (a b)"), 0.0), [], [Sst_r])
        S.barrier()
        banks = P.rot_ps(3, [128, 512], F32)
        pob = P.rot_ps(2, [128, 512], F32)
        pkb = P.rot_ps(1, [128, 512], F32)
        pbf = P.rot_ps(2, [128, 1024], BF16)
        xs = P.rot_sb(3, [64, D], F32)
        xT = P.rot_sb(2, [128, 8, 128], BF16)
        for (xt_, xr_) in xT.items:
            S.pool(lambda: G_.memset(xt_[:].rearrange("p a b -> p (a b)"), 0.0), [], [xr_])
        fbuf = P.rot_sb(1, [64, D], F32)
        lfb = P.rot_sb(2, [64, D], F32)
        kkb = P.rot_sb(1, [64, D], F32)
        eGb = P.rot_sb(1, [64, D], F32)
        enGb = P.rot_sb(1, [64, D], F32)
        qtb = P.rot_sb(2, [64, D], BF16)
        ktb = P.rot_sb(3, [64, D], BF16)
        vb = P.rot_sb(3, [64, D], BF16)
        sgb = P.rot_sb(3, [64, D], F32)
        abcb = P.rot_sb(3, [128, 8, 3], F32)
        qTb = P.rot_sb(3, [128, 8, 64], BF16)
        kTb = P.rot_sb(2, [128, 8, 64], BF16)
        ATb = P.rot_sb(3, [64, 512], BF16)
        smb = P.rot_sb(2, [128, 8, 128], BF16)
        tmpb = P.rot_sb(1, [128, 8, 128], F32)
        sqb = P.rot_sb(1, [64, D], F32)
        msb = P.rot_sb(2, [64, 16], F32)
        onb = P.rot_sb(1, [64, D], F32)
        ogb = P.rot_sb(3, [64, D], BF16)
        stg = P.rot_sb(2, [128, 8, 512], BF16)
        src = self.xsrc(l)
        idf = self.ident_f
        idb = self.ident_bf

        def stage_a(t):
            x_t, x_r = xs.next()
            S.dma("sp", x_t[:], src[t * 64:(t + 1) * 64, :], [self.xres_r[t]], [x_r])
            pt, pt_r = banks.next()
            for k in range(8):
                S.pe(lambda: nc.tensor.transpose(pt[:, k * 64:(k + 1) * 64], x_t[:, k * 128:(k + 1) * 128], idf[0:64, 0:64]),
                     [x_r], [pt_r])
            xT_t, xT_r = xT.next()
            S.act(lambda: nc.scalar.copy(out=xT_t[:, :, 0:64], in_=pt[:, :].rearrange("p (a b) -> p a b", a=8)), [pt_r], [xT_r])

            def proj(cg):
                po, po_r = banks.next()
                for k in range(8):
                    S.pe(lambda: nc.tensor.matmul(po[:, :], lhsT=xT_t[:, k, :], rhs=W[:, k, cg * 512:(cg + 1) * 512],
                                                  start=(k == 0), stop=(k == 7)), [xT_r, w_r[k]], [po_r])
                return po, po_r

            f_t, f_r = fbuf.next()
            for hf in range(2):
                po, po_r = proj(2 + hf)
                S.act(lambda: nc.scalar.activation(out=f_t[:, hf * 512:(hf + 1) * 512], in_=po[0:64, :], func=AF.Sigmoid),
                      [po_r], [f_r])
            v_t, v_r = vb.next()
            for hf in range(2):
                po, po_r = proj(4 + hf)
                S.act(lambda: nc.scalar.copy(out=v_t[:, hf * 512:(hf + 1) * 512], in_=po[0:64, :]), [po_r], [v_r])
            S.dve(lambda: V.tensor_tensor(out=f_t[:], in0=f_t[:], in1=oml[:], op=ALU.mult), [f_r, cr], [f_r])
            S.dve(lambda: V.tensor_tensor(out=f_t[:], in0=f_t[:], in1=lb[:], op=ALU.add), [f_r, cr], [f_r])
            lf_t, lf_r = lfb.next()
            S.act(lambda: nc.scalar.activation(out=lf_t[:], in_=f_t[:], func=AF.Ln), [f_r], [lf_r])
            kk_t, kk_r = kkb.next()
            S.pool(lambda: G_.tensor_scalar(out=kk_t[:], in0=f_t[:], scalar1=-1.0, scalar2=1.0, op0=ALU.mult, op1=ALU.add),
                   [f_r], [kk_r])
            sg_t, sg_r = sgb.next()
            for hf in range(2):
                po, po_r = proj(6 + hf)
                S.act(lambda: nc.scalar.activation(out=sg_t[:, hf * 512:(hf + 1) * 512], in_=po[0:64, :], func=AF.Silu),
                      [po_r], [sg_r])
            eG_t, eG_r = eGb.next()
            enG_t, enG_r = enGb.next()
            for hf in range(2):
                pg_, pg_r = banks.next()
                S.pe(lambda: nc.tensor.matmul(pg_[0:64, :], lhsT=mg[:, :], rhs=lf_t[:, hf * 512:(hf + 1) * 512],
                                              start=True, stop=True), [lf_r], [pg_r])
                S.act(lambda: nc.scalar.activation(out=eG_t[:, hf * 512:(hf + 1) * 512], in_=pg_[0:64, :], func=AF.Exp),
                      [pg_r], [eG_r])
                S.act(lambda: nc.scalar.activation(out=enG_t[:, hf * 512:(hf + 1) * 512], in_=pg_[0:64, :], func=AF.Exp,
                                                   scale=-1.0), [pg_r], [enG_r])
            pst, pst_r = banks.next()
            for h in range(8):
                S.pe(lambda: nc.tensor.matmul(pst[:, h * 3:(h + 1) * 3], lhsT=lf_t[:, h * 128:(h + 1) * 128], rhs=ind[:, :],
                                              start=True, stop=True), [lf_r], [pst_r])
            abc, abc_r = abcb.next()
            S.act(lambda: nc.scalar.activation(out=abc[:].rearrange("p a b -> p (a b)"), in_=pst[:, 0:24], func=AF.Exp),
                  [pst_r], [abc_r])
            qt_t, qt_r = qtb.next()
            for hf in range(2):
                po, po_r = proj(hf)
                S.dve(lambda: V.tensor_tensor(out=qt_t[:, hf * 512:(hf + 1) * 512], in0=po[0:64, :],
                                              in1=eG_t[:, hf * 512:(hf + 1) * 512], op=ALU.mult), [po_r, eG_r], [qt_r])
            kt_t, kt_r = ktb.next()
            S.dve(lambda: V.tensor_tensor(out=kt_t[:], in0=kk_t[:], in1=enG_t[:], op=ALU.mult), [kk_r, enG_r], [kt_r])
            qT_t, qT_r = qTb.next()
            kT_t, kT_r = kTb.next()
            for (src_t, src_r, dst_t, dst_r, eng) in ((qt_t, qt_r, qT_t, qT_r, "dve"), (kt_t, kt_r, kT_t, kT_r, "act")):
                pb_, pb_r = pbf.next()
                for h in range(8):
                    S.pe(lambda: nc.tensor.transpose(pb_[:, h * 64:(h + 1) * 64], src_t[:, h * 128:(h + 1) * 128],
                                                     idb[0:64, 0:64]), [src_r], [pb_r])
                if eng == "dve":
                    S.dve(lambda: V.tensor_copy(out=dst_t[:].rearrange("p a b -> p (a b)"), in_=pb_[:, 0:512]), [pb_r], [dst_r])
                else:
                    S.act(lambda: nc.scalar.copy(out=dst_t[:].rearrange("p a b -> p (a b)"), in_=pb_[:, 0:512]), [pb_r], [dst_r])
            pat, pat_r = banks.next()
            for h in range(8):
                S.pe(lambda: nc.tensor.matmul(pat[0:64, h * 64:(h + 1) * 64], lhsT=kT_t[:, h, :], rhs=qT_t[:, h, :],
                                              start=True, stop=True), [kT_r, qT_r], [pat_r])
            AT_t, AT_r = ATb.next()
            S.dve(lambda: V.tensor_tensor(out=AT_t[:, :], in0=pat[0:64, :], in1=cmask8[:, :], op=ALU.mult), [pat_r], [AT_r])
            return (qT_t, qT_r, AT_t, AT_r, v_t, v_r, kt_t, kt_r, sg_t, sg_r, abc, abc_r)

        def stage_b(t, bufs, stg_t, stg_r):
            (qT_t, qT_r, AT_t, AT_r, v_t, v_r, kt_t, kt_r, sg_t, sg_r, abc, abc_r) = bufs
            tl = t % 8
            sm_t, sm_r = smb.next()
            S.dve(lambda: V.tensor_tensor(out=sm_t[:, :, :], in0=Sst[:, :, :],
                                          in1=abc[:, :, 0:1].to_broadcast([128, 8, 128]), op=ALU.mult),
                  [Sst_r, abc_r], [sm_r])
            po0, po0_r = pob.next()
            po1, po1_r = pob.next()
            for h in range(8):
                ob, ob_r = (po0, po0_r) if h < 4 else (po1, po1_r)
                c0 = (h % 4) * 128
                S.pe(lambda: nc.tensor.matmul(ob[0:64, c0:c0 + 128], lhsT=qT_t[:, h, :], rhs=sm_t[:, h, :],
                                              start=True, stop=False), [qT_r, sm_r], [ob_r])
                S.pe(lambda: nc.tensor.matmul(ob[0:64, c0:c0 + 128], lhsT=AT_t[:, h * 64:(h + 1) * 64],
                                              rhs=v_t[:, h * 128:(h + 1) * 128], start=False, stop=True),
                     [AT_r, v_r], [ob_r])
            tmp_t, tmp_r = tmpb.next()
            for hb in range(2):
                kb, kb_r = pkb.next()
                for hh in range(4):
                    h = hb * 4 + hh
                    S.pe(lambda: nc.tensor.matmul(kb[:, hh * 128:(hh + 1) * 128], lhsT=kt_t[:, h * 128:(h + 1) * 128],
                                                  rhs=v_t[:, h * 128:(h + 1) * 128], start=True, stop=True),
                         [kt_r, v_r], [kb_r])
                S.dve(lambda: V.tensor_tensor(out=tmp_t[:, hb * 4:(hb + 1) * 4, :],
                                              in0=kb[:, :].rearrange("p (a b) -> p a b", a=4),
                                              in1=abc[:, hb * 4:(hb + 1) * 4, 2:3].to_broadcast([128, 4, 128]), op=ALU.mult),
                      [kb_r, abc_r], [tmp_r])
            S.dve(lambda: V.tensor_tensor(out=Sst[:, :, :], in0=Sst[:, :, :],
                                          in1=abc[:, :, 1:2].to_broadcast([128, 8, 128]), op=ALU.mult),
                  [Sst_r, abc_r], [Sst_r])
            S.dve(lambda: V.tensor_tensor(out=Sst[:, :, :], in0=Sst[:, :, :], in1=tmp_t[:, :, :], op=ALU.add),
                  [Sst_r, tmp_r], [Sst_r])
            sq_t, sq_r = sqb.next()
            for hb, (ob, ob_r) in enumerate(((po0, po0_r), (po1, po1_r))):
                S.act(lambda: nc.scalar.activation(out=sq_t[:, hb * 512:(hb + 1) * 512], in_=ob[0:64, :], func=AF.Square),
                      [ob_r], [sq_r])
            ms_t, ms_r = msb.next()
            S.dve(lambda: V.tensor_reduce(out=ms_t[:, 0:8], in_=sq_t[:].rearrange("p (a b) -> p a b", a=8), axis=AX.X,
                                          op=ALU.add), [sq_r], [ms_r])
            S.dve(lambda: V.tensor_scalar(out=ms_t[:, 0:8], in0=ms_t[:, 0:8], scalar1=1.0 / 128.0, scalar2=RMS_EPS,
                                          op0=ALU.mult, op1=ALU.add), [ms_r], [ms_r])
            S.act(lambda: nc.scalar.activation(out=ms_t[:, 0:8], in_=ms_t[:, 0:8], func=AF.Ln), [ms_r], [ms_r])
            S.act(lambda: nc.scalar.activation(out=ms_t[:, 8:16], in_=ms_t[:, 0:8], func=AF.Exp, scale=-0.5), [ms_r], [ms_r])
            on_t, on_r = onb.next()
            for hb, (ob, ob_r) in enumerate(((po0, po0_r), (po1, po1_r))):
                S.dve(lambda: V.tensor_tensor(out=on_t[:, hb * 512:(hb + 1) * 512].rearrange("p (a b) -> p a b", a=4),
                                              in0=ob[0:64, :].rearrange("p (a b) -> p a b", a=4),
                                              in1=ms_t[:, 8 + hb * 4:8 + (hb + 1) * 4].unsqueeze(2).to_broadcast([64, 4, 128]),
                                              op=ALU.mult), [ob_r, ms_r], [on_r])
            S.pool(lambda: G_.tensor_tensor(out=on_t[:], in0=on_t[:], in1=cng[:], op=ALU.mult), [on_r], [on_r])
            og_t, og_r = ogb.next()
            S.pool(lambda: G_.tensor_tensor(out=og_t[:], in0=on_t[:], in1=sg_t[:], op=ALU.mult), [on_r, sg_r], [og_r])
            return (t, og_t, og_r)

        stg_cur = {}

        def stage_c(args):
            t, og_t, og_r = args
            tl = t % 8
            if tl == 0:
                stg_cur["t"], stg_cur["r"] = stg.next()
            stg_t, stg_r = stg_cur["t"], stg_cur["r"]
            pb_, pb_r = pbf.next()
            for h in range(8):
                S.pe(lambda: nc.tensor.transpose(pb_[:, h * 64:(h + 1) * 64], og_t[:, h * 128:(h + 1) * 128], idb[0:64, 0:64]),
                     [og_r], [pb_r])
            S.act(lambda: nc.scalar.copy(out=stg_t[:, :, tl * 64:(tl + 1) * 64],
                                         in_=pb_[:, 0:512].rearrange("p (a b) -> p a b", a=8)), [pb_r], [stg_r])
            if tl == 7:
                g = t // 8
                for k in range(8):
                    S.dma("act", self.ot[k * 128:(k + 1) * 128, g * 512:(g + 1) * 512], stg_t[:, k, :], [stg_r], [self.ot_r])

        nt = T // 64
        pend = stage_a(0)
        pend_c = None
        for t in range(nt):
            nxt = stage_a(t + 1) if t + 1 < nt else None
            if pend_c is not None:
                stage_c(pend_c)
            pend_c = stage_b(t, pend, None, None)
            pend = nxt
        stage_c(pend_c)
        P.close()

    def phase_out_ln_router(self, l, w_out_ap):
        nc, S = self.nc, self.S
        V = nc.vector
        P = Phase(S)
        W = P.sb([128, 8, D], BF16)
        w_r = []
        for k in range(8):
            r = Reg()
            S.dma("pool", W[:, k, :], w_out_ap[k * 128:(k + 1) * 128, :], [], [r])
            w_r.append(r)
        g_bc = P.sb([128, D], F32)
        b_bc = P.sb([128, D], F32)
        rw = P.sb([128, 8, 36], F32)
        rb = P.sb([128, 36], F32)
        u_bf = P.sb([128, 128], BF16)
        ones_bf = P.sb([128, 128], BF16)
        ecb = P.sb([128, 128], F32)
        carry = P.sb([128, 32], F32)
        pid = P.sb([128, 8], F32)
        S.dma("sp", pid[:], self.cd["pid"], [], [Reg()])
        for tl_, ap_ in ((g_bc, self.lng[l * 2]), (b_bc, self.lnb[l * 2]), (rw, self.rw[l].rearrange("(k p) n -> p k n", p=128)),
                         (rb, self.rb[l]), (u_bf, self.cd["u_bf"]), (ones_bf, self.cd["ones_bf"]), (ecb, self.cd["ecb"])):
            S.dma("sp", tl_[:], ap_, [], [Reg()])
        car_r = Reg()
        S.dve(lambda: V.memset(carry[:], 0.0), [], [car_r])
        S.barrier()
        gb_r = Reg()
        oT = Rot([(P.sb([128, 8, 512], BF16), [Reg() for _ in range(8)]) for _ in range(2)])
        xs = P.rot_sb(2, [128, D], F32)
        ys = P.rot_sb(2, [128, D], F32)
        xa = P.rot_sb(2, [128, D], F32)
        xbf = P.rot_sb(8, [128, D], BF16)
        ps_m = P.rot_ps(2, [128, 1024], F32)
        ps_t = P.rot_ps(1, [128, 1024], F32)
        ps_r = P.rot_ps(2, [128, 512], F32)
        xTf = P.rot_sb(2, [128, 8, 128], F32)
        st = P.sb([128, 12], F32)
        mv = P.sb([128, 2], F32)
        rs = P.sb([128, 1], F32)
        scr = (st, Reg(), mv, Reg(), rs, Reg())
        lgs = P.rot_sb(2, [128, 4, 36], F32)
        sm = P.sb([128, 16, 4], F32)
        oh = P.sb([128, 4, 4], F32)
        eg = P.sb([128, 4, 4], F32)
        lem = P.sb([128, 4, 32], F32)
        o1 = P.sb([128, 4, 32], F32)
        o2 = P.sb([128, 4, 32], F32)
        mbf = P.sb([128, 4, 32], BF16)
        rf = P.sb([128, 4, 32], F32)
        tmp = P.sb([128, 4, 32], F32)
        rk = P.sb([128, 4, 2], F32)
        bs = P.sb([128, 4, 2], F32)
        ov = P.sb([128, 4, 2], F32)
        ps_ = P.sb([128, 4, 2], F32)
        pd = P.sb([128, 4, 2], F32)
        possi = P.rot_sb(2, [128, 4, 2], I32)
        rr = Reg()
        src = self.xsrc(l)
        f3 = lambda t: t[:].rearrange("p a b -> p (a b)")
        for g in range(8):
            oT_t, o_regs = oT.next()
            for k in range(8):
                S.dma("sp", oT_t[:, k, :], self.ot[k * 128:(k + 1) * 128, g * 512:(g + 1) * 512], [self.ot_r], [o_regs[k]])
            lg, lg_r = lgs.next()
            xb_list = []
            for tl in range(4):
                t = g * 4 + tl
                x_t, x_r = xs.next()
                S.dma("sp", x_t[:], src[t * 128:(t + 1) * 128, :], [self.xres_r[2 * t], self.xres_r[2 * t + 1]], [x_r])
                pm, pm_r = ps_m.next()
                for hf in range(2):
                    for k in range(8):
                        S.pe(lambda: nc.tensor.matmul(pm[:, hf * 512:(hf + 1) * 512], lhsT=oT_t[:, k, tl * 128:(tl + 1) * 128],
                                                      rhs=W[:, k, hf * 512:(hf + 1) * 512], start=(k == 0), stop=(k == 7)),
                             [o_regs[k], w_r[k]], [pm_r])
                y_t, y_r = ys.next()
                for hf in range(2):
                    S.dve(lambda: V.scalar_tensor_tensor(out=y_t[:, hf * 512:(hf + 1) * 512],
                                                         in0=x_t[:, hf * 512:(hf + 1) * 512], scalar=ALPHA,
                                                         in1=pm[:, hf * 512:(hf + 1) * 512], op0=ALU.mult, op1=ALU.add),
                          [x_r, pm_r], [y_r])
                xa_t, xa_r = xa.next()
                self.layer_norm_tile(P, 128, y_t, y_r, g_bc, b_bc, gb_r, xa_t, xa_r, scr)
                S.dma("pool", self.xres[t * 128:(t + 1) * 128, :], xa_t[:, :], [xa_r],
                      [self.xres_r[2 * t], self.xres_r[2 * t + 1]])
                xb_t, xb_r = xbf.next()
                S.act(lambda: nc.scalar.copy(out=xb_t[:, :], in_=xa_t[:, :]), [xa_r], [xb_r])
                xb_list.append((xb_t, xb_r))
                pt, pt_r = ps_t.next()
                for k in range(8):
                    S.pe(lambda: nc.tensor.transpose(pt[:, k * 128:(k + 1) * 128], xa_t[:, k * 128:(k + 1) * 128],
                                                     self.ident_f[:]), [xa_r], [pt_r])
                xf, xf_r = xTf.next()
                S.act(lambda: nc.scalar.copy(out=xf[:, 0:4, :], in_=pt[:, 0:512].rearrange("p (a b) -> p a b", a=4)),
                      [pt_r], [xf_r])
                S.dve(lambda: V.tensor_copy(out=xf[:, 4:8, :], in_=pt[:, 512:1024].rearrange("p (a b) -> p a b", a=4)),
                      [pt_r], [xf_r])
                pr, pr_r = ps_r.next()
                for k in range(8):
                    S.pe(lambda: nc.tensor.matmul(pr[:, 0:36], lhsT=xf[:, k, :], rhs=rw[:, k, :], start=(k == 0), stop=(k == 7)),
                         [xf_r], [pr_r])
                S.dve(lambda: V.tensor_tensor(out=lg[:, tl, :], in0=pr[:, 0:36], in1=rb[:, :], op=ALU.add), [pr_r], [lg_r])
            R_ = [lg_r, rr]
            LG = lg[:, :, 0:4]
            LE = lg[:, :, 4:36]
            bc4 = lambda j: sm[:, j, :].unsqueeze(2).to_broadcast([128, 4, 4])
            bc32 = lambda j: sm[:, j, :].unsqueeze(2).to_broadcast([128, 4, 32])
            S.dve(lambda: V.tensor_reduce(out=sm[:, 0, :], in_=LG, axis=AX.X, op=ALU.max), R_, [rr])
            S.dve(lambda: V.tensor_tensor(out=oh[:, :, :], in0=LG, in1=bc4(0), op=ALU.is_ge), R_, [rr])
            S.dve(lambda: V.tensor_tensor(out=eg[:, :, :], in0=LG, in1=bc4(0), op=ALU.subtract), R_, [rr])
            S.act(lambda: nc.scalar.activation(out=f3(eg), in_=f3(eg), func=AF.Exp), [rr], [rr])
            S.dve(lambda: V.tensor_reduce(out=sm[:, 1, :], in_=eg[:, :, :], axis=AX.X, op=ALU.add), [rr], [rr])
            S.dve(lambda: V.reciprocal(out=sm[:, 2, :], in_=sm[:, 1, :]), [rr], [rr])
            S.dve(lambda: V.tensor_scalar(out=f3(oh), in0=f3(oh), scalar1=-1.0, scalar2=BIG, op0=ALU.add, op1=ALU.mult),
                  [rr], [rr])
            S.dve(lambda: V.tensor_tensor(out=lem[:].rearrange("p a (g e) -> p (a g) e", g=4),
                                          in0=LE.rearrange("p a (g e) -> p a g e", g=4),
                                          in1=oh[:, :, :].unsqueeze(3).to_broadcast([128, 4, 4, 8]), op=ALU.add)
                  if False else
                  V.tensor_tensor(out=lem[:, :, :].rearrange("p a (g e) -> p a g e", g=4),
                                  in0=LE.rearrange("p a (g e) -> p a g e", g=4),
                                  in1=oh[:, :, :].unsqueeze(3).to_broadcast([128, 4, 4, 8]), op=ALU.add), R_, [rr])
            S.dve(lambda: V.tensor_reduce(out=sm[:, 3, :], in_=lem[:, :, :], axis=AX.X, op=ALU.max), [rr], [rr])
            S.dve(lambda: V.tensor_tensor(out=o1[:, :, :], in0=lem[:, :, :], in1=bc32(3), op=ALU.is_ge), [rr], [rr])
            S.dve(lambda: V.scalar_tensor_tensor(out=f3(lem), in0=f3(o1), scalar=-BIG, in1=f3(lem), op0=ALU.mult, op1=ALU.add),
                  [rr], [rr])
            S.dve(lambda: V.tensor_reduce(out=sm[:, 4, :], in_=lem[:, :, :], axis=AX.X, op=ALU.max), [rr], [rr])
            S.dve(lambda: V.tensor_tensor(out=o2[:, :, :], in0=lem[:, :, :], in1=bc32(4), op=ALU.is_ge), [rr], [rr])
            S.dve(lambda: V.tensor_tensor(out=sm[:, 5, :], in0=sm[:, 4, :], in1=sm[:, 3, :], op=ALU.subtract), [rr], [rr])
            S.act(lambda: nc.scalar.activation(out=sm[:, 6, :], in_=sm[:, 5, :], func=AF.Exp), [rr], [rr])
            S.dve(lambda: V.tensor_scalar(out=sm[:, 7, :], in0=sm[:, 6, :], scalar1=1.0, scalar2=None, op0=ALU.add), [rr], [rr])
            S.dve(lambda: V.reciprocal(out=sm[:, 8, :], in_=sm[:, 7, :]), [rr], [rr])
            pgr = self.pg_r[g]
            S.dve(lambda: V.tensor_tensor(out=self.g12[:, g * 4:(g + 1) * 4, 0], in0=sm[:, 8, :], in1=sm[:, 2, :], op=ALU.mult),
                  [rr], [pgr])
            S.dve(lambda: V.tensor_tensor(out=self.g12[:, g * 4:(g + 1) * 4, 1], in0=self.g12[:, g * 4:(g + 1) * 4, 0],
                                          in1=sm[:, 6, :], op=ALU.mult), [rr, pgr], [pgr])
            S.dve(lambda: V.tensor_tensor(out=f3(mbf), in0=f3(o1), in1=f3(o2), op=ALU.add), [rr], [rr])
            pp, pp_r = ps_r.next()
            S.pe(lambda: nc.tensor.matmul(pp[:, 0:128], lhsT=u_bf[:, :], rhs=f3(mbf), start=True, stop=True), [rr], [pp_r])
            S.pe(lambda: nc.tensor.matmul(pp[:, 128:256], lhsT=ones_bf[:, :], rhs=f3(mbf), start=True, stop=True), [rr], [pp_r])
            for tl in range(4):
                S.dve(lambda: V.tensor_tensor(out=rf[:, tl, :], in0=pp[:, tl * 32:(tl + 1) * 32], in1=carry[:, :], op=ALU.add),
                      [pp_r, car_r], [rr])
                S.dve(lambda: V.tensor_tensor(out=carry[:, :], in0=carry[:, :], in1=pp[:, 128 + tl * 32:128 + (tl + 1) * 32],
                                              op=ALU.add), [pp_r, car_r], [car_r])
            for kk, ok in enumerate((o1, o2)):
                S.dve(lambda: V.tensor_tensor(out=f3(tmp), in0=f3(ok), in1=f3(rf), op=ALU.mult), [rr], [rr])
                S.dve(lambda: V.tensor_reduce(out=rk[:, :, kk], in_=tmp[:, :, :], axis=AX.X, op=ALU.add), [rr], [rr])
                S.dve(lambda: V.tensor_tensor(out=f3(tmp), in0=f3(ok), in1=ecb[:, :], op=ALU.mult), [rr], [rr])
                S.dve(lambda: V.tensor_reduce(out=bs[:, :, kk], in_=tmp[:, :, :], axis=AX.X, op=ALU.add), [rr], [rr])
            S.dve(lambda: V.tensor_scalar(out=f3(ov), in0=f3(rk), scalar1=float(CAP), scalar2=None, op0=ALU.is_ge), [rr], [rr])
            S.dve(lambda: V.tensor_tensor(out=f3(ps_), in0=f3(rk), in1=f3(bs), op=ALU.add), [rr], [rr])
            S.dve(lambda: V.tensor_tensor(out=f3(pd), in0=pid[:, :], in1=f3(ps_), op=ALU.subtract), [rr], [rr])
            S.dve(lambda: V.tensor_tensor(out=f3(pd), in0=f3(pd), in1=f3(ov), op=ALU.mult), [rr], [rr])
            S.dve(lambda: V.tensor_tensor(out=f3(ps_), in0=f3(ps_), in1=f3(pd), op=ALU.add), [rr], [rr])
            S.dve(lambda: V.tensor_copy(out=self.posg[:, g * 4:(g + 1) * 4, :].rearrange("p a b -> p (a b)"), in_=f3(ps_)),
                  [rr], [pgr])
            for tl in range(4):
                xb_t, xb_r = xb_list[tl]
                for kk in range(2):
                    S.idma(self.xs[:, :], bass.IndirectOffsetOnAxis(ap=self.posg[:, g * 4 + tl, kk:kk + 1], axis=0), xb_t[:, :],
                           None, None, [xb_r, pgr], [self.xs_r])
        P.close()

    def phase_moe(self, l, last):
        nc, S = self.nc, self.S
        V = nc.vector
        P = Phase(S)
        g_bc = P.sb([128, D], F32)
        b_bc = P.sb([128, D], F32)
        S.dma("sp", g_bc[:], self.lng[l * 2 + 1], [], [Reg()])
        S.dma("sp", b_bc[:], self.lnb[l * 2 + 1], [], [Reg()])
        S.barrier()
        gb_r = Reg()
        W1 = P.rot_sb(2, [128, 8, 512], BF16)
        W3 = P.rot_sb(2, [128, 8, 512], BF16)
        W2 = P.rot_sb(2, [128, 4, D], BF16)
        S3 = P.rot_sb(1, [128, 8, 512], F32)
        S2 = P.rot_sb(1, [128, 4, D], F32)
        xsl = P.rot_sb(2, [128, 4, D], BF16)
        xgT = P.rot_sb(2, [128, 8, 512], BF16)
        ps_tr = P.rot_ps(2, [128, 1024], BF16)
        ps_h1 = P.rot_ps(2, [128, 512], F32)
        ps_h3 = P.rot_ps(2, [128, 512], F32)
        ps_y = P.rot_ps(2, [128, 512], F32)
        sl = P.rot_sb(2, [128, 512], F32)
        hT = P.rot_sb(2, [128, 4, 512], BF16)
        ysb = P.rot_sb(3, [128, D], F32)
        NB = CAP // 128
        fl = lambda t_: t_[:].rearrange("p a b -> p (a b)")
        wq = {}

        def fetch(e):
            w1_t, w1_r = W1.next()
            w3_t, w3_r = W3.next()
            w2_t, w2_r = W2.next()
            s3_t, s3_r = S3.next()
            s2_t, s2_r = S2.next()
            S.dma("pool", w1_t[:], self.w1[l, e].rearrange("(k p) f -> p k f", p=128), [], [w1_r])
            S.dma("sp", s3_t[:], self.w3[l, e].rearrange("(k p) f -> p k f", p=128), [], [s3_r])
            S.dma("sp", s2_t[:], self.w2[l, e].rearrange("(k p) f -> p k f", p=128), [], [s2_r])
            xs_t, xs_r = xsl.next()
            S.dma("sp", xs_t[:, 0:NB, :], self.xs[e * CAP:(e + 1) * CAP, :].rearrange("(b p) d -> p b d", p=128),
                  [self.xs_r], [xs_r])
            wq[e] = (w1_t, w1_r, w3_t, w3_r, w2_t, w2_r, s3_t, s3_r, s2_t, s2_r, xs_t, xs_r)

        def cast(e):
            (w1_t, w1_r, w3_t, w3_r, w2_t, w2_r, s3_t, s3_r, s2_t, s2_r, xs_t, xs_r) = wq[e]
            S.act(lambda: nc.scalar.copy(out=fl(w3_t), in_=fl(s3_t)), [s3_r], [w3_r])
            S.dve(lambda: V.tensor_copy(out=fl(w2_t), in_=fl(s2_t)), [s2_r], [w2_r])

        fetch(0)
        cast(0)
        for e in range(32):
            (w1_t, w1_r, w3_t, w3_r, w2_t, w2_r, s3_t, s3_r, s2_t, s2_r, xs_t, xs_r) = wq.pop(e)
            if e + 1 < 32:
                fetch(e + 1)
            xg, xg_r = xgT.next()
            for k2 in range(4):
                ptr, ptr_r = ps_tr.next()
                for kk in range(2):
                    k = k2 * 2 + kk
                    for bb in range(NB):
                        S.pe(lambda: nc.tensor.transpose(ptr[:, kk * 512 + bb * 128:kk * 512 + (bb + 1) * 128],
                                                         xs_t[:, bb, k * 128:(k + 1) * 128], self.ident_bf[:]), [xs_r], [ptr_r])
                if k2 % 2 == 0:
                    S.act(lambda: nc.scalar.copy(out=xg[:, k2 * 2:k2 * 2 + 2, :].rearrange("p a b -> p (a b)"), in_=ptr[:, :]),
                          [ptr_r], [xg_r])
                else:
                    S.dve(lambda: V.tensor_copy(out=xg[:, k2 * 2:k2 * 2 + 2, :].rearrange("p a b -> p (a b)"), in_=ptr[:, :]),
                          [ptr_r], [xg_r])
            h_t, h_r = hT.next()
            for fc in range(4):
                p1, p1_r = ps_h1.next()
                p3, p3_r = ps_h3.next()
                for k in range(8):
                    S.pe(lambda: nc.tensor.matmul(p1[:, :], lhsT=w1_t[:, k, fc * 128:(fc + 1) * 128], rhs=xg[:, k, :],
                                                  start=(k == 0), stop=(k == 7)), [w1_r, xg_r], [p1_r])
                for k in range(8):
                    S.pe(lambda: nc.tensor.matmul(p3[:, :], lhsT=w3_t[:, k, fc * 128:(fc + 1) * 128], rhs=xg[:, k, :],
                                                  start=(k == 0), stop=(k == 7)), [w3_r, xg_r], [p3_r])
                s_t, s_r = sl.next()
                S.act(lambda: nc.scalar.activation(out=s_t[:, :], in_=p1[:, :], func=AF.Silu), [p1_r], [s_r])
                S.dve(lambda: V.tensor_tensor(out=h_t[:, fc, :], in0=p3[:, :], in1=s_t[:, :], op=ALU.mult), [p3_r, s_r], [h_r])
            for st_ in range(NB):
                y_t, y_r = ysb.next()
                for dh in range(2):
                    py, py_r = ps_y.next()
                    for fc in range(4):
                        S.pe(lambda: nc.tensor.matmul(py[:, :], lhsT=h_t[:, fc, st_ * 128:(st_ + 1) * 128],
                                                      rhs=w2_t[:, fc, dh * 512:(dh + 1) * 512], start=(fc == 0), stop=(fc == 3)),
                             [h_r, w2_r], [py_r])
                    if dh == 0:
                        S.act(lambda: nc.scalar.copy(out=y_t[:, 0:512], in_=py[:, :]), [py_r], [y_r])
                    else:
                        S.dve(lambda: V.tensor_copy(out=y_t[:, 512:1024], in_=py[:, :]), [py_r], [y_r])
                S.dma("sp", self.yb[e * CAP + st_ * 128:e * CAP + (st_ + 1) * 128, :], y_t[:, :], [y_r], [self.yb_r])
            if e + 1 < 32:
                cast(e + 1)
        P.close()
        P = Phase(S)
        g_bc = P.sb([128, D], F32)
        b_bc = P.sb([128, D], F32)
        S.dma("sp", g_bc[:], self.lng[l * 2 + 1], [], [Reg()])
        S.dma("sp", b_bc[:], self.lnb[l * 2 + 1], [], [Reg()])
        S.barrier()
        xs2 = P.rot_sb(3, [128, D], F32)
        ya = P.rot_sb(4, [128, D], F32)
        ybb = P.rot_sb(4, [128, D], F32)
        ys = P.rot_sb(2, [128, D], F32)
        xo = P.rot_sb(2, [128, D], F32)
        st = P.sb([128, 12], F32)
        mv = P.sb([128, 2], F32)
        rs = P.sb([128, 1], F32)
        scr = (st, Reg(), mv, Reg(), rs, Reg())
        dst = self.out if last else self.xres
        for t in range(32):
            pgr = self.pg_r[t // 4]
            x_t, x_r = xs2.next()
            S.dma("sp", x_t[:], self.xres[t * 128:(t + 1) * 128, :], [self.xres_r[2 * t], self.xres_r[2 * t + 1]], [x_r])
            a_t, a_r = ya.next()
            b_t, b_r = ybb.next()
            S.idma(a_t[:, :], None, self.yb[:, :], bass.IndirectOffsetOnAxis(ap=self.posg[:, t, 0:1], axis=0), NS + 127,
                   [self.yb_r, pgr], [a_r])
            S.idma(b_t[:, :], None, self.yb[:, :], bass.IndirectOffsetOnAxis(ap=self.posg[:, t, 1:2], axis=0), NS + 127,
                   [self.yb_r, pgr], [b_r])
            y_t, y_r = ys.next()
            S.act(lambda: nc.scalar.activation(out=y_t[:, :], in_=x_t[:, :], func=AF.Copy, scale=ALPHA), [x_r], [y_r])
            S.dve(lambda: V.scalar_tensor_tensor(out=y_t[:, :], in0=a_t[:, :], scalar=self.g12[:, t, 0:1], in1=y_t[:, :],
                                                 op0=ALU.mult, op1=ALU.add), [a_r, pgr, y_r], [y_r])
            S.dve(lambda: V.scalar_tensor_tensor(out=y_t[:, :], in0=b_t[:, :], scalar=self.g12[:, t, 1:2], in1=y_t[:, :],
                                                 op0=ALU.mult, op1=ALU.add), [b_r, pgr, y_r], [y_r])
            xo_t, xo_r = xo.next()
            self.layer_norm_tile(P, 128, y_t, y_r, g_bc, b_bc, gb_r, xo_t, xo_r, scr, tail="dve")
            S.dma("sp", dst[t * 128:(t + 1) * 128, :], xo_t[:, :], [xo_r], [self.xres_r[2 * t], self.xres_r[2 * t + 1]])
        P.close()

    def dump(self, src_ap, shape, dt):
        nc, S = self.nc, self.S
        dbg = nc.dram_tensor("dbg", list(shape), dt, kind="ExternalOutput").ap()
        S.barrier()
        n = shape[0] // 128
        for i in range(n):
            S.dma("sp", dbg[i * 128:(i + 1) * 128, :], src_ap[i * 128:(i + 1) * 128, :], [], [Reg()])
        S.barrier()

    def init_scratch(self):
        nc, S = self.nc, self.S
        P = Phase(S)
        zb = P.sb([128, 8, D], BF16)
        zf = P.sb([128, D], F32)
        r = Reg()
        S.dve(lambda: nc.vector.memset(zb[:].rearrange("p a b -> p (a b)"), 0.0), [], [r])
        S.dve(lambda: nc.vector.memset(zf[:], 0.0), [], [r])
        for i in range(NS // 1024):
            S.dma("sp", self.xs[i * 1024:(i + 1) * 1024, :].rearrange("(p a) d -> p a d", p=128), zb[:, :, :], [r], [Reg()])
        S.dma("sp", self.xs[NS:NS + 128, :], zb[:, 0, :], [r], [Reg()])
        S.dma("sp", self.yb[NS:NS + 128, :], zf[:, :], [r], [Reg()])
        P.close()

    def build(self):
        dbg = self.debug
        self.init_scratch()
        for l in range(self.nlayers):
            if l % 2 == 0:
                self.phase_attn_proj(l)
                if dbg == "qkt%d" % l:
                    self.dump(self.qkt.rearrange("a b c -> (a b) c"), [2048, T], BF16)
                    return self.nc
                self.phase_attn(l)
                if dbg == "ot%d" % l:
                    self.dump(self.ot, [D, T], BF16)
                    return self.nc
                self.phase_out_ln_router(l, self.ab_w_out[l // 2])
            else:
                self.phase_hgrn(l)
                if dbg == "ot%d" % l:
                    self.dump(self.ot, [D, T], BF16)
                    return self.nc
                self.phase_out_ln_router(l, self.c_w_out[l // 2])
            if dbg == "xa%d" % l:
                self.dump(self.xres, [T, D], F32)
                return self.nc
            self.phase_moe(l, last=(l == self.nlayers - 1))
        self.S.barrier()
        return self.nc


def host_inputs(inputs):
    f = lambda a: np.ascontiguousarray(np.asarray(a, dtype=np.float32))
    d = {}
    d["ab_w_in"] = f(inputs["ab_w_in"])
    d["ab_w_out"] = f(inputs["ab_w_out"])
    d["c_w_in"] = f(inputs["c_w_in"])
    d["c_w_out"] = f(inputs["c_w_out"])
    d["exp_w1"] = f(inputs["exp_w1"])
    d["exp_w3"] = f(inputs["exp_w3"])
    d["exp_w2"] = f(inputs["exp_w2"])
    cng = np.tile(f(inputs["c_norm_g"]), (1, 8))
    d["cng"] = np.ascontiguousarray(np.broadcast_to(cng[:, None, :], (2, 64, D)))
    d["lbl"] = np.ascontiguousarray(np.broadcast_to(f(inputs["hgrn_lb_logits"])[:, None, :], (2, 64, D)))
    d["lng"] = np.ascontiguousarray(np.broadcast_to(f(inputs["ln_g"]).reshape(8, 1, D), (8, 128, D)))
    d["lnb"] = np.ascontiguousarray(np.broadcast_to(f(inputs["ln_b"]).reshape(8, 1, D), (8, 128, D)))
    d["rw"] = np.ascontiguousarray(np.concatenate([f(inputs["router_g_w"]), f(inputs["router_e_w"])], axis=2))
    rb = np.concatenate([f(inputs["router_g_b"]), f(inputs["router_e_b"])], axis=1)
    d["rb"] = np.ascontiguousarray(np.broadcast_to(rb[:, None, :], (4, 128, 36)))
    return d


_CACHE = {}


def kernel(**inputs):
    x = np.ascontiguousarray(np.asarray(inputs["x"], dtype=np.float32))
    if "prog" not in _CACHE:
        p = Prog()
        p.build()
        _CACHE["prog"] = p
    p = _CACHE["prog"]
    shared = host_inputs(inputs)
    for k, v in p.consts_np.items():
        shared["c_" + k] = v
    in_maps = []
    for c in range(8):
        m = dict(shared)
        m["x"] = x[c]
        in_maps.append(m)
    res = run_bass_kernel_spmd(p.nc, in_maps, core_ids=list(range(8)))
    return np.stack([np.asarray(r["out"], dtype=np.float32) for r in res.results], axis=0)
```

```python
import contextlib
import os
LVL = int(os.environ.get('DBG_LVL', '9'))
import numpy as np
import ml_dtypes
import concourse.bass as bass
import concourse.mybir as mybir
from concourse.bass_utils import run_bass_kernel_spmd

F32 = mybir.dt.float32
BF16 = mybir.dt.bfloat16
AF = mybir.ActivationFunctionType
ALU = mybir.AluOpType
AX = mybir.AxisListType

T = 4096
D = 1024
DEPTH = 4
ALPHA = float((2.0 * DEPTH) ** 0.25)
LN_EPS = 1e-5
RMS_EPS = 1e-6
BIG = 30000.0
EPOCH = 30000
NSLOT = 8
INORDER = tuple(x for x in os.environ.get('INORDER', '').split(',') if x)
CAP = 512
NS = 32 * CAP
I32 = mybir.dt.int32


class Reg:
    __slots__ = ("w", "r", "x")

    def __init__(self, x=False):
        self.w = {}
        self.r = {}
        self.x = x


class Eng:
    def __init__(self, name, e, is_pe=False):
        self.name = name
        self.e = e
        self.count = 0
        self.known = {}
        self.is_pe = is_pe
        self.dcount = 0


class Sched:
    def __init__(self, nc):
        self.nc = nc
        self.E = {
            "pe": Eng("pe", nc.tensor, True),
            "act": Eng("act", nc.scalar),
            "dve": Eng("dve", nc.vector),
            "pool": Eng("pool", nc.gpsimd),
            "sp": Eng("sp", nc.sync),
        }
        self.semh = {}
        self.nsem = 0

    def _sem(self, key):
        h = self.semh.get(key)
        if h is None:
            h = self.nc.alloc_semaphore("s_%s_%s_%d" % key)
            self.semh[key] = h
            self.nsem += 1
        return h

    def _waits(self, E, reads, writes):
        deps = {}
        for r in reads:
            for k, v in r.w.items():
                if deps.get(k, 0) < v:
                    deps[k] = v
            if r.x:
                for k, v in r.r.items():
                    if deps.get(k, 0) < v:
                        deps[k] = v
        for w in writes:
            for k, v in w.w.items():
                if deps.get(k, 0) < v:
                    deps[k] = v
            for k, v in w.r.items():
                if deps.get(k, 0) < v:
                    deps[k] = v
        for k, v in deps.items():
            if k[1] == "c" and k[0] == E.name and (E.is_pe or E.name in INORDER):
                continue
            if E.known.get(k, 0) < v:
                E.e.wait_ge(self._sem(k), v)
                E.known[k] = v

    def op(self, en, fn, reads, writes):
        E = self.E[en]
        self._waits(E, reads, writes)
        ins = fn()
        key = (en, "c", E.count // EPOCH)
        val = E.count % EPOCH + 1
        ins.then_inc(self._sem(key), 1)
        E.count += 1
        for r in reads:
            if r.x:
                r.w = {key: val}
                r.r = {}
            else:
                r.r[key] = val
        for w in writes:
            w.w = {key: val}
            w.r = {}

    def pe(self, fn, reads, writes):
        self.op("pe", fn, reads, writes)

    def act(self, fn, reads, writes):
        self.op("act", fn, reads, writes)

    def dve(self, fn, reads, writes):
        self.op("dve", fn, reads, writes)

    def pool(self, fn, reads, writes):
        self.op("pool", fn, reads, writes)

    def dma(self, qn, out, in_, reads, writes):
        Q = self.E[qn]
        self._waits(Q, reads, writes)
        i = Q.dcount
        slot = i % NSLOT
        tgt = 16 * (i // NSLOT + 1)
        assert tgt < 60000
        key = (qn, "d", slot)
        if i >= NSLOT and Q.known.get(key, 0) < tgt - 16:
            Q.e.wait_ge(self._sem(key), tgt - 16)
            Q.known[key] = tgt - 16
        Q.e.dma_start(out=out, in_=in_).then_inc(self._sem(key), 16)
        Q.dcount += 1
        for r in reads:
            if r.r.get(key, 0) < tgt:
                r.r[key] = tgt
        for w in writes:
            w.w = {key: tgt}
            w.r = {}

    def idma(self, out, out_off, in_, in_off, bound, reads, writes):
        Q = self.E["pool"]
        self._waits(Q, reads, writes)
        i = Q.dcount
        slot = i % NSLOT
        tgt = 16 * (i // NSLOT + 1)
        assert tgt < 60000
        key = ("pool", "d", slot)
        if i >= NSLOT and Q.known.get(key, 0) < tgt - 16:
            Q.e.wait_ge(self._sem(key), tgt - 16)
            Q.known[key] = tgt - 16
        Q.e.indirect_dma_start(out=out, out_offset=out_off, in_=in_, in_offset=in_off).then_inc(self._sem(key), 16)
        Q.dcount += 1
        for r in reads:
            if r.r.get(key, 0) < tgt:
                r.r[key] = tgt
        for w in writes:
            w.w = {key: tgt}
            w.r = {}

    def barrier(self):
        latest = {}
        for E in self.E.values():
            if E.count > 0:
                latest[(E.name, "c", (E.count - 1) // EPOCH)] = (E.count - 1) % EPOCH + 1
            for slot in range(min(NSLOT, E.dcount)):
                n = (E.dcount - slot + NSLOT - 1) // NSLOT
                latest[(E.name, "d", slot)] = 16 * n
        for E in self.E.values():
            for k, v in latest.items():
                if E.is_pe and k[0] == "pe" and k[1] == "c":
                    continue
                if E.known.get(k, 0) < v:
                    E.e.wait_ge(self._sem(k), v)
                    E.known[k] = v


class Phase:
    cnt = 0

    def __init__(self, S):
        self.S = S
        self.nc = S.nc
        self.stack = contextlib.ExitStack()

    def sb(self, shape, dt):
        Phase.cnt += 1
        return self.stack.enter_context(self.nc.sbuf_tensor("sb%d" % Phase.cnt, list(shape), dt))

    def ps(self, shape, dt=F32):
        Phase.cnt += 1
        return self.stack.enter_context(self.nc.psum_tensor("ps%d" % Phase.cnt, list(shape), dt))

    def rot_sb(self, n, shape, dt):
        return Rot([(self.sb(shape, dt), Reg()) for _ in range(n)])

    def rot_ps(self, n, shape, dt=F32):
        return Rot([(self.ps(shape, dt), Reg(True)) for _ in range(n)])

    def close(self):
        self.S.barrier()
        self.stack.close()


class Rot:
    def __init__(self, items):
        self.items = items
        self.i = 0

    def next(self):
        it = self.items[self.i % len(self.items)]
        self.i += 1
        return it


def make_consts():
    bf = ml_dtypes.bfloat16
    c = {}
    c["ident_bf"] = np.eye(128, dtype=np.float32).astype(bf)
    c["ident_f"] = np.eye(128, dtype=np.float32)
    half = 8
    inv = (np.float32(500000.0) ** (-np.arange(half, dtype=np.float32) / np.float32(half))).astype(np.float32)
    ang = (np.arange(T, dtype=np.float32)[:, None] * inv[None, :]).astype(np.float32)
    cos = np.cos(ang.astype(np.float64)).astype(np.float32)
    sin = np.sin(ang.astype(np.float64)).astype(np.float32)
    c2 = np.concatenate([cos, cos], axis=1)
    s2 = np.concatenate([-sin, sin], axis=1)
    c2e = np.tile(c2, (1, 8))
    s2e = np.tile(s2, (1, 8))
    c["c2e"] = np.ascontiguousarray(c2e.reshape(32, 128, 128).transpose(1, 0, 2).reshape(128, 4096))
    c["s2e"] = np.ascontiguousarray(s2e.reshape(32, 128, 128).transpose(1, 0, 2).reshape(128, 4096))

    def mult(d):
        m = np.zeros_like(d, dtype=np.float32)
        m += ((d >= 0) & (d <= 128))
        m += ((d >= 0) & (d % 4 == 0) & (d <= 512))
        m += ((d >= 0) & (d % 16 == 0) & (d <= 2048))
        return m

    kl = np.arange(128)[:, None]
    ql = np.arange(512)[None, :]
    dm = np.zeros((128, 20, 512), np.float32)
    for di in range(20):
        delta = -384 + 128 * di
        dm[:, di, :] = mult(delta + ql - kl)
    with np.errstate(divide="ignore"):
        ldm = np.where(dm > 0, 8.0 * np.log(np.maximum(dm, 1e-30).astype(np.float64)), -BIG)
    ldm_hi = ldm.astype(np.float32).astype(bf)
    ldm_lo = (ldm - ldm_hi.astype(np.float64)).astype(np.float32)
    ldm_lo = np.where(dm > 0, ldm_lo, 0.0).astype(bf)
    c["ldm_hi"] = ldm_hi.reshape(128, 20 * 512)
    c["dmm"] = dm[:, 0:8, :].reshape(128, 8 * 512).astype(bf)
    c["dm_lo_need"] = np.array([float(np.any(ldm_lo[:, di, :].astype(np.float32) != 0)) for di in range(20)], np.float32)
    cm = np.zeros((128, 4, 512), np.float32)
    for ci in range(4):
        delta = -384 + 128 * ci
        cm[:, ci, :] = (delta + ql - kl >= 0)
    c["lcm"] = ((cm - 1.0) * BIG).reshape(128, 4 * 512).astype(bf)
    koh = np.zeros((16, T), np.float32)
    for b in range(16):
        koh[b, b * 256:(b + 1) * 256] = 1.0
    c["koh"] = koh.astype(bf)
    tt = np.arange(32)[:, None]
    bb = np.arange(16)[None, :]
    own = tt // 2
    valid = (bb < own).astype(np.float32)
    negv = (valid - 1.0) * BIG
    ownm1 = (bb == own).astype(np.float32) - 1.0
    c["valid"] = np.ascontiguousarray(np.broadcast_to(valid.reshape(1, 512), (128, 512))).astype(np.float32)
    c["negv"] = np.ascontiguousarray(np.broadcast_to(negv.reshape(1, 512), (128, 512))).astype(np.float32)
    c["ownm1"] = np.ascontiguousarray(np.broadcast_to(ownm1.reshape(1, 512), (128, 512))).astype(np.float32)
    j = np.arange(64)[:, None]
    i = np.arange(64)[None, :]
    c["mg"] = ((j <= i).astype(np.float32) - (j <= 31).astype(np.float32)).astype(np.float32)
    ind = np.zeros((64, 3), np.float32)
    ind[:, 0] = (np.arange(64) <= 31)
    ind[:, 1] = 1.0
    ind[:, 2] = (np.arange(64) > 31)
    c["ind"] = ind
    c["cmask8"] = np.ascontiguousarray(np.tile((i >= j).astype(np.float32), (1, 8)))
    c["ones_f"] = np.ones((128, 64), np.float32)
    tp = np.arange(128)[:, None]
    tq = np.arange(128)[None, :]
    c["u_bf"] = (tp < tq).astype(np.float32).astype(bf)
    c["ones_bf"] = np.ones((128, 128), np.float32).astype(bf)
    ecb = np.tile((np.arange(32, dtype=np.float32) * CAP)[None, :], (1, 4))
    c["ecb"] = np.ascontiguousarray(np.broadcast_to(ecb, (128, 128))).astype(np.float32)
    c["pid"] = np.ascontiguousarray(np.broadcast_to((NS + np.arange(128, dtype=np.float32))[:, None], (128, 8)))
    return c


CONST_DT = {"ident_bf": BF16, "ldm_hi": BF16, "dmm": BF16, "lcm": BF16, "koh": BF16, "u_bf": BF16, "ones_bf": BF16}


class Prog:
    def __init__(self, nlayers=DEPTH, debug=None):
        self.nc = nc = bass.Bass("TRN2", target_bir_lowering=False)
        self.S = Sched(nc)
        self.nlayers = nlayers
        self.debug = debug
        self.consts_np = make_consts()
        di = lambda name, shape, dt=F32: nc.dram_tensor(name, list(shape), dt, kind="ExternalInput").ap()
        self.x_in = di("x", [T, D])
        self.ab_w_in = di("ab_w_in", [2, D, 3072])
        self.ab_w_out = di("ab_w_out", [2, D, D])
        self.c_w_in = di("c_w_in", [2, D, 4096])
        self.c_w_out = di("c_w_out", [2, D, D])
        self.cng = di("cng", [2, 64, D])
        self.lbl = di("lbl", [2, 64, D])
        self.lng = di("lng", [8, 128, D])
        self.lnb = di("lnb", [8, 128, D])
        self.rw = di("rw", [4, D, 36])
        self.rb = di("rb", [4, 128, 36])
        self.w1 = di("exp_w1", [4, 32, D, 512])
        self.w3 = di("exp_w3", [4, 32, D, 512])
        self.w2 = di("exp_w2", [4, 32, 512, D])
        self.cd = {}
        self.dm_lo_need = [bool(v) for v in self.consts_np.pop("dm_lo_need")]
        for k, v in self.consts_np.items():
            self.cd[k] = di("c_" + k, v.shape, CONST_DT.get(k, F32))
        self.out = nc.dram_tensor("out", [T, D], F32, kind="ExternalOutput").ap()
        dt_ = lambda name, shape, dt: nc.dram_tensor(name, list(shape), dt).ap()
        self.xres = dt_("xres", [T, D], F32)
        self.xres_r = [Reg() for _ in range(64)]
        self.qkt = dt_("qkt", [4, 512, T], BF16)
        self.qkt_r = Reg()
        self.vd = dt_("vd", [T, D], BF16)
        self.vd_r = Reg()
        self.ot = dt_("ot", [D, T], BF16)
        self.ot_r = Reg()
        self.xt = dt_("xt", [D, T], BF16)
        self.xt_r = Reg()
        self.ident_bf = nc.alloc_sbuf_tensor("ident_bf", [128, 128], BF16)
        self.ident_f = nc.alloc_sbuf_tensor("ident_f", [128, 128], F32)
        self.posg = nc.alloc_sbuf_tensor("posg", [128, 32, 2], I32)
        self.g12 = nc.alloc_sbuf_tensor("g12", [128, 32, 2], F32)
        self.pg_r = [Reg() for _ in range(8)]
        self.xs = dt_("xs", [NS + 128, D], BF16)
        self.xs_r = Reg()
        self.yb = dt_("yb", [NS + 128, D], F32)
        self.yb_r = Reg()
        self.cr = Reg()
        S = self.S
        S.dma("sp", self.ident_bf[:], self.cd["ident_bf"], [], [self.cr])
        r2 = Reg()
        S.dma("sp", self.ident_f[:], self.cd["ident_f"], [], [r2])
        self.cr2 = r2

    def xsrc(self, l):
        return self.x_in if l == 0 else self.xres

    def layer_norm_tile(self, P, np_, y, y_r, g_bc, b_bc, gb_r, out_t, out_r, scr, tail="pool"):
        nc, S = self.nc, self.S
        st, st_r, mv, mv_r, rs, rs_r = scr
        S.dve(lambda: nc.vector.bn_stats(out=st[:np_, 0:6], in_=y[:np_, 0:512]), [y_r], [st_r])
        S.dve(lambda: nc.vector.bn_stats(out=st[:np_, 6:12], in_=y[:np_, 512:1024]), [y_r], [st_r])
        S.dve(lambda: nc.vector.bn_aggr(out=mv[:np_, :], in_=st[:np_, :]), [st_r], [mv_r])
        S.dve(lambda: nc.vector.tensor_scalar(out=rs[:np_, :], in0=mv[:np_, 1:2], scalar1=LN_EPS, scalar2=None,
                                              op0=ALU.add), [mv_r], [rs_r])
        S.act(lambda: nc.scalar.activation(out=rs[:np_, :], in_=rs[:np_, :], func=AF.Ln), [rs_r], [rs_r])
        S.act(lambda: nc.scalar.activation(out=rs[:np_, :], in_=rs[:np_, :], func=AF.Exp, scale=-0.5), [rs_r], [rs_r])
        V = nc.vector
        S.dve(lambda: V.scalar_tensor_tensor(out=out_t[:np_, :], in0=y[:np_, :], scalar=mv[:np_, 0:1], in1=g_bc[:np_, :],
                                             op0=ALU.subtract, op1=ALU.mult), [y_r, mv_r, gb_r], [out_r])
        S.dve(lambda: V.scalar_tensor_tensor(out=out_t[:np_, :], in0=out_t[:np_, :], scalar=rs[:np_, 0:1], in1=b_bc[:np_, :],
                                             op0=ALU.mult, op1=ALU.add), [out_r, rs_r, gb_r], [out_r])

    def phase_attn_proj(self, l):
        nc, S = self.nc, self.S
        j = l // 2
        P = Phase(S)
        W = P.sb([128, 8, 3072], BF16)
        w_r = []
        for k in range(8):
            r = Reg()
            S.dma("pool", W[:, k, :], self.ab_w_in[j, k * 128:(k + 1) * 128, :], [], [r])
            w_r.append(r)
        c2e = P.sb([128, 32, 128], F32)
        s2e = P.sb([128, 32, 128], F32)
        tab_r = Reg()
        tab_r2 = Reg()
        S.dma("sp", c2e[:].rearrange("p a b -> p (a b)"), self.cd["c2e"], [], [tab_r])
        S.dma("sp", s2e[:].rearrange("p a b -> p (a b)"), self.cd["s2e"], [], [tab_r2])
        xs = P.rot_sb(2, [128, D], F32)
        xT = P.rot_sb(2, [128, 8, 128], BF16)
        ps_t = P.rot_ps(2, [128, 512], F32)
        ps_o = P.rot_ps(3, [128, 512], F32)
        ps_tr = P.rot_ps(2, [128, 1024], BF16)
        qs = P.rot_sb(3, [128, 512], BF16)
        t1 = P.rot_sb(2, [128, 8, 16], F32)
        t2 = P.rot_sb(2, [128, 8, 16], F32)
        stg = P.rot_sb(2, [128, 16, 512], BF16)
        vst = P.rot_sb(2, [128, 1024], BF16)
        src = self.xsrc(l)
        for g in range(8):
            stg_t, stg_r = stg.next()
            for tl in range(4):
                t = g * 4 + tl
                x_t, x_r = xs.next()
                S.dma("sp", x_t[:], src[t * 128:(t + 1) * 128, :], [self.xres_r[2 * t], self.xres_r[2 * t + 1]], [x_r])
                xT_t, xT_r = xT.next()
                for hb in range(2):
                    pt, pt_r = ps_t.next()
                    for kk in range(4):
                        k = hb * 4 + kk
                        S.pe(lambda: nc.tensor.transpose(pt[:, kk * 128:(kk + 1) * 128], x_t[:, k * 128:(k + 1) * 128],
                                                         self.ident_f[:]), [x_r, self.cr2], [pt_r])
                    S.act(lambda: nc.scalar.copy(out=xT_t[:, hb * 4:(hb + 1) * 4, :].rearrange("p a b -> p (a b)"),
                                                 in_=pt[:, :]), [pt_r], [xT_r])
                v_t, v_r = vst.next()
                for cg in range(6):
                    if LVL < 2:
                        break
                    po, po_r = ps_o.next()
                    for k in range(8):
                        S.pe(lambda: nc.tensor.matmul(po[:, :], lhsT=xT_t[:, k, :], rhs=W[:, k, cg * 512:(cg + 1) * 512],
                                                      start=(k == 0), stop=(k == 7)), [xT_r, w_r[k]], [po_r])
                    if cg in (2, 5):
                        off = 0 if cg == 2 else 512
                        S.act(lambda: nc.scalar.copy(out=v_t[:, off:off + 512], in_=po[:, :]), [po_r], [v_r])
                        continue
                    cgi = {0: 0, 1: 1, 3: 2, 4: 3}[cg]
                    if LVL < 3:
                        continue
                    q_t, q_r = qs.next()
                    S.act(lambda: nc.scalar.copy(out=q_t[:, :], in_=po[:, :]), [po_r], [q_r])
                    a1, a1_r = t1.next()
                    a2, a2_r = t2.next()
                    pov = po[:, :].rearrange("p (h d) -> p h d", h=8)
                    qv = q_t[:, :].rearrange("p (h d) -> p h d", h=8)
                    cv = c2e[:, t, :].rearrange("p (h d) -> p h d", h=8)
                    sv = s2e[:, t, :].rearrange("p (h d) -> p h d", h=8)
                    S.dve(lambda: nc.vector.tensor_tensor(out=a1[:, :, :], in0=pov[:, :, 0:16], in1=cv, op=ALU.mult),
                          [po_r, tab_r], [a1_r])
                    S.dve(lambda: nc.vector.tensor_tensor(out=a2[:, :, 0:8], in0=pov[:, :, 8:16], in1=sv[:, :, 0:8],
                                                          op=ALU.mult), [po_r, tab_r2], [a2_r])
                    S.dve(lambda: nc.vector.tensor_tensor(out=a2[:, :, 8:16], in0=pov[:, :, 0:8], in1=sv[:, :, 8:16],
                                                          op=ALU.mult), [po_r, tab_r2], [a2_r])
                    S.dve(lambda: nc.vector.tensor_tensor(out=qv[:, :, 0:16], in0=a1[:, :, :], in1=a2[:, :, :], op=ALU.add),
                          [a1_r, a2_r], [q_r])
                    if LVL < 4:
                        continue
                    ptr, ptr_r = ps_tr.next()
                    for pr in range(4):
                        S.pe(lambda: nc.tensor.transpose(ptr[:, pr * 128:(pr + 1) * 128], q_t[:, pr * 128:(pr + 1) * 128],
                                                         self.ident_bf[:]), [q_r, self.cr], [ptr_r])
                    S.dve(lambda: nc.vector.tensor_copy(
                        out=stg_t[:, cgi * 4:(cgi + 1) * 4, tl * 128:(tl + 1) * 128],
                        in_=ptr[:, 0:512].rearrange("p (a b) -> p a b", a=4)), [ptr_r], [stg_r])
                if LVL >= 2:
                    S.dma("act", self.vd[t * 128:(t + 1) * 128, :], v_t[:, :], [v_r], [self.vd_r])
            for cgi in range(4):
                if LVL < 5:
                    break
                S.dma("sp", self.qkt[cgi, :, g * 512:(g + 1) * 512].rearrange("(a p) t -> p a t", p=128),
                      stg_t[:, cgi * 4:(cgi + 1) * 4, :], [stg_r], [self.qkt_r])
        P.close()

    def phase_attn(self, l):
        nc, S = self.nc, self.S
        P = Phase(S)
        dmh = P.sb([128, 20, 512], BF16)
        dmm = P.sb([128, 8, 512], BF16)
        cm = P.sb([128, 4, 512], BF16)
        S.dma("sp", dmh[:].rearrange("p a b -> p (a b)"), self.cd["ldm_hi"], [], [Reg()])
        S.dma("sp", dmm[:].rearrange("p a b -> p (a b)"), self.cd["dmm"], [], [Reg()])
        S.dma("sp", cm[:].rearrange("p a b -> p (a b)"), self.cd["lcm"], [], [Reg()])
        valid = P.sb([128, 512], F32)
        negv = P.sb([128, 512], F32)
        ownm1 = P.sb([128, 512], F32)
        ones_f = P.sb([128, 64], F32)
        k_r = Reg()
        for tl_, nm in ((valid, "valid"), (negv, "negv"), (ownm1, "ownm1"), (ones_f, "ones_f")):
            r = Reg()
            S.dma("sp", tl_[:], self.cd[nm], [], [r])
            k_r = r
        cst_r = Reg()
        QT = [P.sb([128, T], BF16) for _ in range(2)]
        KT = [P.sb([128, T], BF16) for _ in range(2)]
        VA = [P.sb([128, 32, 128], BF16) for _ in range(2)]
        QT_r = [Reg(), Reg()]
        QTb_r = [Reg(), Reg()]
        KT_r = [Reg(), Reg()]
        KTc_r = [Reg(), Reg()]
        VA_r = [[Reg() for _ in range(4)] for _ in range(2)]
        VAo_r = [Reg(), Reg()]
        for b in range(2):
            S.pool(lambda: nc.gpsimd.memset(QT[b][0:64, :], 0.0), [], [QTb_r[b]])
            S.pool(lambda: nc.gpsimd.memset(KT[b][0:64, :], 0.0), [], [KTc_r[b]])
            S.dma("sp", KT[b][0:16, :], self.cd["koh"], [KTc_r[b]], [KTc_r[b]])
            S.pool(lambda: nc.gpsimd.memset(VA[b][:, :, 64:128], 1.0), [], [VAo_r[b]])
        S.barrier()
        ps_s = P.rot_ps(4, [128, 512], F32)
        ps_acc = P.rot_ps(2, [128, 512], F32)
        ps_g = P.ps([128, 512], F32)
        ps_g_r = Reg(True)
        ps_b = P.rot_ps(1, [128, 1024], BF16)
        pb = P.rot_sb(6, [128, 512], BF16)
        km = P.sb([128, 16], F32)
        kmh = P.sb([128, 16], BF16)
        kml = P.sb([128, 16], BF16)
        kmt = P.sb([128, 16], F32)
        km_r = Reg()
        gm = P.sb([128, 32, 16], F32)
        gm_r = Reg()
        m8 = P.sb([128, 32, 8], F32)
        m8_r = Reg()
        sel = P.sb([128, 32, 16], F32)
        sel_r = Reg()
        bia = P.sb([128, 32, 16], BF16)
        bia_r = Reg()
        den = P.rot_sb(2, [128, 512], F32)
        bcs = P.rot_sb(2, [64, 512], F32)
        ost = P.rot_sb(2, [64, 512], BF16)
        def prep_steps(h):
            b = h % 2
            moba = h < 8
            qc, kc = (0, 1) if moba else (2, 3)
            hh = h % 8
            steps = []

            def loads():
                S.dma("sp", QT[b][64:128, :], self.qkt[qc, hh * 64:(hh + 1) * 64, :], [self.qkt_r], [QT_r[b]])
                S.dma("sp", KT[b][64:128, :], self.qkt[kc, hh * 64:(hh + 1) * 64, :], [self.qkt_r], [KT_r[b]])
                for q4 in range(4):
                    S.dma("sp", VA[b][:, q4 * 8:(q4 + 1) * 8, 0:64],
                          self.vd[q4 * 1024:(q4 + 1) * 1024, h * 64:(h + 1) * 64].rearrange("(t p) c -> p t c", p=128),
                          [self.vd_r], [VA_r[b][q4]])
            steps.append(loads)
            if not moba:
                if h in (8, 9):
                    steps.append(lambda: S.pool(lambda: nc.gpsimd.memset(QT[b][0:16, :], 0.0), [], [QTb_r[b]]))
                return steps

            def gate1():
                S.dve(lambda: nc.vector.tensor_reduce(out=km[64:128, :],
                                                      in_=KT[b][64:128, :].rearrange("p (a c) -> p a c", a=16),
                                                      axis=AX.X, op=ALU.add), [KT_r[b]], [km_r])
                S.dve(lambda: nc.vector.tensor_scalar(out=km[64:128, :], in0=km[64:128, :], scalar1=1.0 / 256.0, scalar2=None,
                                                      op0=ALU.mult), [km_r], [km_r])
                S.dve(lambda: nc.vector.tensor_copy(out=kmh[64:128, :], in_=km[64:128, :]), [km_r], [km_r])
                S.dve(lambda: nc.vector.tensor_tensor(out=kmt[64:128, :], in0=km[64:128, :], in1=kmh[64:128, :],
                                                      op=ALU.subtract), [km_r], [km_r])
                S.dve(lambda: nc.vector.tensor_copy(out=kml[64:128, :], in_=kmt[64:128, :]), [km_r], [km_r])
                for t in range(32):
                    S.pe(lambda: nc.tensor.matmul(ps_g[:, t * 16:(t + 1) * 16], lhsT=QT[b][64:128, t * 128:(t + 1) * 128],
                                                  rhs=kmh[64:128, :], start=True, stop=False), [QT_r[b], km_r], [ps_g_r])
                    S.pe(lambda: nc.tensor.matmul(ps_g[:, t * 16:(t + 1) * 16], lhsT=QT[b][64:128, t * 128:(t + 1) * 128],
                                                  rhs=kml[64:128, :], start=False, stop=True), [QT_r[b], km_r], [ps_g_r])
                gmf = gm[:].rearrange("p a b -> p (a b)")
                S.dve(lambda: nc.vector.tensor_tensor(out=gmf, in0=ps_g[:, :], in1=negv[:], op=ALU.add), [ps_g_r], [gm_r])
            steps.append(gate1)

            def gate2():
                for t in range(32):
                    S.dve(lambda: nc.vector.max(out=m8[:, t, :], in_=gm[:, t, :]), [gm_r], [m8_r])
            steps.append(gate2)

            def gate3():
                S.dve(lambda: nc.vector.tensor_tensor(out=sel[:, :, :], in0=gm[:, :, :],
                                                      in1=m8[:, :, 2:3].to_broadcast([128, 32, 16]), op=ALU.is_ge),
                      [gm_r, m8_r], [sel_r])
                self_f = sel[:].rearrange("p a b -> p (a b)")
                S.dve(lambda: nc.vector.tensor_tensor(out=self_f, in0=self_f, in1=valid[:], op=ALU.mult), [sel_r], [sel_r])
                S.dve(lambda: nc.vector.tensor_tensor(out=self_f, in0=self_f, in1=ownm1[:], op=ALU.add), [sel_r], [sel_r])
                S.dve(lambda: nc.vector.tensor_scalar(out=bia[:].rearrange("p a b -> p (a b)"), in0=self_f, scalar1=BIG,
                                                      scalar2=None, op0=ALU.mult), [sel_r], [bia_r])
            steps.append(gate3)

            def mk_tr(g4):
                def tr():
                    pbt, pbt_r = ps_b.next()
                    for tt in range(8):
                        t = g4 * 8 + tt
                        S.pe(lambda: nc.tensor.transpose(pbt[0:16, tt * 128:(tt + 1) * 128], bia[:, t, :], self.ident_bf[:]),
                             [bia_r], [pbt_r])
                    S.act(lambda: nc.scalar.copy(out=QT[b][0:16, g4 * 1024:(g4 + 1) * 1024], in_=pbt[0:16, :]),
                          [pbt_r], [QTb_r[b]])
                return tr
            for g4 in range(4):
                steps.append(mk_tr(g4))
            return steps

        def attend(h, steps):
            b = h % 2
            moba = h < 8
            lo = 0
            blocks = []
            for Q in range(8):
                ms = list(range(0, 4 * Q + 4)) if moba else list(range(max(0, 4 * Q - 16), 4 * Q + 4))
                for mi, m in enumerate(ms):
                    blocks.append((Q, mi, m, len(ms)))
            n = len(blocks)
            every = max(1, n // (len(steps) + 1)) if steps else n + 1
            sq = {}
            LOOK = 3

            def issue_S(i):
                Q, mi, m, nm = blocks[i]
                di_ = (512 * Q - 128 * m + 384) // 128
                extra = []
                if moba:
                    if di_ <= 3:
                        extra.append(cm[:, di_, :])
                elif not self.dm_lo_need[di_]:
                    extra.append(dmh[:, di_, :])
                sp_, sp_r = ps_s.next()
                S.pe(lambda: nc.tensor.matmul(sp_[:, :], lhsT=KT[b][lo:128, m * 128:(m + 1) * 128],
                                              rhs=QT[b][lo:128, Q * 512:(Q + 1) * 512], start=True, stop=(not extra)),
                     [KT_r[b], KTc_r[b], QT_r[b], QTb_r[b]], [sp_r])
                for xi, xm in enumerate(extra):
                    S.pe(lambda: nc.tensor.matmul(sp_[:, :], lhsT=self.ident_bf[:, :], rhs=xm, start=False,
                                                  stop=(xi == len(extra) - 1)), [], [sp_r])
                sq[i] = (sp_, sp_r)

            fin = []

            def fin_pe(Q, acc, acc_r, dn, dn_r):
                bp, bp_r = ps_s.next()
                S.pe(lambda: nc.tensor.matmul(bp[0:64, :], lhsT=ones_f[64:65, 0:64], rhs=dn[64:65, :], start=True, stop=True),
                     [dn_r], [bp_r])
                bc, bc_r = bcs.next()
                S.act(lambda: nc.scalar.copy(out=bc[:, :], in_=bp[0:64, :]), [bp_r], [bc_r])
                o_t, o_r = ost.next()
                S.dve(lambda: nc.vector.tensor_tensor(out=o_t[:, :], in0=acc[0:64, :], in1=bc[:, :], op=ALU.mult),
                      [acc_r, bc_r], [o_r])
                S.dma("sp", self.ot[h * 64:(h + 1) * 64, Q * 512:(Q + 1) * 512], o_t[:, :], [o_r], [self.ot_r])

            for i in range(min(LOOK, n)):
                issue_S(i)
            acc = acc_r = None
            for i in range(n):
                Q, mi, m, nm = blocks[i]
                di_ = (512 * Q - 128 * m + 384) // 128
                sp_, sp_r = sq.pop(i)
                p_t, p_r = pb.next()
                S.act(lambda: nc.scalar.activation(out=p_t[:, :], in_=sp_[:, :], func=AF.Exp, scale=0.125), [sp_r], [p_r])
                if (not moba) and self.dm_lo_need[di_]:
                    S.dve(lambda: nc.vector.tensor_tensor(out=p_t[:, :], in0=p_t[:, :], in1=dmm[:, di_, :], op=ALU.mult),
                          [p_r], [p_r])
                if i + LOOK < n:
                    issue_S(i + LOOK)
                if mi == 0:
                    acc, acc_r = ps_acc.next()
                S.pe(lambda: nc.tensor.matmul(acc[:, :], lhsT=VA[b][:, m, :], rhs=p_t[:, :],
                                              start=(mi == 0), stop=(mi == nm - 1)), [p_r, VA_r[b][m // 8], VAo_r[b]], [acc_r])
                for f in fin:
                    f[0] -= 1
                while fin and fin[0][0] <= 0:
                    f = fin.pop(0)
                    fin_pe(*f[1:])
                if mi == nm - 1:
                    dn, dn_r = den.next()
                    S.act(lambda: nc.scalar.copy(out=dn[64:65, :], in_=acc[64:65, :]), [acc_r], [dn_r])
                    S.dve(lambda: nc.vector.reciprocal(out=dn[64:65, :], in_=dn[64:65, :]), [dn_r], [dn_r])
                    fin.append([2, Q, acc, acc_r, dn, dn_r])
                if steps and (i + 1) % every == 0:
                    steps.pop(0)()
            while fin:
                f = fin.pop(0)
                fin_pe(*f[1:])
            while steps:
                steps.pop(0)()

        for st_ in prep_steps(0):
            st_()
        for h in range(16):
            nxt = prep_steps(h + 1) if h + 1 < 16 else []
            attend(h, nxt)
        P.close()


    def phase_hgrn(self, l):
        nc, S = self.nc, self.S
        j = l // 2
        P = Phase(S)
        V = nc.vector
        G_ = nc.gpsimd
        W = P.sb([128, 8, 4096], BF16)
        w_r = []
        for k in range(8):
            r = Reg()
            S.dma("pool", W[:, k, :], self.c_w_in[j, k * 128:(k + 1) * 128, :], [], [r])
            w_r.append(r)
        mg = P.sb([64, 64], F32)
        ind = P.sb([64, 3], F32)
        cmask8 = P.sb([64, 512], F32)
        cng = P.sb([64, D], F32)
        lb = P.sb([64, D], F32)
        oml = P.sb([64, D], F32)
        l0 = P.sb([64, D], F32)
        cr = Reg()
        for tl_, ap_ in ((mg, self.cd["mg"]), (ind, self.cd["ind"]), (cmask8, self.cd["cmask8"]), (cng, self.cng[j]),
                         (lb, self.lbl[1]), (l0, self.lbl[0])):
            S.dma("sp", tl_[:], ap_, [], [Reg()])
        S.barrier()
        if j == 0:
            S.dve(lambda: V.memset(lb[:], 0.0), [], [cr])
            S.dve(lambda: V.memset(oml[:], 1.0), [], [cr])
        else:
            S.dve(lambda: V.tensor_tensor(out=lb[:], in0=lb[:], in1=l0[:], op=ALU.subtract), [], [cr])
            S.act(lambda: nc.scalar.activation(out=lb[:], in_=lb[:], func=AF.Sigmoid), [cr], [cr])
            S.dve(lambda: V.tensor_scalar(out=oml[:], in0=lb[:], scalar1=-1.0, scalar2=1.0, op0=ALU.mult, op1=ALU.add),
                  [cr], [cr])
        Sst = P.sb([128, 8, 128], F32)
        Sst_r = Reg()
        S.dve(lambda: V.memset(Sst[:].rearrange("p a b -> p (a b)"), 0.0), [], [Sst_r])
        S.barrier()
        banks = P.rot_ps(3, [128, 512], F32)
        pob = P.rot_ps(2, [128, 512], F32)
        pkb = P.rot_ps(1, [128, 512], F32)
        pbf = P.rot_ps(2, [128, 1024], BF16)
        xs = P.rot_sb(3, [64, D], F32)
        xT = P.rot_sb(2, [128, 8, 128], BF16)
        for (xt_, xr_) in xT.items:
            S.pool(lambda: G_.memset(xt_[:].rearrange("p a b -> p (a b)"), 0.0), [], [xr_])
        fbuf = P.rot_sb(1, [64, D], F32)
        lfb = P.rot_sb(2, [64, D], F32)
        kkb = P.rot_sb(1, [64, D], F32)
        eGb = P.rot_sb(1, [64, D], F32)
        enGb = P.rot_sb(1, [64, D], F32)
        qtb = P.rot_sb(2, [64, D], BF16)
        ktb = P.rot_sb(3, [64, D], BF16)
        vb = P.rot_sb(3, [64, D], BF16)
        sgb = P.rot_sb(3, [64, D], F32)
        abcb = P.rot_sb(3, [128, 8, 3], F32)
        qTb = P.rot_sb(3, [128, 8, 64], BF16)
        kTb = P.rot_sb(2, [128, 8, 64], BF16)
        ATb = P.rot_sb(3, [64, 512], BF16)
        smb = P.rot_sb(2, [128, 8, 128], BF16)
        tmpb = P.rot_sb(1, [128, 8, 128], F32)
        sqb = P.rot_sb(1, [64, D], F32)
        msb = P.rot_sb(2, [64, 16], F32)
        onb = P.rot_sb(1, [64, D], F32)
        ogb = P.rot_sb(3, [64, D], BF16)
        stg = P.rot_sb(2, [128, 8, 512], BF16)
        src = self.xsrc(l)
        idf = self.ident_f
        idb = self.ident_bf

        def stage_a(t):
            x_t, x_r = xs.next()
            S.dma("sp", x_t[:], src[t * 64:(t + 1) * 64, :], [self.xres_r[t]], [x_r])
            pt, pt_r = banks.next()
            for k in range(8):
                S.pe(lambda: nc.tensor.transpose(pt[:, k * 64:(k + 1) * 64], x_t[:, k * 128:(k + 1) * 128], idf[0:64, 0:64]),
                     [x_r], [pt_r])
            xT_t, xT_r = xT.next()
            S.act(lambda: nc.scalar.copy(out=xT_t[:, :, 0:64], in_=pt[:, :].rearrange("p (a b) -> p a b", a=8)), [pt_r], [xT_r])

            def proj(cg):
                po, po_r = banks.next()
                for k in range(8):
                    S.pe(lambda: nc.tensor.matmul(po[:, :], lhsT=xT_t[:, k, :], rhs=W[:, k, cg * 512:(cg + 1) * 512],
                                                  start=(k == 0), stop=(k == 7)), [xT_r, w_r[k]], [po_r])
                return po, po_r

            f_t, f_r = fbuf.next()
            for hf in range(2):
                po, po_r = proj(2 + hf)
                S.act(lambda: nc.scalar.activation(out=f_t[:, hf * 512:(hf + 1) * 512], in_=po[0:64, :], func=AF.Sigmoid),
                      [po_r], [f_r])
            v_t, v_r = vb.next()
            for hf in range(2):
                po, po_r = proj(4 + hf)
                S.act(lambda: nc.scalar.copy(out=v_t[:, hf * 512:(hf + 1) * 512], in_=po[0:64, :]), [po_r], [v_r])
            S.dve(lambda: V.tensor_tensor(out=f_t[:], in0=f_t[:], in1=oml[:], op=ALU.mult), [f_r, cr], [f_r])
            S.dve(lambda: V.tensor_tensor(out=f_t[:], in0=f_t[:], in1=lb[:], op=ALU.add), [f_r, cr], [f_r])
            lf_t, lf_r = lfb.next()
            S.act(lambda: nc.scalar.activation(out=lf_t[:], in_=f_t[:], func=AF.Ln), [f_r], [lf_r])
            kk_t, kk_r = kkb.next()
            S.pool(lambda: G_.tensor_scalar(out=kk_t[:], in0=f_t[:], scalar1=-1.0, scalar2=1.0, op0=ALU.mult, op1=ALU.add),
                   [f_r], [kk_r])
            sg_t, sg_r = sgb.next()
            for hf in range(2):
                po, po_r = proj(6 + hf)
                S.act(lambda: nc.scalar.activation(out=sg_t[:, hf * 512:(hf + 1) * 512], in_=po[0:64, :], func=AF.Silu),
                      [po_r], [sg_r])
            eG_t, eG_r = eGb.next()
            enG_t, enG_r = enGb.next()
            for hf in range(2):
                pg_, pg_r = banks.next()
                S.pe(lambda: nc.tensor.matmul(pg_[0:64, :], lhsT=mg[:, :], rhs=lf_t[:, hf * 512:(hf + 1) * 512],
                                              start=True, stop=True), [lf_r], [pg_r])
                S.act(lambda: nc.scalar.activation(out=eG_t[:, hf * 512:(hf + 1) * 512], in_=pg_[0:64, :], func=AF.Exp),
                      [pg_r], [eG_r])
                S.act(lambda: nc.scalar.activation(out=enG_t[:, hf * 512:(hf + 1) * 512], in_=pg_[0:64, :], func=AF.Exp,
                                                   scale=-1.0), [pg_r], [enG_r])
            pst, pst_r = banks.next()
            for h in range(8):
                S.pe(lambda: nc.tensor.matmul(pst[:, h * 3:(h + 1) * 3], lhsT=lf_t[:, h * 128:(h + 1) * 128], rhs=ind[:, :],
                                              start=True, stop=True), [lf_r], [pst_r])
            abc, abc_r = abcb.next()
            S.act(lambda: nc.scalar.activation(out=abc[:].rearrange("p a b -> p (a b)"), in_=pst[:, 0:24], func=AF.Exp),
                  [pst_r], [abc_r])
            qt_t, qt_r = qtb.next()
            for hf in range(2):
                po, po_r = proj(hf)
                S.dve(lambda: V.tensor_tensor(out=qt_t[:, hf * 512:(hf + 1) * 512], in0=po[0:64, :],
                                              in1=eG_t[:, hf * 512:(hf + 1) * 512], op=ALU.mult), [po_r, eG_r], [qt_r])
            kt_t, kt_r = ktb.next()
            S.dve(lambda: V.tensor_tensor(out=kt_t[:], in0=kk_t[:], in1=enG_t[:], op=ALU.mult), [kk_r, enG_r], [kt_r])
            qT_t, qT_r = qTb.next()
            kT_t, kT_r = kTb.next()
            for (src_t, src_r, dst_t, dst_r, eng) in ((qt_t, qt_r, qT_t, qT_r, "dve"), (kt_t, kt_r, kT_t, kT_r, "act")):
                pb_, pb_r = pbf.next()
                for h in range(8):
                    S.pe(lambda: nc.tensor.transpose(pb_[:, h * 64:(h + 1) * 64], src_t[:, h * 128:(h + 1) * 128],
                                                     idb[0:64, 0:64]), [src_r], [pb_r])
                if eng == "dve":
                    S.dve(lambda: V.tensor_copy(out=dst_t[:].rearrange("p a b -> p (a b)"), in_=pb_[:, 0:512]), [pb_r], [dst_r])
                else:
                    S.act(lambda: nc.scalar.copy(out=dst_t[:].rearrange("p a b -> p (a b)"), in_=pb_[:, 0:512]), [pb_r], [dst_r])
            pat, pat_r = banks.next()
            for h in range(8):
                S.pe(lambda: nc.tensor.matmul(pat[0:64, h * 64:(h + 1) * 64], lhsT=kT_t[:, h, :], rhs=qT_t[:, h, :],
                                              start=True, stop=True), [kT_r, qT_r], [pat_r])
            AT_t, AT_r = ATb.next()
            S.dve(lambda: V.tensor_tensor(out=AT_t[:, :], in0=pat[0:64, :], in1=cmask8[:, :], op=ALU.mult), [pat_r], [AT_r])
            return (qT_t, qT_r, AT_t, AT_r, v_t, v_r, kt_t, kt_r, sg_t, sg_r, abc, abc_r)

        def stage_b(t, bufs, stg_t, stg_r):
            (qT_t, qT_r, AT_t, AT_r, v_t, v_r, kt_t, kt_r, sg_t, sg_r, abc, abc_r) = bufs
            tl = t % 8
            sm_t, sm_r = smb.next()
            S.dve(lambda: V.tensor_tensor(out=sm_t[:, :, :], in0=Sst[:, :, :],
                                          in1=abc[:, :, 0:1].to_broadcast([128, 8, 128]), op=ALU.mult),
                  [Sst_r, abc_r], [sm_r])
            po0, po0_r = pob.next()
            po1, po1_r = pob.next()
            for h in range(8):
                ob, ob_r = (po0, po0_r) if h < 4 else (po1, po1_r)
                c0 = (h % 4) * 128
                S.pe(lambda: nc.tensor.matmul(ob[0:64, c0:c0 + 128], lhsT=qT_t[:, h, :], rhs=sm_t[:, h, :],
                                              start=True, stop=False), [qT_r, sm_r], [ob_r])
                S.pe(lambda: nc.tensor.matmul(ob[0:64, c0:c0 + 128], lhsT=AT_t[:, h * 64:(h + 1) * 64],
                                              rhs=v_t[:, h * 128:(h + 1) * 128], start=False, stop=True),
                     [AT_r, v_r], [ob_r])
            tmp_t, tmp_r = tmpb.next()
            for hb in range(2):
                kb, kb_r = pkb.next()
                for hh in range(4):
                    h = hb * 4 + hh
                    S.pe(lambda: nc.tensor.matmul(kb[:, hh * 128:(hh + 1) * 128], lhsT=kt_t[:, h * 128:(h + 1) * 128],
                                                  rhs=v_t[:, h * 128:(h + 1) * 128], start=True, stop=True),
                         [kt_r, v_r], [kb_r])
                S.dve(lambda: V.tensor_tensor(out=tmp_t[:, hb * 4:(hb + 1) * 4, :],
                                              in0=kb[:, :].rearrange("p (a b) -> p a b", a=4),
                                              in1=abc[:, hb * 4:(hb + 1) * 4, 2:3].to_broadcast([128, 4, 128]), op=ALU.mult),
                      [kb_r, abc_r], [tmp_r])
            S.dve(lambda: V.tensor_tensor(out=Sst[:, :, :], in0=Sst[:, :, :],
                                          in1=abc[:, :, 1:2].to_broadcast([128, 8, 128]), op=ALU.mult),
                  [Sst_r, abc_r], [Sst_r])
            S.dve(lambda: V.tensor_tensor(out=Sst[:, :, :], in0=Sst[:, :, :], in1=tmp_t[:, :, :], op=ALU.add),
                  [Sst_r, tmp_r], [Sst_r])
            sq_t, sq_r = sqb.next()
            for hb, (ob, ob_r) in enumerate(((po0, po0_r), (po1, po1_r))):
                S.act(lambda: nc.scalar.activation(out=sq_t[:, hb * 512:(hb + 1) * 512], in_=ob[0:64, :], func=AF.Square),
                      [ob_r], [sq_r])
            ms_t, ms_r = msb.next()
            S.dve(lambda: V.tensor_reduce(out=ms_t[:, 0:8], in_=sq_t[:].rearrange("p (a b) -> p a b", a=8), axis=AX.X,
                                          op=ALU.add), [sq_r], [ms_r])
            S.dve(lambda: V.tensor_scalar(out=ms_t[:, 0:8], in0=ms_t[:, 0:8], scalar1=1.0 / 128.0, scalar2=RMS_EPS,
                                          op0=ALU.mult, op1=ALU.add), [ms_r], [ms_r])
            S.act(lambda: nc.scalar.activation(out=ms_t[:, 0:8], in_=ms_t[:, 0:8], func=AF.Ln), [ms_r], [ms_r])
            S.act(lambda: nc.scalar.activation(out=ms_t[:, 8:16], in_=ms_t[:, 0:8], func=AF.Exp, scale=-0.5), [ms_r], [ms_r])
            on_t, on_r = onb.next()
            for hb, (ob, ob_r) in enumerate(((po0, po0_r), (po1, po1_r))):
                S.dve(lambda: V.tensor_tensor(out=on_t[:, hb * 512:(hb + 1) * 512].rearrange("p (a b) -> p a b", a=4),
                                              in0=ob[0:64, :].rearrange("p (a b) -> p a b", a=4),
                                              in1=ms_t[:, 8 + hb * 4:8 + (hb + 1) * 4].unsqueeze(2).to_broadcast([64, 4, 128]),
                                              op=ALU.mult), [ob_r, ms_r], [on_r])
            S.pool(lambda: G_.tensor_tensor(out=on_t[:], in0=on_t[:], in1=cng[:], op=ALU.mult), [on_r], [on_r])
            og_t, og_r = ogb.next()
            S.pool(lambda: G_.tensor_tensor(out=og_t[:], in0=on_t[:], in1=sg_t[:], op=ALU.mult), [on_r, sg_r], [og_r])
            return (t, og_t, og_r)

        stg_cur = {}

        def stage_c(args):
            t, og_t, og_r = args
            tl = t % 8
            if tl == 0:
                stg_cur["t"], stg_cur["r"] = stg.next()
            stg_t, stg_r = stg_cur["t"], stg_cur["r"]
            pb_, pb_r = pbf.next()
            for h in range(8):
                S.pe(lambda: nc.tensor.transpose(pb_[:, h * 64:(h + 1) * 64], og_t[:, h * 128:(h + 1) * 128], idb[0:64, 0:64]),
                     [og_r], [pb_r])
            S.act(lambda: nc.scalar.copy(out=stg_t[:, :, tl * 64:(tl + 1) * 64],
                                         in_=pb_[:, 0:512].rearrange("p (a b) -> p a b", a=8)), [pb_r], [stg_r])
            if tl == 7:
                g = t // 8
                for k in range(8):
                    S.dma("act", self.ot[k * 128:(k + 1) * 128, g * 512:(g + 1) * 512], stg_t[:, k, :], [stg_r], [self.ot_r])

        nt = T // 64
        pend = stage_a(0)
        pend_c = None
        for t in range(nt):
            nxt = stage_a(t + 1) if t + 1 < nt else None
            if pend_c is not None:
                stage_c(pend_c)
            pend_c = stage_b(t, pend, None, None)
            pend = nxt
        stage_c(pend_c)
        P.close()

    def phase_out_ln_router(self, l, w_out_ap):
        nc, S = self.nc, self.S
        V = nc.vector
        P = Phase(S)
        W = P.sb([128, 8, D], BF16)
        w_r = []
        for k in range(8):
            r = Reg()
            S.dma("pool", W[:, k, :], w_out_ap[k * 128:(k + 1) * 128, :], [], [r])
            w_r.append(r)
        g_bc = P.sb([128, D], F32)
        b_bc = P.sb([128, D], F32)
        rw = P.sb([128, 8, 36], F32)
        rb = P.sb([128, 36], F32)
        u_bf = P.sb([128, 128], BF16)
        ones_bf = P.sb([128, 128], BF16)
        ecb = P.sb([128, 128], F32)
        carry = P.sb([128, 32], F32)
        pid = P.sb([128, 8], F32)
        S.dma("sp", pid[:], self.cd["pid"], [], [Reg()])
        for tl_, ap_ in ((g_bc, self.lng[l * 2]), (b_bc, self.lnb[l * 2]), (rw, self.rw[l].rearrange("(k p) n -> p k n", p=128)),
                         (rb, self.rb[l]), (u_bf, self.cd["u_bf"]), (ones_bf, self.cd["ones_bf"]), (ecb, self.cd["ecb"])):
            S.dma("sp", tl_[:], ap_, [], [Reg()])
        car_r = Reg()
        S.dve(lambda: V.memset(carry[:], 0.0), [], [car_r])
        S.barrier()
        gb_r = Reg()
        oT = Rot([(P.sb([128, 8, 512], BF16), [Reg() for _ in range(8)]) for _ in range(2)])
        xs = P.rot_sb(3, [128, D], F32)
        ys = P.rot_sb(2, [128, D], F32)
        xa = P.rot_sb(3, [128, D], F32)
        xbf = P.rot_sb(8, [128, D], BF16)
        ps_m = P.rot_ps(2, [128, 1024], F32)
        ps_t = P.rot_ps(1, [128, 1024], F32)
        ps_r = P.rot_ps(2, [128, 512], F32)
        xTf = P.rot_sb(2, [128, 8, 128], F32)
        st = P.sb([128, 12], F32)
        mv = P.sb([128, 2], F32)
        rs = P.sb([128, 1], F32)
        scr = (st, Reg(), mv, Reg(), rs, Reg())
        lgs = P.rot_sb(2, [128, 4, 36], F32)
        sm = P.sb([128, 16, 4], F32)
        oh = P.sb([128, 4, 4], F32)
        eg = P.sb([128, 4, 4], F32)
        lem = P.sb([128, 4, 32], F32)
        o1 = P.sb([128, 4, 32], F32)
        o2 = P.sb([128, 4, 32], F32)
        mbf = P.sb([128, 4, 32], BF16)
        rf = P.sb([128, 4, 32], F32)
        tmp = P.sb([128, 4, 32], F32)
        rk = P.sb([128, 4, 2], F32)
        bs = P.sb([128, 4, 2], F32)
        ov = P.sb([128, 4, 2], F32)
        ps_ = P.sb([128, 4, 2], F32)
        pd = P.sb([128, 4, 2], F32)
        possi = P.rot_sb(2, [128, 4, 2], I32)
        rr = Reg()
        src = self.xsrc(l)
        f3 = lambda t: t[:].rearrange("p a b -> p (a b)")
        grp = {}

        def stage_a(t):
            g, tl = t // 4, t % 4
            if tl == 0:
                oT_t, o_regs = oT.next()
                for k in range(8):
                    S.dma("sp", oT_t[:, k, :], self.ot[k * 128:(k + 1) * 128, g * 512:(g + 1) * 512], [self.ot_r], [o_regs[k]])
                lg, lg_r = lgs.next()
                grp[g] = dict(oT=oT_t, oregs=o_regs, lg=lg, lg_r=lg_r, xb=[])
            G = grp[g]
            oT_t, o_regs = G["oT"], G["oregs"]
            x_t, x_r = xs.next()
            S.dma("sp", x_t[:], src[t * 128:(t + 1) * 128, :], [self.xres_r[2 * t], self.xres_r[2 * t + 1]], [x_r])
            pm, pm_r = ps_m.next()
            for hf in range(2):
                for k in range(8):
                    S.pe(lambda: nc.tensor.matmul(pm[:, hf * 512:(hf + 1) * 512], lhsT=oT_t[:, k, tl * 128:(tl + 1) * 128],
                                                  rhs=W[:, k, hf * 512:(hf + 1) * 512], start=(k == 0), stop=(k == 7)),
                         [o_regs[k], w_r[k]], [pm_r])
            y_t, y_r = ys.next()
            for hf in range(2):
                S.dve(lambda: V.scalar_tensor_tensor(out=y_t[:, hf * 512:(hf + 1) * 512],
                                                     in0=x_t[:, hf * 512:(hf + 1) * 512], scalar=ALPHA,
                                                     in1=pm[:, hf * 512:(hf + 1) * 512], op0=ALU.mult, op1=ALU.add),
                      [x_r, pm_r], [y_r])
            xa_t, xa_r = xa.next()
            self.layer_norm_tile(P, 128, y_t, y_r, g_bc, b_bc, gb_r, xa_t, xa_r, scr)
            S.dma("sp", self.xres[t * 128:(t + 1) * 128, :], xa_t[:, :], [xa_r],
                  [self.xres_r[2 * t], self.xres_r[2 * t + 1]])
            xb_t, xb_r = xbf.next()
            S.act(lambda: nc.scalar.copy(out=xb_t[:, :], in_=xa_t[:, :]), [xa_r], [xb_r])
            G["xb"].append((xb_t, xb_r))
            return (xa_t, xa_r)

        def stage_b(t, xa_t, xa_r):
            g, tl = t // 4, t % 4
            G = grp[g]
            lg, lg_r = G["lg"], G["lg_r"]
            pt, pt_r = ps_t.next()
            for k in range(8):
                S.pe(lambda: nc.tensor.transpose(pt[:, k * 128:(k + 1) * 128], xa_t[:, k * 128:(k + 1) * 128],
                                                 self.ident_f[:]), [xa_r], [pt_r])
            xf, xf_r = xTf.next()
            S.act(lambda: nc.scalar.copy(out=xf[:, 0:4, :], in_=pt[:, 0:512].rearrange("p (a b) -> p a b", a=4)),
                  [pt_r], [xf_r])
            S.dve(lambda: V.tensor_copy(out=xf[:, 4:8, :], in_=pt[:, 512:1024].rearrange("p (a b) -> p a b", a=4)),
                  [pt_r], [xf_r])
            pr, pr_r = ps_r.next()
            for k in range(8):
                S.pe(lambda: nc.tensor.matmul(pr[:, 0:36], lhsT=xf[:, k, :], rhs=rw[:, k, :], start=(k == 0), stop=(k == 7)),
                     [xf_r], [pr_r])
            S.dve(lambda: V.tensor_tensor(out=lg[:, tl, :], in0=pr[:, 0:36], in1=rb[:, :], op=ALU.add), [pr_r], [lg_r])

        pend = stage_a(0)
        for t in range(32):
            nxt = stage_a(t + 1) if t + 1 < 32 else None
            stage_b(t, *pend)
            pend = nxt
            if t % 4 != 3:
                continue
            g = t // 4
            lg, lg_r = grp[g]["lg"], grp[g]["lg_r"]
            xb_list = grp[g]["xb"]
            R_ = [lg_r, rr]
            LG = lg[:, :, 0:4]
            LE = lg[:, :, 4:36]
            bc4 = lambda j: sm[:, j, :].unsqueeze(2).to_broadcast([128, 4, 4])
            bc32 = lambda j: sm[:, j, :].unsqueeze(2).to_broadcast([128, 4, 32])
            S.dve(lambda: V.tensor_reduce(out=sm[:, 0, :], in_=LG, axis=AX.X, op=ALU.max), R_, [rr])
            S.dve(lambda: V.tensor_tensor(out=oh[:, :, :], in0=LG, in1=bc4(0), op=ALU.is_ge), R_, [rr])
            S.dve(lambda: V.tensor_tensor(out=eg[:, :, :], in0=LG, in1=bc4(0), op=ALU.subtract), R_, [rr])
            S.act(lambda: nc.scalar.activation(out=f3(eg), in_=f3(eg), func=AF.Exp), [rr], [rr])
            S.dve(lambda: V.tensor_reduce(out=sm[:, 1, :], in_=eg[:, :, :], axis=AX.X, op=ALU.add), [rr], [rr])
            S.dve(lambda: V.reciprocal(out=sm[:, 2, :], in_=sm[:, 1, :]), [rr], [rr])
            S.dve(lambda: V.tensor_scalar(out=f3(oh), in0=f3(oh), scalar1=-1.0, scalar2=BIG, op0=ALU.add, op1=ALU.mult),
                  [rr], [rr])
            S.dve(lambda: V.tensor_tensor(out=lem[:].rearrange("p a (g e) -> p (a g) e", g=4),
                                          in0=LE.rearrange("p a (g e) -> p a g e", g=4),
                                          in1=oh[:, :, :].unsqueeze(3).to_broadcast([128, 4, 4, 8]), op=ALU.add)
                  if False else
                  V.tensor_tensor(out=lem[:, :, :].rearrange("p a (g e) -> p a g e", g=4),
                                  in0=LE.rearrange("p a (g e) -> p a g e", g=4),
                                  in1=oh[:, :, :].unsqueeze(3).to_broadcast([128, 4, 4, 8]), op=ALU.add), R_, [rr])
            S.dve(lambda: V.tensor_reduce(out=sm[:, 3, :], in_=lem[:, :, :], axis=AX.X, op=ALU.max), [rr], [rr])
            S.dve(lambda: V.tensor_tensor(out=o1[:, :, :], in0=lem[:, :, :], in1=bc32(3), op=ALU.is_ge), [rr], [rr])
            S.dve(lambda: V.scalar_tensor_tensor(out=f3(lem), in0=f3(o1), scalar=-BIG, in1=f3(lem), op0=ALU.mult, op1=ALU.add),
                  [rr], [rr])
            S.dve(lambda: V.tensor_reduce(out=sm[:, 4, :], in_=lem[:, :, :], axis=AX.X, op=ALU.max), [rr], [rr])
            S.dve(lambda: V.tensor_tensor(out=o2[:, :, :], in0=lem[:, :, :], in1=bc32(4), op=ALU.is_ge), [rr], [rr])
            S.dve(lambda: V.tensor_tensor(out=sm[:, 5, :], in0=sm[:, 4, :], in1=sm[:, 3, :], op=ALU.subtract), [rr], [rr])
            S.act(lambda: nc.scalar.activation(out=sm[:, 6, :], in_=sm[:, 5, :], func=AF.Exp), [rr], [rr])
            S.dve(lambda: V.tensor_scalar(out=sm[:, 7, :], in0=sm[:, 6, :], scalar1=1.0, scalar2=None, op0=ALU.add), [rr], [rr])
            S.dve(lambda: V.reciprocal(out=sm[:, 8, :], in_=sm[:, 7, :]), [rr], [rr])
            pgr = self.pg_r[g]
            S.dve(lambda: V.tensor_tensor(out=self.g12[:, g * 4:(g + 1) * 4, 0], in0=sm[:, 8, :], in1=sm[:, 2, :], op=ALU.mult),
                  [rr], [pgr])
            S.dve(lambda: V.tensor_tensor(out=self.g12[:, g * 4:(g + 1) * 4, 1], in0=self.g12[:, g * 4:(g + 1) * 4, 0],
                                          in1=sm[:, 6, :], op=ALU.mult), [rr, pgr], [pgr])
            S.dve(lambda: V.tensor_tensor(out=f3(mbf), in0=f3(o1), in1=f3(o2), op=ALU.add), [rr], [rr])
            pp, pp_r = ps_r.next()
            S.pe(lambda: nc.tensor.matmul(pp[:, 0:128], lhsT=u_bf[:, :], rhs=f3(mbf), start=True, stop=True), [rr], [pp_r])
            S.pe(lambda: nc.tensor.matmul(pp[:, 128:256], lhsT=ones_bf[:, :], rhs=f3(mbf), start=True, stop=True), [rr], [pp_r])
            for tl in range(4):
                S.dve(lambda: V.tensor_tensor(out=rf[:, tl, :], in0=pp[:, tl * 32:(tl + 1) * 32], in1=carry[:, :], op=ALU.add),
                      [pp_r, car_r], [rr])
                S.dve(lambda: V.tensor_tensor(out=carry[:, :], in0=carry[:, :], in1=pp[:, 128 + tl * 32:128 + (tl + 1) * 32],
                                              op=ALU.add), [pp_r, car_r], [car_r])
            for kk, ok in enumerate((o1, o2)):
                S.dve(lambda: V.tensor_tensor(out=f3(tmp), in0=f3(ok), in1=f3(rf), op=ALU.mult), [rr], [rr])
                S.dve(lambda: V.tensor_reduce(out=rk[:, :, kk], in_=tmp[:, :, :], axis=AX.X, op=ALU.add), [rr], [rr])
                S.dve(lambda: V.tensor_tensor(out=f3(tmp), in0=f3(ok), in1=ecb[:, :], op=ALU.mult), [rr], [rr])
                S.dve(lambda: V.tensor_reduce(out=bs[:, :, kk], in_=tmp[:, :, :], axis=AX.X, op=ALU.add), [rr], [rr])
            S.dve(lambda: V.tensor_scalar(out=f3(ov), in0=f3(rk), scalar1=float(CAP), scalar2=None, op0=ALU.is_ge), [rr], [rr])
            S.dve(lambda: V.tensor_tensor(out=f3(ps_), in0=f3(rk), in1=f3(bs), op=ALU.add), [rr], [rr])
            S.dve(lambda: V.tensor_tensor(out=f3(pd), in0=pid[:, :], in1=f3(ps_), op=ALU.subtract), [rr], [rr])
            S.dve(lambda: V.tensor_tensor(out=f3(pd), in0=f3(pd), in1=f3(ov), op=ALU.mult), [rr], [rr])
            S.dve(lambda: V.tensor_tensor(out=f3(ps_), in0=f3(ps_), in1=f3(pd), op=ALU.add), [rr], [rr])
            S.dve(lambda: V.tensor_scalar(out=f3(ps_), in0=f3(ps_), scalar1=0.0, scalar2=float(NS + 127), op0=ALU.max,
                                          op1=ALU.min), [rr], [rr])
            S.dve(lambda: V.tensor_copy(out=self.posg[:, g * 4:(g + 1) * 4, :].rearrange("p a b -> p (a b)"), in_=f3(ps_)),
                  [rr], [pgr])
            for tl in range(4):
                xb_t, xb_r = xb_list[tl]
                for kk in range(2):
                    S.idma(self.xs[:, :], bass.IndirectOffsetOnAxis(ap=self.posg[:, g * 4 + tl, kk:kk + 1], axis=0), xb_t[:, :],
                           None, None, [xb_r, pgr], [self.xs_r])
        P.close()

    def phase_moe(self, l, last):
        nc, S = self.nc, self.S
        V = nc.vector
        P = Phase(S)
        g_bc = P.sb([128, D], F32)
        b_bc = P.sb([128, D], F32)
        S.dma("sp", g_bc[:], self.lng[l * 2 + 1], [], [Reg()])
        S.dma("sp", b_bc[:], self.lnb[l * 2 + 1], [], [Reg()])
        S.barrier()
        gb_r = Reg()
        W1 = P.rot_sb(2, [128, 8, 512], BF16)
        W3 = P.rot_sb(2, [128, 8, 512], BF16)
        W2 = P.rot_sb(2, [128, 4, D], BF16)
        S3 = P.rot_sb(1, [128, 8, 512], F32)
        S2 = P.rot_sb(1, [128, 4, D], F32)
        xsl = P.rot_sb(2, [128, 4, D], BF16)
        xgT = P.rot_sb(2, [128, 8, 512], BF16)
        ps_tr = P.rot_ps(2, [128, 1024], BF16)
        ps_h1 = P.rot_ps(2, [128, 512], F32)
        ps_h3 = P.rot_ps(2, [128, 512], F32)
        ps_y = P.rot_ps(2, [128, 512], F32)
        sl = P.rot_sb(2, [128, 512], F32)
        hT = P.rot_sb(2, [128, 4, 512], BF16)
        ysb = P.rot_sb(3, [128, D], F32)
        NB = CAP // 128
        fl = lambda t_: t_[:].rearrange("p a b -> p (a b)")
        wq = {}

        def fetch(e):
            w1_t, w1_r = W1.next()
            w3_t, w3_r = W3.next()
            w2_t, w2_r = W2.next()
            s3_t, s3_r = S3.next()
            s2_t, s2_r = S2.next()
            S.dma("pool", w1_t[:], self.w1[l, e].rearrange("(k p) f -> p k f", p=128), [], [w1_r])
            S.dma("sp", s3_t[:], self.w3[l, e].rearrange("(k p) f -> p k f", p=128), [], [s3_r])
            S.dma("sp", s2_t[:], self.w2[l, e].rearrange("(k p) f -> p k f", p=128), [], [s2_r])
            xs_t, xs_r = xsl.next()
            S.dma("sp", xs_t[:, 0:NB, :], self.xs[e * CAP:(e + 1) * CAP, :].rearrange("(b p) d -> p b d", p=128),
                  [self.xs_r], [xs_r])
            wq[e] = (w1_t, w1_r, w3_t, w3_r, w2_t, w2_r, s3_t, s3_r, s2_t, s2_r, xs_t, xs_r)

        def cast(e):
            (w1_t, w1_r, w3_t, w3_r, w2_t, w2_r, s3_t, s3_r, s2_t, s2_r, xs_t, xs_r) = wq[e]
            S.act(lambda: nc.scalar.copy(out=fl(w3_t), in_=fl(s3_t)), [s3_r], [w3_r])
            S.dve(lambda: V.tensor_copy(out=fl(w2_t), in_=fl(s2_t)), [s2_r], [w2_r])

        fetch(0)
        cast(0)
        for e in range(32):
            (w1_t, w1_r, w3_t, w3_r, w2_t, w2_r, s3_t, s3_r, s2_t, s2_r, xs_t, xs_r) = wq.pop(e)
            if e + 1 < 32:
                fetch(e + 1)
            xg, xg_r = xgT.next()
            for k2 in range(4):
                ptr, ptr_r = ps_tr.next()
                for kk in range(2):
                    k = k2 * 2 + kk
                    for bb in range(NB):
                        S.pe(lambda: nc.tensor.transpose(ptr[:, kk * 512 + bb * 128:kk * 512 + (bb + 1) * 128],
                                                         xs_t[:, bb, k * 128:(k + 1) * 128], self.ident_bf[:]), [xs_r], [ptr_r])
                if k2 % 2 == 0:
                    S.act(lambda: nc.scalar.copy(out=xg[:, k2 * 2:k2 * 2 + 2, :].rearrange("p a b -> p (a b)"), in_=ptr[:, :]),
                          [ptr_r], [xg_r])
                else:
                    S.dve(lambda: V.tensor_copy(out=xg[:, k2 * 2:k2 * 2 + 2, :].rearrange("p a b -> p (a b)"), in_=ptr[:, :]),
                          [ptr_r], [xg_r])
            h_t, h_r = hT.next()
            for fc in range(4):
                p1, p1_r = ps_h1.next()
                p3, p3_r = ps_h3.next()
                for k in range(8):
                    S.pe(lambda: nc.tensor.matmul(p1[:, :], lhsT=w1_t[:, k, fc * 128:(fc + 1) * 128], rhs=xg[:, k, :],
                                                  start=(k == 0), stop=(k == 7)), [w1_r, xg_r], [p1_r])
                for k in range(8):
                    S.pe(lambda: nc.tensor.matmul(p3[:, :], lhsT=w3_t[:, k, fc * 128:(fc + 1) * 128], rhs=xg[:, k, :],
                                                  start=(k == 0), stop=(k == 7)), [w3_r, xg_r], [p3_r])
                s_t, s_r = sl.next()
                S.act(lambda: nc.scalar.activation(out=s_t[:, :], in_=p1[:, :], func=AF.Silu), [p1_r], [s_r])
                S.dve(lambda: V.tensor_tensor(out=h_t[:, fc, :], in0=p3[:, :], in1=s_t[:, :], op=ALU.mult), [p3_r, s_r], [h_r])
            for st_ in range(NB):
                y_t, y_r = ysb.next()
                for dh in range(2):
                    py, py_r = ps_y.next()
                    for fc in range(4):
                        S.pe(lambda: nc.tensor.matmul(py[:, :], lhsT=h_t[:, fc, st_ * 128:(st_ + 1) * 128],
                                                      rhs=w2_t[:, fc, dh * 512:(dh + 1) * 512], start=(fc == 0), stop=(fc == 3)),
                             [h_r, w2_r], [py_r])
                    if dh == 0:
                        S.act(lambda: nc.scalar.copy(out=y_t[:, 0:512], in_=py[:, :]), [py_r], [y_r])
                    else:
                        S.dve(lambda: V.tensor_copy(out=y_t[:, 512:1024], in_=py[:, :]), [py_r], [y_r])
                S.dma("sp", self.yb[e * CAP + st_ * 128:e * CAP + (st_ + 1) * 128, :], y_t[:, :], [y_r], [self.yb_r])
            if e + 1 < 32:
                cast(e + 1)
        P.close()
        P = Phase(S)
        g_bc = P.sb([128, D], F32)
        b_bc = P.sb([128, D], F32)
        S.dma("sp", g_bc[:], self.lng[l * 2 + 1], [], [Reg()])
        S.dma("sp", b_bc[:], self.lnb[l * 2 + 1], [], [Reg()])
        S.barrier()
        xs2 = P.rot_sb(3, [128, D], F32)
        ya = P.rot_sb(4, [128, D], F32)
        ybb = P.rot_sb(4, [128, D], F32)
        ys = P.rot_sb(2, [128, D], F32)
        xo = P.rot_sb(2, [128, D], F32)
        st = P.sb([128, 12], F32)
        mv = P.sb([128, 2], F32)
        rs = P.sb([128, 1], F32)
        scr = (st, Reg(), mv, Reg(), rs, Reg())
        dst = self.out if last else self.xres
        for t in range(32):
            pgr = self.pg_r[t // 4]
            x_t, x_r = xs2.next()
            S.dma("sp", x_t[:], self.xres[t * 128:(t + 1) * 128, :], [self.xres_r[2 * t], self.xres_r[2 * t + 1]], [x_r])
            a_t, a_r = ya.next()
            b_t, b_r = ybb.next()
            S.idma(a_t[:, :], None, self.yb[:, :], bass.IndirectOffsetOnAxis(ap=self.posg[:, t, 0:1], axis=0), NS + 127,
                   [self.yb_r, pgr], [a_r])
            S.idma(b_t[:, :], None, self.yb[:, :], bass.IndirectOffsetOnAxis(ap=self.posg[:, t, 1:2], axis=0), NS + 127,
                   [self.yb_r, pgr], [b_r])
            y_t, y_r = ys.next()
            S.act(lambda: nc.scalar.activation(out=y_t[:, :], in_=x_t[:, :], func=AF.Copy, scale=ALPHA), [x_r], [y_r])
            S.dve(lambda: V.scalar_tensor_tensor(out=y_t[:, :], in0=a_t[:, :], scalar=self.g12[:, t, 0:1], in1=y_t[:, :],
                                                 op0=ALU.mult, op1=ALU.add), [a_r, pgr, y_r], [y_r])
            S.dve(lambda: V.scalar_tensor_tensor(out=y_t[:, :], in0=b_t[:, :], scalar=self.g12[:, t, 1:2], in1=y_t[:, :],
                                                 op0=ALU.mult, op1=ALU.add), [b_r, pgr, y_r], [y_r])
            xo_t, xo_r = xo.next()
            self.layer_norm_tile(P, 128, y_t, y_r, g_bc, b_bc, gb_r, xo_t, xo_r, scr, tail="dve")
            S.dma("sp", dst[t * 128:(t + 1) * 128, :], xo_t[:, :], [xo_r], [self.xres_r[2 * t], self.xres_r[2 * t + 1]])
        P.close()

    def dump(self, src_ap, shape, dt):
        nc, S = self.nc, self.S
        dbg = nc.dram_tensor("dbg", list(shape), dt, kind="ExternalOutput").ap()
        S.barrier()
        n = shape[0] // 128
        for i in range(n):
            S.dma("sp", dbg[i * 128:(i + 1) * 128, :], src_ap[i * 128:(i + 1) * 128, :], [], [Reg()])
        S.barrier()

    def init_scratch(self):
        nc, S = self.nc, self.S
        P = Phase(S)
        zb = P.sb([128, 8, D], BF16)
        zf = P.sb([128, D], F32)
        r = Reg()
        S.dve(lambda: nc.vector.memset(zb[:].rearrange("p a b -> p (a b)"), 0.0), [], [r])
        S.dve(lambda: nc.vector.memset(zf[:], 0.0), [], [r])
        for i in range(NS // 1024):
            S.dma("sp", self.xs[i * 1024:(i + 1) * 1024, :].rearrange("(p a) d -> p a d", p=128), zb[:, :, :], [r], [Reg()])
        S.dma("sp", self.xs[NS:NS + 128, :], zb[:, 0, :], [r], [Reg()])
        S.dma("sp", self.yb[NS:NS + 128, :], zf[:, :], [r], [Reg()])
        P.close()

    def build(self):
        dbg = self.debug
        self.init_scratch()
        for l in range(self.nlayers):
            if l % 2 == 0:
                self.phase_attn_proj(l)
                if dbg == "qkt%d" % l:
                    self.dump(self.qkt.rearrange("a b c -> (a b) c"), [2048, T], BF16)
                    return self.nc
                self.phase_attn(l)
                if dbg == "ot%d" % l:
                    self.dump(self.ot, [D, T], BF16)
                    return self.nc
                self.phase_out_ln_router(l, self.ab_w_out[l // 2])
            else:
                self.phase_hgrn(l)
                if dbg == "ot%d" % l:
                    self.dump(self.ot, [D, T], BF16)
                    return self.nc
                self.phase_out_ln_router(l, self.c_w_out[l // 2])
            if dbg == "xa%d" % l:
                self.dump(self.xres, [T, D], F32)
                return self.nc
            self.phase_moe(l, last=(l == self.nlayers - 1))
        self.S.barrier()
        return self.nc


def host_inputs(inputs):
    f = lambda a: np.ascontiguousarray(np.asarray(a, dtype=np.float32))
    d = {}
    d["ab_w_in"] = f(inputs["ab_w_in"])
    d["ab_w_out"] = f(inputs["ab_w_out"])
    d["c_w_in"] = f(inputs["c_w_in"])
    d["c_w_out"] = f(inputs["c_w_out"])
    d["exp_w1"] = f(inputs["exp_w1"])
    d["exp_w3"] = f(inputs["exp_w3"])
    d["exp_w2"] = f(inputs["exp_w2"])
    cng = np.tile(f(inputs["c_norm_g"]), (1, 8))
    d["cng"] = np.ascontiguousarray(np.broadcast_to(cng[:, None, :], (2, 64, D)))
    d["lbl"] = np.ascontiguousarray(np.broadcast_to(f(inputs["hgrn_lb_logits"])[:, None, :], (2, 64, D)))
    d["lng"] = np.ascontiguousarray(np.broadcast_to(f(inputs["ln_g"]).reshape(8, 1, D), (8, 128, D)))
    d["lnb"] = np.ascontiguousarray(np.broadcast_to(f(inputs["ln_b"]).reshape(8, 1, D), (8, 128, D)))
    d["rw"] = np.ascontiguousarray(np.concatenate([f(inputs["router_g_w"]), f(inputs["router_e_w"])], axis=2))
    rb = np.concatenate([f(inputs["router_g_b"]), f(inputs["router_e_b"])], axis=1)
    d["rb"] = np.ascontiguousarray(np.broadcast_to(rb[:, None, :], (4, 128, 36)))
    return d


_CACHE = {}


def kernel(**inputs):
    x = np.ascontiguousarray(np.asarray(inputs["x"], dtype=np.float32))
    if "prog" not in _CACHE:
        p = Prog()
        p.build()
        _CACHE["prog"] = p
    p = _CACHE["prog"]
    shared = host_inputs(inputs)
    for k, v in p.consts_np.items():
        shared["c_" + k] = v
    in_maps = []
    for c in range(8):
        m = dict(shared)
        m["x"] = x[c]
        in_maps.append(m)
    res = run_bass_kernel_spmd(p.nc, in_maps, core_ids=list(range(8)))
    return np.stack([np.asarray(r["out"], dtype=np.float32) for r in res.results], axis=0)
```
